# Optimizing a Trainium2 kernel written in Bass

```python
import math
import jax, jax.numpy as jnp
from jax import lax
import numpy as np

D_MODEL = 1024
BATCH = 4
SEQ = 8192
DEPTH = 1

CHUNK = 64
QBLK = 128
P_DIM = 256
MLA_HEADS = 8
Q_LORA = 256
KV_LORA = 128
NOPE_D = 64
ROPE_D = 32
V_D = 64
ROPE_THETA = 10000.0
ML_HEADS = 4
ML_HD = 128
ML_W = ML_HEADS * ML_HD
CONV_K = 4
MIX_W = MLA_HEADS * V_D + ML_W
IN_W = Q_LORA + KV_LORA + ROPE_D + ML_W + ML_W + ML_HEADS + ML_HEADS
N_GROUPS = 4
EXPERTS_PER_GROUP = 8
N_EXPERTS = N_GROUPS * EXPERTS_PER_GROUP
TOP_K = 2
D_EXPERT = 256
MOE_BLK = 128
EPS = 1e-6

kernel_name = 'hybrid_mla_mlstm_hier_moe'


def _rmsnorm(x, g):
    xf = x.astype(jnp.float32)
    r = lax.rsqrt(jnp.mean(xf * xf, axis=-1, keepdims=True) + EPS)
    return (xf * r * g.astype(jnp.float32)).astype(x.dtype)


def _split_in(proj):
    sizes = (Q_LORA, KV_LORA, ROPE_D, ML_W, ML_W, ML_HEADS, ML_HEADS)
    offs = np.cumsum(sizes)[:-1].tolist()
    return jnp.split(proj, offs, axis=-1)


def _rope_cs(positions):
    inv = 1.0 / (ROPE_THETA ** (jnp.arange(0, ROPE_D, 2, dtype=jnp.float32) / ROPE_D))
    ang = positions.astype(jnp.float32)[..., None] * inv
    return jnp.cos(ang), jnp.sin(ang)


def _apply_rope(x, cos, sin):
    xf = x.astype(jnp.float32)
    x1, x2 = xf[..., :ROPE_D // 2], xf[..., ROPE_D // 2:]
    return jnp.concatenate([x1 * cos - x2 * sin, x1 * sin + x2 * cos], axis=-1).astype(x.dtype)


def _mla_attention(q_nope, q_rope, k_nope, k_rope, v):
    B, S = q_nope.shape[:2]
    nq = S // QBLK
    scale = 1.0 / math.sqrt(NOPE_D + ROPE_D)
    k_chunk = jnp.arange(S) // CHUNK

    def blocks(t):
        return t.reshape(B, nq, QBLK, MLA_HEADS, t.shape[-1]).transpose(1, 0, 3, 2, 4)

    def one(args):
        qn, qr, blk = args
        s = (jnp.einsum('bhqd,bkhd->bhqk', qn, k_nope)
             + jnp.einsum('bhqr,bkr->bhqk', qr, k_rope)).astype(jnp.float32) * scale
        q_chunk = (blk * QBLK + jnp.arange(QBLK)) // CHUNK
        mask = k_chunk[None, :] <= q_chunk[:, None]
        s = jnp.where(mask, s, -1e30)
        pr = jax.nn.softmax(s, axis=-1).astype(v.dtype)
        return jnp.einsum('bhqk,bkhv->bhqv', pr, v)

    o = lax.map(one, (blocks(q_nope), blocks(q_rope), jnp.arange(nq)))
    return o.transpose(1, 0, 3, 2, 4).reshape(B, S, MLA_HEADS * V_D)


def _causal_conv(x, w, b):
    S = x.shape[1]
    xp = jnp.pad(x, ((0, 0), (CONV_K - 1, 0), (0, 0)))
    return sum(xp[:, k:k + S] * w[k] for k in range(CONV_K)) + b


def _mlstm_chunkwise(q, k, v, i_pre, f_pre):
    B, H, S, dk = q.shape
    dv = v.shape[-1]
    L = CHUNK
    nc = S // L

    def chunks(t):
        return t.reshape(B, H, nc, L, t.shape[-1]).transpose(2, 0, 1, 3, 4).astype(jnp.float32)

    qc, kc, vc = chunks(q), chunks(k), chunks(v)
    ic = i_pre.astype(jnp.float32).reshape(B, H, nc, L).transpose(2, 0, 1, 3)
    logf = jax.nn.log_sigmoid(f_pre.astype(jnp.float32)).reshape(B, H, nc, L).transpose(2, 0, 1, 3)
    bc = jnp.cumsum(logf, axis=-1)
    tril = jnp.tril(jnp.ones((L, L), dtype=bool))

    def step(carry, inp):
        C, n, m = carry
        q_, k_, v_, i_, b_ = inp
        D = b_[..., :, None] - b_[..., None, :] + i_[..., None, :]
        D = jnp.where(tril, D, -jnp.inf)
        inter = b_ + m[..., None]
        m_t = jnp.maximum(jnp.max(D, axis=-1), inter)
        w_intra = jnp.exp(D - m_t[..., None])
        w_inter = jnp.exp(inter - m_t)
        qk = jnp.einsum('bhtd,bhsd->bhts', q_, k_) * w_intra
        num = jnp.einsum('bhts,bhsv->bhtv', qk, v_) + w_inter[..., None] * jnp.einsum('bhtd,bhdv->bhtv', q_, C)
        den = jnp.sum(qk, axis=-1) + w_inter * jnp.einsum('bhtd,bhd->bht', q_, n)
        den = jnp.maximum(jnp.abs(den), jnp.exp(-m_t))
        h = num / den[..., None]
        bL = b_[..., -1]
        w_s = bL[..., None] - b_ + i_
        m_new = jnp.maximum(bL + m, jnp.max(w_s, axis=-1))
        ws = jnp.exp(w_s - m_new[..., None])
        decay = jnp.exp(bL + m - m_new)
        C_new = decay[..., None, None] * C + jnp.einsum('bhs,bhsd,bhsv->bhdv', ws, k_, v_)
        n_new = decay[..., None] * n + jnp.einsum('bhs,bhsd->bhd', ws, k_)
        return (C_new, n_new, m_new), h

    init = (jnp.zeros((B, H, dk, dv), jnp.float32), jnp.zeros((B, H, dk), jnp.float32),
            jnp.zeros((B, H), jnp.float32))
    _, hs = lax.scan(step, init, (qc, kc, vc, ic, bc))
    return hs.transpose(1, 2, 0, 3, 4).reshape(B, H, S, dv)


def _hier_moe(h, w_rg, b_rg, w_re, b_re, w_g, w_u, w_d):
    B, S, D = h.shape
    T = B * S
    hf = h.reshape(T, D)
    g_prob = jax.nn.softmax((hf @ w_rg).astype(jnp.float32) + b_rg.astype(jnp.float32), axis=-1)
    g_top, g_idx = lax.top_k(g_prob, 1)
    e_logits = ((hf @ w_re).astype(jnp.float32) + b_re.astype(jnp.float32)).reshape(T, N_GROUPS, EXPERTS_PER_GROUP)
    e_in = jnp.take_along_axis(e_logits, g_idx[:, :, None], axis=1)[:, 0]
    e_top, e_idx = lax.top_k(jax.nn.softmax(e_in, axis=-1), TOP_K)
    wts = g_top * e_top / jnp.sum(e_top, axis=-1, keepdims=True)
    eid = g_idx * EXPERTS_PER_GROUP + e_idx
    M = T * TOP_K
    P = M + N_EXPERTS * MOE_BLK
    nb = P // MOE_BLK
    flat_e = eid.reshape(M).astype(jnp.int32)
    flat_w = wts.reshape(M)
    flat_tok = jnp.repeat(jnp.arange(T, dtype=jnp.int32), TOP_K)
    order = jnp.argsort(flat_e)
    sorted_e = flat_e[order]
    counts = jnp.zeros((N_EXPERTS,), jnp.int32).at[flat_e].add(1)
    starts = jnp.cumsum(counts) - counts
    padded = (counts + MOE_BLK - 1) // MOE_BLK * MOE_BLK
    pad_ends = jnp.cumsum(padded)
    pad_starts = pad_ends - padded
    dest = pad_starts[sorted_e] + (jnp.arange(M, dtype=jnp.int32) - starts[sorted_e])
    row_tok = jnp.full((P,), T, jnp.int32).at[dest].set(flat_tok[order])
    row_w = jnp.zeros((P,), jnp.float32).at[dest].set(flat_w[order])
    block_e = jnp.minimum(jnp.searchsorted(pad_ends, jnp.arange(nb, dtype=jnp.int32) * MOE_BLK, side='right'),
                          N_EXPERTS - 1)
    x_pad = jnp.concatenate([hf, jnp.zeros((1, D), hf.dtype)], axis=0)
    x_rows = x_pad[row_tok].reshape(nb, MOE_BLK, D)

    def expert(args):
        xb, e = args
        return (jax.nn.silu(xb @ w_g[e]) * (xb @ w_u[e])) @ w_d[e]

    y = lax.map(expert, (x_rows, block_e)).reshape(P, D)
    out = jnp.zeros((T + 1, D), jnp.float32).at[row_tok].add(y.astype(jnp.float32) * row_w[:, None])[:T]
    return out.reshape(B, S, D).astype(h.dtype)


def setup_inputs(seed: int = 0) -> dict:
    key = jax.random.key(seed)
    ks = jax.random.split(key, 40)
    f32 = jnp.float32

    def nrm(k, shape, fan_in):
        return jax.random.normal(k, shape, f32) * (fan_in ** -0.5)

    def gain(k, shape):
        return 1.0 + 0.05 * jax.random.normal(k, shape, f32)

    x = jax.random.normal(ks[0], (BATCH, SEQ, D_MODEL), f32)
    p = jax.random.normal(ks[1], (DEPTH, BATCH, SEQ, P_DIM), f32)
    positions = (jax.random.randint(ks[2], (BATCH, 1), 0, 4096, dtype=jnp.int32)
                 + jnp.arange(SEQ, dtype=jnp.int32)[None, :])
    return {
        'x': x,
        'p': p,
        'positions': positions,
        'norm_mix_g': gain(ks[3], (DEPTH, D_MODEL)),
        'w_in': nrm(ks[4], (DEPTH, D_MODEL, IN_W), D_MODEL),
        'q_norm_g': gain(ks[5], (DEPTH, Q_LORA)),
        'w_uq': nrm(ks[6], (DEPTH, Q_LORA, MLA_HEADS * (NOPE_D + ROPE_D)), Q_LORA),
        'kv_norm_g': gain(ks[7], (DEPTH, KV_LORA)),
        'w_ukv': nrm(ks[8], (DEPTH, KV_LORA, MLA_HEADS * (NOPE_D + V_D)), KV_LORA),
        'conv_w': nrm(ks[9], (DEPTH, CONV_K, ML_W), CONV_K),
        'conv_b': 0.02 * jax.random.normal(ks[10], (DEPTH, ML_W), f32),
        'w_mq': nrm(ks[11], (DEPTH, ML_HEADS, ML_HD, ML_HD), ML_HD),
        'w_mk': nrm(ks[12], (DEPTH, ML_HEADS, ML_HD, ML_HD), ML_HD),
        'w_mv': nrm(ks[13], (DEPTH, ML_HEADS, ML_HD, ML_HD), ML_HD),
        'b_igate': 0.1 * jax.random.normal(ks[14], (DEPTH, ML_HEADS), f32),
        'b_fgate': jnp.linspace(3.0, 6.0, ML_HEADS, dtype=f32)[None, :] + 0.1 * jax.random.normal(ks[15], (DEPTH, ML_HEADS), f32),
        'mh_norm_g': gain(ks[16], (DEPTH, ML_W)),
        'ml_skip': gain(ks[17], (DEPTH, ML_W)),
        'w_o': nrm(ks[18], (DEPTH, MIX_W, D_MODEL), MIX_W),
        'norm_ffn_g': gain(ks[19], (DEPTH, D_MODEL)),
        'w_router_group': nrm(ks[20], (DEPTH, D_MODEL, N_GROUPS), D_MODEL),
        'b_router_group': 0.01 * jax.random.normal(ks[21], (DEPTH, N_GROUPS), f32),
        'w_router_expert': nrm(ks[22], (DEPTH, D_MODEL, N_EXPERTS), D_MODEL),
        'b_router_expert': 0.01 * jax.random.normal(ks[23], (DEPTH, N_EXPERTS), f32),
        'w_gate_e': nrm(ks[24], (DEPTH, N_EXPERTS, D_MODEL, D_EXPERT), D_MODEL),
        'w_up_e': nrm(ks[25], (DEPTH, N_EXPERTS, D_MODEL, D_EXPERT), D_MODEL),
        'w_down_e': nrm(ks[26], (DEPTH, N_EXPERTS, D_EXPERT, D_MODEL), D_EXPERT),
        'norm_ple_g': gain(ks[27], (DEPTH, D_MODEL)),
        'w_ple': nrm(ks[28], (DEPTH, P_DIM, D_MODEL), P_DIM),
        'w_ple_gate': nrm(ks[29], (DEPTH, D_MODEL, D_MODEL), D_MODEL),
        'final_norm_g': gain(ks[30], (D_MODEL,)),
    }


def reference(x, p, positions, norm_mix_g, w_in, q_norm_g, w_uq, kv_norm_g, w_ukv, conv_w, conv_b,
              w_mq, w_mk, w_mv, b_igate, b_fgate, mh_norm_g, ml_skip, w_o, norm_ffn_g,
              w_router_group, b_router_group, w_router_expert, b_router_expert,
              w_gate_e, w_up_e, w_down_e, norm_ple_g, w_ple, w_ple_gate, final_norm_g):
    B, S, _ = x.shape
    cos, sin = _rope_cs(positions)
    for i in range(DEPTH):
        h = _rmsnorm(x, norm_mix_g[i])
        c_q, c_kv, k_r, x_m, z, i_pre, f_pre = _split_in(h @ w_in[i])
        q = (_rmsnorm(c_q, q_norm_g[i]) @ w_uq[i]).reshape(B, S, MLA_HEADS, NOPE_D + ROPE_D)
        kv = (_rmsnorm(c_kv, kv_norm_g[i]) @ w_ukv[i]).reshape(B, S, MLA_HEADS, NOPE_D + V_D)
        q_nope, q_rope = q[..., :NOPE_D], _apply_rope(q[..., NOPE_D:], cos[:, :, None], sin[:, :, None])
        k_nope, v = kv[..., :NOPE_D], kv[..., NOPE_D:]
        k_rope = _apply_rope(k_r, cos, sin)
        attn_out = _mla_attention(q_nope, q_rope, k_nope, k_rope, v)
        xc = jax.nn.silu(_causal_conv(x_m, conv_w[i], conv_b[i]))
        xch = xc.reshape(B, S, ML_HEADS, ML_HD)
        xmh = x_m.reshape(B, S, ML_HEADS, ML_HD)
        mq = jnp.einsum('bshd,hde->bhse', xch, w_mq[i])
        mk = jnp.einsum('bshd,hde->bhse', xch, w_mk[i]) * (ML_HD ** -0.5)
        mv = jnp.einsum('bshd,hde->bhse', xmh, w_mv[i])
        ig = (i_pre + b_igate[i]).transpose(0, 2, 1)
        fg = (f_pre + b_fgate[i]).transpose(0, 2, 1)
        hm = _mlstm_chunkwise(mq, mk, mv, ig, fg).transpose(0, 2, 1, 3)
        mu = jnp.mean(hm, axis=-1, keepdims=True)
        var = jnp.mean(jnp.square(hm - mu), axis=-1, keepdims=True)
        hm = ((hm - mu) * lax.rsqrt(var + EPS)).reshape(B, S, ML_W) * mh_norm_g[i].astype(jnp.float32)
        ml_out = ((hm.astype(x.dtype) + ml_skip[i] * xc) * jax.nn.silu(z)).astype(x.dtype)
        x = x + jnp.concatenate([attn_out, ml_out], axis=-1) @ w_o[i]
        x = x + _hier_moe(_rmsnorm(x, norm_ffn_g[i]), w_router_group[i], b_router_group[i],
                          w_router_expert[i], b_router_expert[i], w_gate_e[i], w_up_e[i], w_down_e[i])
        hp = _rmsnorm(x, norm_ple_g[i])
        x = x + (p[i] @ w_ple[i]) * jax.nn.sigmoid(hp @ w_ple_gate[i])
    return _rmsnorm(x, final_norm_g)
```

```python
import numpy as np
from contextlib import ExitStack
import concourse.bass as bass
import concourse.mybir as mybir
from concourse.bass_utils import run_bass_kernel_spmd

F32 = mybir.dt.float32
BF16 = mybir.dt.bfloat16
I32 = mybir.dt.int32
ALU = mybir.AluOpType
AF = mybir.ActivationFunctionType
AX = mybir.AxisListType

NDS = 12
EPS = 1e-6
CAP = 384
NEXP = 32
GB = 4
C_CKV, C_XM, C_MISC, C_GI, C_GF, C_CQ, C_Z, NCOL = 0, 128, 640, 736, 740, 744, 1000, 1512
QSCALE = 1.0 / np.sqrt(96.0)


class Prog:
    def __init__(self, nc):
        self.nc = nc
        self.ops = []
        self.last_w = {}
        self.readers = {}
        self.dma_count = {}
        self.dma_ops = {}

    def add(self, eng, fn, reads=(), writes=(), dma=False):
        i = len(self.ops)
        deps = set()
        ex = [r for r in reads if isinstance(r, tuple) and isinstance(r[0], str) and r[0].startswith('ps')]
        if ex:
            reads = [r for r in reads if r not in ex]
            writes = list(writes) + ex
        for r in reads:
            if r in self.last_w:
                deps.add(self.last_w[r])
        for w in writes:
            if w in self.last_w:
                deps.add(self.last_w[w])
            deps.update(self.readers.get(w, ()))
        for r in reads:
            self.readers.setdefault(r, []).append(i)
        for w in writes:
            self.last_w[w] = i
            self.readers[w] = []
        deps.discard(i)
        op = dict(eng=eng, fn=fn, deps=deps, dma=dma, sig=None)
        if dma:
            lst = self.dma_ops.setdefault(eng, [])
            op['dk'] = len(lst)
            if len(lst) >= NDS:
                deps.add(lst[len(lst) - NDS])
            lst.append(i)
        self.ops.append(op)
        return i

    def barrier(self):
        engs = ['pe', 'act', 'dve', 'pool', 'sp']
        last = []
        for e in engs:
            for j in range(len(self.ops) - 1, -1, -1):
                o = self.ops[j]
                if o['eng'] == e and not o['dma'] and o['fn'] is not None:
                    last.append(j)
                    break
        for e, lst in self.dma_ops.items():
            last.extend(lst[-NDS:])
        for e in engs:
            i = len(self.ops)
            self.ops.append(dict(eng=e, fn=None, deps=set(last), dma=False, sig=None))

    def emit(self, stack):
        nc = self.nc
        ops = self.ops
        engs = ['pe', 'act', 'dve', 'pool', 'sp']
        csem = {e: stack.enter_context(nc.semaphore('c_' + e)) for e in engs}
        dsem = {e: [stack.enter_context(nc.semaphore('d_%s_%d' % (e, k))) for k in range(NDS)]
                for e in self.dma_ops}
        need = [False] * len(ops)
        for o in ops:
            for d in o['deps']:
                p = ops[d]
                if (not p['dma']) and (not o['dma']) and p['eng'] == 'pe' and o['eng'] == 'pe' and o['fn'] is not None:
                    continue
                need[d] = True
        cnt = {e: 0 for e in engs}
        for i, o in enumerate(ops):
            if o['dma']:
                k = o['dk']
                o['sig'] = (dsem[o['eng']][k % NDS], 16 * (k // NDS + 1), 16)
            elif need[i] and o['fn'] is not None:
                cnt[o['eng']] += 1
                o['sig'] = (csem[o['eng']], cnt[o['eng']], 1)
        per = {e: [o for o in ops if o['eng'] == e] for e in engs}

        def run(ename, eobj):
            waited = {}
            for o in per[ename]:
                for d in sorted(o['deps']):
                    p = ops[d]
                    if p['sig'] is None:
                        continue
                    if (not p['dma']) and (not o['dma']) and p['eng'] == 'pe' and ename == 'pe' and o['fn'] is not None:
                        continue
                    sem, val, _ = p['sig']
                    if waited.get(sem.name, 0) < val:
                        eobj.wait_ge(sem, val)
                        waited[sem.name] = val
                if o['fn'] is None:
                    continue
                ins = o['fn'](eobj)
                if o['sig'] is not None:
                    sem, val, inc = o['sig']
                    ins.then_inc(sem, inc)

        with nc.Block() as block:
            @block.tensor
            def _(e):
                run('pe', e)

            @block.scalar
            def _(e):
                run('act', e)

            @block.vector
            def _(e):
                run('dve', e)

            @block.gpsimd
            def _(e):
                run('pool', e)

            @block.sync
            def _(e):
                run('sp', e)


class Ring:
    def __init__(self, kb, stack, name, shape, dtype, n):
        self.t = [stack.enter_context(kb.nc.sbuf_tensor('s_%s_%d' % (name, k), shape, dtype)) for k in range(n)]
        self.name = name
        self.i = 0

    def next(self):
        k = self.i % len(self.t)
        self.i += 1
        return self.t[k], (self.name, k)


class KB:
    def __init__(self, nc):
        self.nc = nc
        self.P = Prog(nc)
        self.psi = 0
        self.ps = None

    def sb(self, st, name, shape, dt):
        return st.enter_context(self.nc.sbuf_tensor('s_' + name, shape, dt))

    def psum(self):
        k = self.psi % 8
        self.psi += 1
        return self.ps[k], ('ps', k)

    def dma(self, out, in_, r=(), w=(), q='sp'):
        self.P.add(q, lambda e: e.dma_start(out=out, in_=in_), r, w, dma=True)

    def act(self, out, in_, func, r, w, bias=None, scale=None, accum=None):
        kw = {}
        if bias is not None:
            kw['bias'] = bias
        if scale is not None:
            kw['scale'] = scale
        if accum is not None:
            kw['accum_out'] = accum
        self.P.add('act', lambda e: e.activation(out=out, in_=in_, func=func, **kw), r, w)

    def tt(self, eng, out, in0, in1, op, r, w):
        self.P.add(eng, lambda e: e.tensor_tensor(out=out, in0=in0, in1=in1, op=op), r, w)

    def ts(self, eng, out, in0, s1, s2, op0, op1, r, w):
        if s2 is None:
            self.P.add(eng, lambda e: e.tensor_scalar(out=out, in0=in0, scalar1=s1, scalar2=None, op0=op0), r, w)
        else:
            self.P.add(eng, lambda e: e.tensor_scalar(out=out, in0=in0, scalar1=s1, scalar2=s2, op0=op0, op1=op1), r, w)

    def stt(self, out, in0, scalar, in1, op0, op1, r, w):
        self.P.add('dve', lambda e: e.scalar_tensor_tensor(out=out, in0=in0, scalar=scalar, in1=in1, op0=op0, op1=op1), r, w)

    def copy(self, eng, out, in_, r, w):
        if eng == 'act':
            self.P.add('act', lambda e: e.copy(out=out, in_=in_), r, w)
        else:
            self.P.add(eng, lambda e: e.tensor_copy(out=out, in_=in_), r, w)

    def memset(self, eng, ap, val, w):
        self.P.add(eng, lambda e: e.memset(ap, val), (), w)

    def mm(self, out, lhsT, rhs, start, stop, r, w, skip=False):
        if skip:
            self.P.add('pe', lambda e: e.matmul(out, lhsT, rhs, start=start, stop=stop, skip_group_check=True), r, w)
        else:
            self.P.add('pe', lambda e: e.matmul(out, lhsT, rhs, start=start, stop=stop), r, w)

    def tr(self, out, in_, ident, r, w):
        self.P.add('pe', lambda e: e.transpose(out=out, in_=in_, identity=ident), r, w)

    def scan(self, out, d0, d1, init, op0, op1, r, w):
        self.P.add('dve', lambda e: e.tensor_tensor_scan(out=out, data0=d0, data1=d1, initial=init, op0=op0, op1=op1), r, w)


def bc(ap, shape):
    return ap.broadcast_to(shape)


def build(NVB, stop_after=None, dbg=False):
    VT = NVB * 128
    NOB = NVB // 2
    OT = NOB * 128
    NG = NVB // GB
    GT = GB * 128
    GO = GT // 2
    nc = bass.Bass("TRN2", target_bir_lowering=False)
    D = {}

    def din(name, shape, dt=F32):
        D[name] = nc.dram_tensor(name, list(shape), dt, kind="ExternalInput").ap()
        return D[name]

    def dscr(name, shape, dt):
        D[name] = nc.dram_tensor(name, list(shape), dt, kind="Internal").ap()
        return D[name]

    def dout(name, shape, dt=F32):
        D[name] = nc.dram_tensor(name, list(shape), dt, kind="ExternalOutput").ap()
        return D[name]

    xv = din('xv', [VT, 1024]); posk = din('posk', [32, VT], I32); posq = din('posq', [32, OT], I32)
    pvd = din('pv', [OT, 256])
    valid4 = din('valid4', [4, VT]); ibias4 = din('ibias4', [4, VT]); kbias = din('kbias', [1, VT])
    ident_d = din('ident', [128, 128]); tril_d = din('trilT', [128, 128]); attm_d = din('attmask', [128, 128])
    perm_d = din('permB', [96, 96]); invf_d = din('invfreq', [96, 1]); dmask_d = din('dmask', [4, 4 * GB])
    ustr_d = din('ustrict', [128, 128]); din('iota32', [128, 32])
    w_in_d = din('w_in_r', [1024, NCOL]); gmix_d = din('gmix_col', [128, 8])
    gq_d = din('gq_col', [128, 2]); gkv_d = din('gkv_col', [128, 1])
    w_uq_d = din('w_uq', [256, 768]); w_ukv_d = din('w_ukv', [128, 1024])
    convw_d = din('convw_col', [128, 16]); convb_d = din('convb_col', [128, 4])
    w_mq_d = din('w_mq', [4, 128, 128]); w_mk_d = din('w_mk', [4, 128, 128]); w_mv_d = din('w_mv', [4, 128, 128])
    bi_d = din('bi_col', [4, 1]); bf_d = din('bf_col', [4, 1])
    gmh_d = din('gmh_col', [128, 4]); skip_d = din('skip_col', [128, 4])
    w_o_d = din('w_o', [1024, 1024]); gffn_d = din('gffn_rep', [128, 1024])
    w_rt_d = din('w_router', [1024, 36]); b_rt_d = din('b_router_rep', [128, 36])
    w_ge_d = din('w_gate_e', [NEXP, 1024, 256]); w_ue_d = din('w_up_e', [NEXP, 1024, 256]); w_de_d = din('w_down_e', [NEXP, 256, 1024])
    gple_d = din('gple_rep', [128, 1024]); w_ple_d = din('w_ple', [256, 1024]); w_pg_d = din('w_ple_gate', [1024, 1024])
    gfin_d = din('gfin_rep', [128, 1024])
    out_d = dout('out', [OT, 1024])
    mlo_s = dscr('mlo_s', [4, 128, OT], BF16)
    x1_s = dscr('x1_s', [OT, 1024], F32)
    xs_s = dscr('xs_s', [NEXP * CAP, 1024], BF16)
    ys_s = dscr('ys_s', [NEXP * CAP, 1024], F32)
    cq_s = dscr('cq_s', [2, 128, OT], BF16); ckv_s = dscr('ckv_s', [128, VT], BF16)
    kr_s = dscr('kr_s', [32, VT], BF16); tab_s = dscr('tab_s', [2, 32, OT], F32)
    dbgo = {}

    def dbg_out(name, shape, dt=F32):
        dbgo[name] = dout(name, shape, dt)
        return dbgo[name]

    dbg_mlo = dbg_out('d_mlo', [4, 128, OT], BF16) if dbg else None
    kb = KB(nc)
    P = kb.P
    with ExitStack() as top:
        sb = lambda name, shape, dt: kb.sb(top, name, shape, dt)
        identF = sb('identF', [128, 128], F32); identB = sb('identB', [128, 128], BF16)
        trilT = sb('trilT', [128, 128], F32); attm = sb('attm', [128, 128], BF16)
        permB = sb('permB', [96, 96], F32); invf = sb('invf', [96, 1], F32)
        dmask = sb('dmask', [4, 4 * GB], F32); onesF = sb('onesF', [128, 128], F32)
        mhalf = sb('mhalf', [128, 1], F32); epsc = sb('epsc', [128, 1], F32)
        for t, d in ((identF, ident_d), (trilT, tril_d), (permB, perm_d), (invf, invf_d), (dmask, dmask_d)):
            kb.dma(t[:], d, w=[t.name[2:]])
        kb.dma(attm[:], attm_d, w=['attm'], q='pool')
        kb.copy('dve', identB[:], identF[:], ['identF'], ['identB'])
        kb.memset('pool', onesF[:], 1.0, ['onesF'])
        kb.memset('pool', mhalf[:], -0.5, ['mhalf'])
        kb.memset('pool', epsc[:], EPS, ['epsc'])
        zt = sb('zt', [128, 2, 1024], BF16)
        kb.memset('pool', zt[:], 0.0, ['zt'])
        C1 = 6.28125
        C2 = 2.0 * np.pi - 6.28125
        MAGIC = 12582912.0

        with ExitStack() as sa:
            sba = lambda name, shape, dt: kb.sb(sa, name, shape, dt)
            kb.ps = [sa.enter_context(nc.psum_tensor('psA%d' % k, [128, 512], F32)) for k in range(8)]
            win = sba('win', [128, 8, NCOL], BF16)
            gmix = sba('gmix', [128, 8], F32); gq = sba('gq', [128, 2], F32); gkv = sba('gkv', [128, 1], F32)
            convw = sba('convw', [128, 16], F32); convb = sba('convb', [128, 4], F32)
            cdiag = sba('cdiag', [128, 16, 128], F32)
            wmq = sba('wmq', [128, 4, 128], BF16); wmk = sba('wmk', [128, 4, 128], BF16); wmv = sba('wmv', [128, 4, 128], BF16)
            bi = sba('bi', [4, 1], F32); bfc = sba('bfc', [4, 1], F32); nbf = sba('nbf', [4, 1], F32)
            gmh = sba('gmh', [128, 4], F32); skipc = sba('skipc', [128, 4], F32)
            ones4 = sba('ones4', [4, 128], F32); onesr = sba('onesr', [4, GT], F32)
            for t, d in ((gmix, gmix_d), (gq, gq_d), (gkv, gkv_d), (convw, convw_d), (convb, convb_d),
                         (bi, bi_d), (bfc, bf_d), (gmh, gmh_d), (skipc, skip_d)):
                kb.dma(t[:], d, w=[t.name[2:]])
            kb.ts('pool', nbf[:], bfc[:], -1.0, 0.0, ALU.mult, ALU.add, ['bfc'], ['nbf'])
            kb.memset('pool', ones4[:], 1.0, ['ones4'])
            kb.memset('pool', onesr[:], 1.0, ['onesr'])
            for t, d in ((wmq, w_mq_d), (wmk, w_mk_d), (wmv, w_mv_d)):
                kb.dma(t[:], d.rearrange("h d e -> d h e"), w=[t.name[2:]], q='pool')
            with ExitStack() as sw:
                stg = Ring(kb, sw, 'wstg', [128, NCOL], F32, 8)
                w_in_v = w_in_d.rearrange("(c p) n -> p c n", p=128)
                for c in range(8):
                    t, k = stg.next()
                    kb.dma(t[:], w_in_v[:, c, :], w=[k])
                    kb.ts('dve', win[:, c, :], t[:], gmix[:, c:c + 1], None, ALU.mult, None, [k, 'gmix'], ['win'])
            P.barrier()
            zf_todo = list(range(0, NEXP * CAP, 256))

            def zero_fill(n):
                for _ in range(n):
                    if zf_todo:
                        r0 = zf_todo.pop(0)
                        kb.dma(xs_s[r0:r0 + 256, :].rearrange('(p k) d -> p k d', k=2), zt[:], r=['zt'], w=['xs_s'], q='pool')
            for j in range(16):
                kb.ts('pool', cdiag[:, j, :], identF[:], convw[:, j:j + 1], 0.0, ALU.mult, ALU.add, ['identF', 'convw'], ['cdiag'])

            xt_r = Ring(kb, sa, 'xt', [128, 1024], F32, 4)
            hb_r = Ring(kb, sa, 'hb', [128, 1024], BF16, 3)
            st_r = Ring(kb, sa, 'st', [128, 4], F32, 4)
            hT_r = Ring(kb, sa, 'hT', [128, 8, GT], BF16, 1)
            sq_r = Ring(kb, sa, 'sq', [128, GT], F32, 2)
            rv_r = Ring(kb, sa, 'rv', [128, GT], F32, 2)
            lat_r = Ring(kb, sa, 'lat', [128, GT], BF16, 2)
            xmF = sba('xmF', [128, 4, 4 + GT], F32)
            krF = sba('krF', [96, GT], F32)
            rt1 = sba('rt1', [96, GT], F32); rt2 = sba('rt2', [96, GT], F32); krB = sba('krB', [96, GT], BF16)
            tab_r = Ring(kb, sa, 'tab', [96, 2, GT], F32, 2)
            posi = sba('posi', [96, GT], I32); angf = sba('angf', [96, GT], F32); rtmp = sba('rtmp', [96, GT], F32); rk = sba('rk', [96, GT], F32)
            gU = sba('gU', [4, GT], F32); gS = sba('gS', [4, GT], F32); gNB = sba('gNB', [4, GT], F32)
            gG = sba('gG', [4, GT], F32); gW = sba('gW', [4, GT], F32); gE = sba('gE', [4, GT], F32)
            gval = sba('gval', [4, GT], F32); gib = sba('gib', [4, GT], F32)
            Rall = sba('Rall', [4, GB + 1], F32); dec4 = sba('dec4', [4, GB], F32); Rm = sba('Rm', [4, 4, GB], F32)
            cNB = sba('cNB', [4, 1], F32); cG = sba('cG', [4, 1], F32)
            xmB_r = Ring(kb, sa, 'xmB', [128, 4, GT], BF16, 2)
            xcB_r = Ring(kb, sa, 'xcB', [128, 4, GT], BF16, 2)
            xcFo_r = Ring(kb, sa, 'xcFo', [128, 4, GO], F32, 2)
            zsF_r = Ring(kb, sa, 'zsF', [128, 4, GO], F32, 2)
            wcol_r = Ring(kb, sa, 'wcol', [128, GB, 8], F32, 2)
            dcol_r = Ring(kb, sa, 'dcol', [128, 4, GB], F32, 2)
            qT_r = Ring(kb, sa, 'qT', [128, 4, GO], BF16, 2)
            kT_r = Ring(kb, sa, 'kT', [128, 4, GO], BF16, 2)
            ktm_r = Ring(kb, sa, 'ktm', [128, 4, 128], BF16, 4)
            wv_r = Ring(kb, sa, 'wv', [128, 4, 132], BF16, 4)
            a0_r = Ring(kb, sa, 'a0', [128, 4, 128], BF16, 3)
            Cst = sba('Cst', [128, 4, 132], F32); Cd32 = sba('Cd32', [128, 4, 132], F32); Cd16 = sba('Cd16', [128, 4, 132], BF16)
            hN_r = Ring(kb, sa, 'hN', [128, 4, 128], F32, 2); hc_r = Ring(kb, sa, 'hc', [128, 4, 128], F32, 2)
            bst = sba('bst', [128, 4, 6], F32); bmv = sba('bmv', [128, 4, 2], F32); rstd = sba('rstd', [128, 4], F32)
            dcl = sba('dcl', [128, 4], F32); rden = sba('rden', [128, 4], F32)
            f1_r = Ring(kb, sa, 'f1', [128, 4, 128], F32, 2); f2_r = Ring(kb, sa, 'f2', [128, 4, 128], F32, 2)
            mlo_r = Ring(kb, sa, 'mlo', [128, 4, 128], BF16, 2)
            kb.memset('pool', xmF[:], 0.0, ['xmF'])
            kb.memset('pool', Cst[:], 0.0, ['Cst'])
            kb.memset('pool', cNB[:], 0.0, ['cNB'])
            kb.memset('pool', cG[:], 0.0, ['cG'])
            xv_b = xv.rearrange("(n p) d -> n p d", p=128)
            rows = slice(64, 96)
            v3 = lambda t: t[:].rearrange("p (b t) -> p b t", t=128)
            GS = {}

            def tables(g):
                tab, ktab = tab_r.next()
                T0 = g * GT
                kb.dma(posi[rows, :], posk[:, T0:T0 + GT], w=['posi'])
                kb.copy('dve', angf[rows, :], posi[rows, :], ['posi'], ['angf'])
                kb.ts('dve', angf[rows, :], angf[rows, :], invf[rows, 0:1], None, ALU.mult, None, ['angf', 'invf'], ['angf'])
                kb.ts('dve', rk[rows, :], angf[rows, :], 1.0 / (2 * np.pi), MAGIC, ALU.mult, ALU.add, ['angf'], ['rk'])
                kb.ts('dve', rk[rows, :], rk[rows, :], -MAGIC, None, ALU.add, None, ['rk'], ['rk'])
                kb.stt(rtmp[rows, :], rk[rows, :], -C1, angf[rows, :], ALU.mult, ALU.add, ['rk', 'angf'], ['rtmp'])
                kb.stt(rtmp[rows, :], rk[rows, :], -C2, rtmp[rows, :], ALU.mult, ALU.add, ['rk', 'rtmp'], ['rtmp'])
                kb.ts('dve', rtmp[rows, :], rtmp[rows, :], np.pi, -np.pi, ALU.min, ALU.max, ['rtmp'], ['rtmp'])
                kb.act(tab[rows, 1, :], rtmp[rows, :], AF.Sin, ['rtmp'], [ktab])
                kb.act(rtmp[rows, :], rtmp[rows, :], AF.Abs, ['rtmp'], ['rtmp'])
                kb.act(tab[rows, 0, :], rtmp[rows, :], AF.Sin, ['rtmp', 'halfpi'], [ktab], bias=halfpi[rows, 0:1], scale=-1.0)
                for ti in range(2):
                    kb.dma(tab_s[ti, :, g * GO:(g + 1) * GO].rearrange("p (b t) -> p b t", t=128),
                           tab[rows, ti, :].rearrange("p (b t) -> p b t", t=128)[:, 1::2, :], r=[ktab], w=[('tab_s', g)])
                return tab, ktab

            halfpi = sba('halfpi', [96, 1], F32)
            kb.memset('pool', halfpi[:], float(np.pi / 2), ['halfpi'])

            def P_part(g, part):
                T0 = g * GT
                O0 = g * GO
                if part == 0:
                    GS[g] = dict(tab=GS.pop(('tab', g)))
                    hT, khT = hT_r.next()
                    GS[g].update(hT=hT, khT=khT, xts=[])
                    for j in range(GB):
                        xt, kx = xt_r.next()
                        kb.dma(xt[:], xv_b[g * GB + j], w=[kx])
                        GS[g]['xts'].append((xt, kx))
                st = GS[g]
                hT, khT = st['hT'], st['khT']
                def a1_act(js):
                    for j in js:
                        xt, kx = st['xts'][j]
                        hb, kh = hb_r.next(); stt_, ks = st_r.next()
                        kb.act(hb[:], xt[:], AF.Square, [kx], [kh, ks], accum=stt_[:, 0:1])
                        kb.act(stt_[:, 1:2], stt_[:, 0:1], AF.Ln, [ks, 'epsc'], [ks], bias=epsc[:], scale=1.0 / 1024)
                        kb.act(stt_[:, 2:3], stt_[:, 1:2], AF.Exp, [ks], [ks], scale=-0.5)
                        kb.act(hb[:], xt[:], AF.Copy, [kx, ks], [kh], scale=stt_[:, 2:3])
                        st.setdefault('hbs', {})[j] = (hb, kh)

                def a1_pe(js):
                    for j in js:
                        hb, kh = st['hbs'].pop(j)
                        pst, kp = kb.psum()
                        pstb = pst[:].bitcast(BF16)
                        for c in range(8):
                            kb.tr(pstb[:, c * 128:(c + 1) * 128], hb[:, c * 128:(c + 1) * 128], identB[:], [kh, 'identB'], [kp])
                        kb.copy('dve', hT[:, :, j * 128:(j + 1) * 128], pstb.rearrange("p (c t) -> p c t", c=8), [kp], [khT])

                if part == 0:
                    a1_act((0, 1))
                    return
                if part == 1:
                    a1_pe((0, 1))
                    a1_act((2, 3))
                    return
                if part == 2:
                    a1_pe((2, 3))
                rhs_all = lambda c: hT[:, c, :]
                hT_own = lambda c: hT[:, c, :].rearrange("p (b t) -> p b t", t=128)[:, 1::2, :]
                if part == 2:
                    tab, ktab = st['tab']
                    xmB, kxmB = xmB_r.next(); zsF, kzs = zsF_r.next()
                    st.update(xmB=xmB, kxmB=kxmB, zsF=zsF, kzs=kzs)
                    for h in range(4):
                        ps, kp = kb.psum()
                        for c in range(8):
                            kb.mm(ps[:, :], win[:, c, C_XM + h * 128:C_XM + (h + 1) * 128], rhs_all(c), c == 0, c == 7, ['win', khT], [kp])
                        kb.copy('act', xmF[:, h, 4:4 + GT], ps[:, :], [kp], ['xmF'])
                        kb.copy('dve', xmB[:, h, :], ps[:, :], [kp], [kxmB])
                    psi_, kpi = kb.psum()
                    for c in range(8):
                        kb.mm(psi_[0:4, :], win[:, c, C_GI:C_GI + 4], rhs_all(c), c == 0, c == 7, ['win', khT], [kpi])
                    psf_, kpf = kb.psum()
                    for c in range(8):
                        kb.mm(psf_[0:4, :], win[:, c, C_GF:C_GF + 4], rhs_all(c), c == 0, c == 7, ['win', khT], [kpf])
                    kb.act(gU[:], psi_[0:4, :], AF.Identity, [kpi, 'bi'], ['gU'], bias=bi[:])
                    kb.act(gS[:], psf_[0:4, :], AF.Exp, [kpf, 'nbf'], ['gS'], bias=nbf[:], scale=-1.0)
                    kb.act(gS[:], gS[:], AF.Ln, ['gS'], ['gS'], bias=1.0)
                    ps, kp = kb.psum()
                    for c in range(8):
                        kb.mm(ps[:, :], win[:, c, C_CKV:C_CKV + 128], rhs_all(c), c == 0, c == 7, ['win', khT], [kp])
                    sq, ksq = sq_r.next(); rv, krv = rv_r.next(); lat, klat = lat_r.next()
                    kb.act(sq[:], ps[:, :], AF.Square, [kp], [ksq])
                    ps2, kp2 = kb.psum()
                    kb.mm(ps2[:, :], onesF[:], sq[:], True, True, ['onesF', ksq], [kp2])
                    kb.act(rv[:], ps2[:, :], AF.Ln, [kp2, 'epsc'], [krv], bias=epsc[:], scale=1.0 / 128)
                    kb.act(rv[:], rv[:], AF.Exp, [krv], [krv], scale=-0.5)
                    kb.stt(lat[:], ps[:, :], gkv[:, 0:1], rv[:], ALU.mult, ALU.mult, [kp, 'gkv', krv], [klat])
                    kb.dma(ckv_s[:, T0:T0 + GT], lat[:], r=[klat], w=[('ckv_s', g)])
                    ps, kp = kb.psum()
                    for c in range(8):
                        kb.mm(ps[0:96, :], win[:, c, C_MISC:C_MISC + 96], rhs_all(c), c == 0, c == 7, ['win', khT], [kp])
                    kb.copy('act', krF[rows, :], ps[rows, :], [kp], ['krF'])
                    ps2, kp2 = kb.psum()
                    kb.mm(ps2[0:96, :], permB[rows, :], krF[rows, :], True, True, ['permB', 'krF'], [kp2])
                    kb.tt('dve', rt1[rows, :], krF[rows, :], tab[rows, 0, :], ALU.mult, ['krF', ktab], ['rt1'])
                    kb.tt('dve', rt2[rows, :], ps2[rows, :], tab[rows, 1, :], ALU.mult, [kp2, ktab], ['rt2'])
                    kb.tt('dve', krB[rows, :], rt1[rows, :], rt2[rows, :], ALU.add, ['rt1', 'rt2'], ['krB'])
                    kb.dma(kr_s[:, T0:T0 + GT], krB[rows, :], r=['krB'], w=[('kr_s', g)])
                    psq = []
                    for q_ in range(2):
                        ps, kp = kb.psum()
                        for c in range(8):
                            kb.mm(ps[:, 0:GO], win[:, c, C_CQ + q_ * 128:C_CQ + (q_ + 1) * 128], hT_own(c), c == 0, c == 7, ['win', khT], [kp])
                        psq.append((ps, kp))
                    sq, ksq = sq_r.next(); rv, krv = rv_r.next(); lat, klat = lat_r.next()
                    for q_ in range(2):
                        kb.act(sq[:, q_ * GO:(q_ + 1) * GO], psq[q_][0][:, 0:GO], AF.Square, [psq[q_][1]], [ksq])
                    ps2, kp2 = kb.psum()
                    for q_ in range(2):
                        kb.mm(ps2[:, 0:GO], onesF[:], sq[:, q_ * GO:(q_ + 1) * GO], q_ == 0, q_ == 1, ['onesF', ksq], [kp2])
                    kb.act(rv[:, 0:GO], ps2[:, 0:GO], AF.Ln, [kp2, 'epsc'], [krv], bias=epsc[:], scale=1.0 / 256)
                    kb.act(rv[:, 0:GO], rv[:, 0:GO], AF.Exp, [krv], [krv], scale=-0.5)
                    for q_ in range(2):
                        kb.stt(lat[:, q_ * GO:(q_ + 1) * GO], psq[q_][0][:, 0:GO], gq[:, q_:q_ + 1], rv[:, 0:GO], ALU.mult, ALU.mult,
                               [psq[q_][1], 'gq', krv], [klat])
                    kb.dma(cq_s[:, :, O0:O0 + GO].rearrange("q p t -> p q t"), lat[:].rearrange("p (q t) -> p q t", q=2), r=[klat], w=[('cq_s', g)])
                    for h in range(4):
                        ps, kp = kb.psum()
                        for c in range(8):
                            kb.mm(ps[:, 0:GO], win[:, c, C_Z + h * 128:C_Z + (h + 1) * 128], hT_own(c), c == 0, c == 7, ['win', khT], [kp])
                        kb.act(zsF[:, h, :], ps[:, 0:GO], AF.Silu, [kp], [kzs])
                    return
                xcB, kxcB = xcB_r.next(); xcFo, kxcFo = xcFo_r.next()
                wcol, kwcol = wcol_r.next(); dcol, kdcol = dcol_r.next()
                qT, kqT = qT_r.next(); kT, kkT = kT_r.next()
                st.update(xcB=xcB, kxcB=kxcB, xcFo=xcFo, kxcFo=kxcFo, wcol=wcol, kwcol=kwcol, dcol=dcol, kdcol=kdcol, qT=qT, kqT=kqT, kT=kT, kkT=kkT)
                for h in range(4):
                    ps, kp = kb.psum()
                    for k in range(4):
                        kb.mm(ps[:, :], cdiag[:, h * 4 + k, :], xmF[:, h, 1 + k:1 + k + GT], k == 0, k == 3, ['cdiag', 'xmF'], [kp])
                    kb.act(xcB[:, h, :], ps[:, :], AF.Silu, [kp, 'convb'], [kxcB], bias=convb[:, h:h + 1])
                    kb.act(xcFo[:, h, :].rearrange("p (b t) -> p b t", t=128), ps[:, :].rearrange("p (b t) -> p b t", t=128)[:, 1::2, :],
                           AF.Silu, [kp, 'convb'], [kxcFo], bias=convb[:, h:h + 1])
                kb.copy('pool', xmF[:, :, 1:4], xmF[:, :, 1 + GT:4 + GT], ['xmF'], ['xmF'])
                kb.dma(gval[:], valid4[:, T0:T0 + GT], w=['gval'])
                kb.dma(gib[:], ibias4[:, T0:T0 + GT], w=['gib'])
                kb.tt('dve', gS[:], gS[:], gval[:], ALU.mult, ['gS', 'gval'], ['gS'])
                kb.scan(gNB[:], onesr[:], gS[:], cNB[:, 0:1], ALU.mult, ALU.add, ['onesr', 'gS', 'cNB'], ['gNB'])
                kb.tt('dve', gU[:], gU[:], gNB[:], ALU.add, ['gU', 'gNB'], ['gU'])
                kb.tt('dve', gU[:], gU[:], gib[:], ALU.add, ['gU', 'gib'], ['gU'])
                kb.scan(gG[:], onesr[:], gU[:], cG[:, 0:1], ALU.mult, ALU.max, ['onesr', 'gU', 'cG'], ['gG'])
                kb.copy('dve', Rall[:, 0:1], cG[:], ['cG'], ['Rall'])
                kb.copy('dve', Rall[:, 1:GB + 1], gG[:].rearrange("p (b t) -> p b t", t=128)[:, :, 127], ['gG'], ['Rall'])
                kb.copy('dve', cNB[:], gNB[:, GT - 1:GT], ['gNB'], ['cNB'])
                kb.copy('dve', cG[:], gG[:, GT - 1:GT], ['gG'], ['cG'])
                Rb = bc(Rall[:, 1:GB + 1].unsqueeze(2), [4, GB, 128])
                kb.tt('dve', v3(gW), v3(gU), Rb, ALU.subtract, ['gU', 'Rall'], ['gW'])
                kb.tt('dve', v3(gE), v3(gNB), Rb, ALU.subtract, ['gNB', 'Rall'], ['gE'])
                kb.tt('dve', dec4[:], Rall[:, 0:GB], Rall[:, 1:GB + 1], ALU.subtract, ['Rall'], ['dec4'])
                kb.act(dec4[:], dec4[:], AF.Exp, ['dec4'], ['dec4'])
                kb.act(gW[:], gW[:], AF.Exp, ['gW'], ['gW'])
                kb.act(gE[:], gE[:], AF.Exp, ['gE'], ['gE'])
                kb.tt('dve', Rm[:], bc(dec4[:].unsqueeze(1), [4, 4, GB]), dmask[:].rearrange("p (h b) -> p h b", b=GB), ALU.mult,
                      ['dec4', 'dmask'], ['Rm'])
                psg, kpg = kb.psum()
                for b_ in range(GB):
                    kb.tr(psg[:, b_ * 8:b_ * 8 + 4], gW[:, b_ * 128:(b_ + 1) * 128], identF[0:4, 0:4], ['gW', 'identF'], [kpg])
                    kb.tr(psg[:, b_ * 8 + 4:b_ * 8 + 8], gE[:, b_ * 128:(b_ + 1) * 128], identF[0:4, 0:4], ['gE', 'identF'], [kpg])
                kb.copy('dve', wcol[:].rearrange("p b e -> p (b e)"), psg[:, 0:GB * 8], [kpg], [kwcol])
                psd, kpd = kb.psum()
                kb.mm(psd[:, 0:4 * GB], ones4[:], Rm[:].rearrange("p h b -> p (h b)"), True, True, ['ones4', 'Rm'], [kpd])
                kb.copy('dve', dcol[:].rearrange("p h b -> p (h b)"), psd[:, 0:4 * GB], [kpd], [kdcol])
                xc_own = lambda h: xcB[:, h, :].rearrange("p (b t) -> p b t", t=128)[:, 1::2, :]
                for hp in range(2):
                    psq_, kpq = kb.psum()
                    psk_, kpk = kb.psum()
                    for hh in range(2):
                        h = hp * 2 + hh
                        kb.mm(psq_[:, hh * GO:(hh + 1) * GO], wmq[:, h, :], xc_own(h), True, True, ['wmq', kxcB], [kpq])
                        kb.mm(psk_[:, hh * GO:(hh + 1) * GO], wmk[:, h, :], xc_own(h), True, True, ['wmk', kxcB], [kpk])
                    kb.copy('act', qT[:, hp * 2:hp * 2 + 2, :].rearrange("p h t -> p (h t)"), psq_[:, 0:2 * GO], [kpq], [kqT])
                    kb.act(kT[:, hp * 2:hp * 2 + 2, :].rearrange("p h t -> p (h t)"), psk_[:, 0:2 * GO], AF.Copy, [kpk], [kkT], scale=float(128 ** -0.5))
                if g + 1 < NG:
                    GS[('tab', g + 1)] = tables(g + 1)

            def M_pre(g, j):
                st = GS[g]
                xmB, kxmB = st['xmB'], st['kxmB']
                xcB, kxcB = st['xcB'], st['kxcB']
                wcol, kwcol = st['wcol'], st['kwcol']
                qT, kqT, kT, kkT = st['qT'], st['kqT'], st['kT'], st['kkT']
                owned = (j % 2 == 1)
                ob = j // 2
                bsl = slice(j * 128, (j + 1) * 128)
                ktm, kkt = ktm_r.next(); wv, kwv = wv_r.next()
                psk_, kpk = kb.psum()
                psv_, kpv = kb.psum()
                for h in range(4):
                    kb.mm(psk_[:, h * 128:(h + 1) * 128], xcB[:, h, bsl], wmk[:, h, :], True, True, [kxcB, 'wmk'], [kpk])
                for h in range(4):
                    kb.mm(psv_[:, h * 128:(h + 1) * 128], xmB[:, h, bsl], wmv[:, h, :], True, True, [kxmB, 'wmv'], [kpv])
                kb.act(ktm[:].rearrange("p h d -> p (h d)"), psk_[:, :], AF.Copy, [kpk], [kkt], scale=float(128 ** -0.5))
                wsl = wcol[:, j, 0:4]
                kb.tt('dve', wv[:, :, 0:128], psv_[:, :].rearrange("p (h d) -> p h d", d=128), bc(wsl.unsqueeze(2), [128, 4, 128]), ALU.mult,
                      [kpv, kwcol], [kwv])
                kb.copy('pool', wv[:, :, 128:129], wsl.unsqueeze(2), [kwcol], [kwv])
                a0, ka0 = None, None
                if owned:
                    osl = slice(ob * 128, (ob + 1) * 128)
                    pss, kps = kb.psum()
                    for h in range(4):
                        kb.mm(pss[:, h * 128:(h + 1) * 128], kT[:, h, osl], qT[:, h, osl], True, True, [kkT, kqT], [kps])
                    a0, ka0 = a0_r.next()
                    kb.tt('dve', a0[:], pss[:, :].rearrange("p (h t) -> p h t", t=128), bc(trilT[:].unsqueeze(1), [128, 4, 128]), ALU.mult,
                          [kps, 'trilT'], [ka0])
                st.setdefault('pre', {})[j] = (ktm, kkt, wv, kwv, a0, ka0)

            def M_block(g, j):
                st = GS[g]
                xmB, kxmB, zsF, kzs = st['xmB'], st['kxmB'], st['zsF'], st['kzs']
                xcB, kxcB, xcFo, kxcFo = st['xcB'], st['kxcB'], st['xcFo'], st['kxcFo']
                wcol, kwcol, dcol, kdcol = st['wcol'], st['kwcol'], st['dcol'], st['kdcol']
                qT, kqT, kT, kkT = st['qT'], st['kqT'], st['kT'], st['kkT']
                owned = (j % 2 == 1)
                ob = j // 2
                bsl = slice(j * 128, (j + 1) * 128)
                ktm, kkt, wv, kwv, a0, ka0 = st['pre'].pop(j)
                dsl = dcol[:, :, j]
                kb.tt('dve', Cd32[:, :, 0:129], Cst[:, :, 0:129], bc(dsl.unsqueeze(2), [128, 4, 129]), ALU.mult, ['Cst', kdcol], ['Cd32'])
                if owned:
                    kb.copy('act', Cd16[:, :, 0:129], Cd32[:, :, 0:129], ['Cd32'], ['Cd16'])
                    osl = slice(ob * 128, (ob + 1) * 128)
                    pn = []
                    for hp in range(2):
                        psn, kpn = kb.psum()
                        for hh in range(2):
                            h = hp * 2 + hh
                            kb.mm(psn[:, hh * 256:hh * 256 + 129], a0[:, h, :], wv[:, h, 0:129], True, False, [ka0, kwv], [kpn])
                            kb.mm(psn[:, hh * 256:hh * 256 + 129], qT[:, h, osl], Cd16[:, h, 0:129], False, True, [kqT, 'Cd16'], [kpn])
                        pn.append((psn, kpn))
                for hp in range(2):
                    psu, kpu = kb.psum()
                    for hh in range(2):
                        h = hp * 2 + hh
                        kb.mm(psu[:, hh * 256:hh * 256 + 129], ktm[:, h, :], wv[:, h, 0:129], True, True, [kkt, kwv], [kpu])
                    kb.tt('dve', Cst[:, hp * 2:hp * 2 + 2, 0:129], Cd32[:, hp * 2:hp * 2 + 2, 0:129],
                          psu[:, :].rearrange("p (h c) -> p h c", c=256)[:, :, 0:129], ALU.add, ['Cd32', kpu], ['Cst'])
                if owned:
                    hN, khN = hN_r.next(); hc, khc = hc_r.next(); f1, kf1 = f1_r.next(); f2, kf2 = f2_r.next(); mlo, kmlo = mlo_r.next()
                    ecl = wcol[:, j, 4:8]
                    for hp in range(2):
                        psn, kpn = pn[hp]
                        pv3 = psn[:, :].rearrange("p (h c) -> p h c", c=256)
                        kb.stt(rden[:, hp * 2:hp * 2 + 2], pv3[:, :, 128], -1.0, ecl[:, hp * 2:hp * 2 + 2], ALU.mult, ALU.max, [kpn, kwcol], ['rden'])
                        kb.tt('dve', dcl[:, hp * 2:hp * 2 + 2], pv3[:, :, 128], rden[:, hp * 2:hp * 2 + 2], ALU.max, [kpn, 'rden'], ['dcl'])
                    kb.P.add('dve', lambda e: e.reciprocal(out=rden[:], in_=dcl[:]), ['dcl'], ['rden'])
                    for hp in range(2):
                        psn, kpn = pn[hp]
                        pv3 = psn[:, :].rearrange("p (h c) -> p h c", c=256)
                        kb.tt('dve', hN[:, hp * 2:hp * 2 + 2, :], pv3[:, :, 0:128], bc(rden[:, hp * 2:hp * 2 + 2].unsqueeze(2), [128, 2, 128]), ALU.mult,
                              [kpn, 'rden'], [khN])
                    for h in range(4):
                        kb.P.add('dve', (lambda h: lambda e: e.bn_stats(out=bst[:, h, :], in_=hN[:, h, :]))(h), [khN], ['bst'])
                    for h in range(4):
                        kb.P.add('dve', (lambda h: lambda e: e.bn_aggr(out=bmv[:, h, :], in_=bst[:, h, :]))(h), ['bst'], ['bmv'])
                    kb.ts('dve', rstd[:], bmv[:, :, 1], EPS, None, ALU.add, None, ['bmv'], ['rstd'])
                    kb.tt('pool', rstd[:], rstd[:], bc(mhalf[:], [128, 4]), ALU.pow, ['rstd', 'mhalf'], ['rstd'])
                    for h in range(4):
                        kb.ts('dve', hc[:, h, :], hN[:, h, :], bmv[:, h, 0:1], rstd[:, h:h + 1], ALU.subtract, ALU.mult, [khN, 'bmv', 'rstd'], [khc])
                    pst, kpt = kb.psum()
                    for h in range(4):
                        kb.tr(pst[:, h * 128:(h + 1) * 128], hc[:, h, :], identF[:], [khc, 'identF'], [kpt])
                    kb.tt('dve', f1[:], pst[:, :].rearrange("p (h t) -> p h t", t=128), bc(gmh[:].unsqueeze(2), [128, 4, 128]), ALU.mult,
                          [kpt, 'gmh'], [kf1])
                    kb.tt('pool', f2[:], xcFo[:, :, osl], bc(skipc[:].unsqueeze(2), [128, 4, 128]), ALU.mult, [kxcFo, 'skipc'], [kf2])
                    kb.tt('pool', f1[:], f1[:], f2[:], ALU.add, [kf1, kf2], [kf1])
                    kb.tt('pool', mlo[:], f1[:], zsF[:, :, osl], ALU.mult, [kf1, kzs], [kmlo])
                    gob = g * (GB // 2) + ob
                    kb.dma(mlo_s[:, :, gob * 128:(gob + 1) * 128].rearrange("h p t -> p h t"), mlo[:], r=[kmlo], w=[('mlo_s', gob)])
                    if dbg:
                        kb.dma(dbg_mlo[:, :, gob * 128:(gob + 1) * 128].rearrange("h p t -> p h t"), mlo[:], r=[kmlo], w=[('d_mlo', gob)])

            steps = [lambda: GS.__setitem__(('tab', 0), tables(0))]
            for part in range(4):
                steps.append((lambda part: lambda: P_part(0, part))(part))
            pq = [(g_, p_) for g_ in range(1, NG) for p_ in range(4)]
            for _ in range(2):
                if pq:
                    steps.append((lambda gp: lambda: P_part(*gp))(pq.pop(0)))
            for g in range(NG):
                steps.append((lambda g: lambda: M_pre(g, 0))(g))
                steps.append((lambda g: lambda: M_pre(g, 1))(g))
                steps.append((lambda g: lambda: M_pre(g, 2))(g))
                for j in range(GB):
                    steps.append((lambda g, j: lambda: M_block(g, j))(g, j))
                    if j + 3 < GB:
                        steps.append((lambda g, j: lambda: M_pre(g, j + 3))(g, j))
                    if pq:
                        steps.append((lambda gp: lambda: P_part(*gp))(pq.pop(0)))
            for i_, f_ in enumerate(steps):
                f_()
                if i_ % 3 == 2:
                    zero_fill(1)
            zero_fill(len(zf_todo))
        P.barrier()
        if stop_after == 'A':
            fin_r = [('mlo_s', gob) for gob in range(NOB)] + [('d_mlo', gob) for gob in range(NOB)] + [('ckv_s', g) for g in range(NG)] + \
                [('cq_s', g) for g in range(NG)] + [('kr_s', g) for g in range(NG)] + [('tab_s', g) for g in range(NG)]
            P.add('sp', None, reads=[k for k in fin_r if k in P.last_w])
            P.emit(top)
            return nc, D, dbgo

        attnTok = sb('attnTok', [128, NOB, 512], BF16)
        NQG = NOB // 4
        with ExitStack() as sbk:
            sbb = lambda name, shape, dt: kb.sb(sbk, name, shape, dt)
            stt2 = [sbk.enter_context(nc.psum_tensor('psS%d' % k, [128, 1024], F32)) for k in range(3)]
            pvt = [sbk.enter_context(nc.psum_tensor('psV%d' % k, [128, 512], F32)) for k in range(2)]
            wuq = sbb('wuq', [128, 2, 768], BF16); wukv = sbb('wukv', [128, 1024], BF16)
            kb.dma(wuq[:], w_uq_d.rearrange("(c p) n -> p c n", p=128), w=['wuq'], q='pool')
            kb.dma(wukv[:], w_ukv_d, w=['wukv'], q='pool')
            cosq = sbb('cosq', [96, OT], F32); sinq = sbb('sinq', [96, OT], F32)
            cqnT = sbb('cqnT', [128, 2, OT], BF16); ckvnT = sbb('ckvnT', [128, VT], BF16)
            KhT = sbb('KhT', [97, VT], BF16)
            allA = [('ckv_s', g_) for g_ in range(NG)] + [('cq_s', g_) for g_ in range(NG)] + [('kr_s', g_) for g_ in range(NG)] + [('tab_s', g_) for g_ in range(NG)]
            kb.dma(ckvnT[:], ckv_s, r=allA, w=['ckvnT'])
            kb.dma(cqnT[:], cq_s.rearrange("q p t -> p q t"), r=allA, w=['cqnT'])
            kb.dma(KhT[64:96, :], kr_s, r=allA, w=['KhT_r'])
            kb.dma(KhT[96:97, :], kbias, w=['KhT_b'], q='pool')
            kb.dma(cosq[64:96, :], tab_s[0], r=allA, w=['tabQ'])
            kb.dma(sinq[64:96, :], tab_s[1], r=allA, w=['tabQ'])
            kb.ts('dve', cosq[64:96, :], cosq[64:96, :], float(QSCALE), None, ALU.mult, None, ['tabQ'], ['tabQ'])
            kb.ts('dve', sinq[64:96, :], sinq[64:96, :], float(QSCALE), None, ALU.mult, None, ['tabQ'], ['tabQ'])
            Vh = sbb('Vh', [128, NVB, 65], BF16)
            QhT = sbb('QhT', [97, OT], BF16)
            kb.memset('pool', Vh[:, :, 64:65], 1.0, ['Vh1'])
            kb.memset('pool', QhT[96:97, :], 1.0, ['QhT1'])
            qraw = sbb('qraw', [96, 512], F32); qt1 = sbb('qt1', [96, 512], F32); qt2 = sbb('qt2', [96, 512], F32)
            pt_r = Ring(kb, sbk, 'pt', [128, 2, 512], BF16, 4)
            rs = sbb('rs', [128, 4], F32)
            KR = ['KhT_r', 'KhT_b']
            CKV = ['ckvnT']
            CQN = ['cqnT']
            sti = [0]

            def st_next():
                k = sti[0] % 3
                sti[0] += 1
                return stt2[k], ('psS', k)

            for h in range(8):
                for ch in range(VT // 512):
                    st_, kst = st_next()
                    kb.mm(st_[0:64, 0:512], wukv[:, h * 128:h * 128 + 64], ckvnT[:, ch * 512:(ch + 1) * 512], True, True, ['wukv'] + CKV, [kst])
                    kb.copy('act' if ch % 2 == 0 else 'dve', KhT[0:64, ch * 512:(ch + 1) * 512], st_[0:64, 0:512], [kst], ['KhT_n'])
                for v8 in range(NVB // 8):
                    st_, kst = st_next()
                    for bb in range(8):
                        blk = v8 * 8 + bb
                        kb.mm(st_[:, bb * 64:(bb + 1) * 64], ckvnT[:, blk * 128:(blk + 1) * 128], wukv[:, h * 128 + 64:h * 128 + 128], True, True,
                              ['wukv'] + CKV, [kst])
                    kb.copy('dve' if v8 % 2 == 0 else 'act', Vh[:, v8 * 8:(v8 + 1) * 8, 0:64], st_[:, 0:512].rearrange("p (b d) -> p b d", d=64), [kst], ['Vh'])
                for qc in range(OT // 512):
                    cs = slice(qc * 512, (qc + 1) * 512)
                    st_, kst = st_next()
                    for c in range(2):
                        kb.mm(st_[0:96, 0:512], wuq[:, c, h * 96:(h + 1) * 96], cqnT[:, c, cs], c == 0, c == 1, ['wuq'] + CQN, [kst])
                    kb.act(QhT[0:64, cs], st_[0:64, 0:512], AF.Copy, [kst], ['QhT'], scale=float(QSCALE))
                    kb.copy('act', qraw[64:96, :], st_[64:96, 0:512], [kst], ['qraw'])
                    st2_, kst2 = st_next()
                    kb.mm(st2_[0:96, 0:512], permB[64:96, :], qraw[64:96, :], True, True, ['permB', 'qraw'], [kst2])
                    kb.tt('pool', qt1[64:96, :], qraw[64:96, :], cosq[64:96, cs], ALU.mult, ['qraw', 'tabQ'], ['qt1'])
                    kb.tt('dve', qt2[64:96, :], st2_[64:96, 0:512], sinq[64:96, cs], ALU.mult, [kst2, 'tabQ'], ['qt2'])
                    kb.tt('pool', QhT[64:96, cs], qt1[64:96, :], qt2[64:96, :], ALU.add, ['qt1', 'qt2'], ['QhT'])
                KALL = ['KhT_n'] + KR
                batches = []
                for qg in range(NQG):
                    kbs = list(range(8 * qg + 8))
                    nfull = 8 * qg + 2
                    i = 0
                    while i < nfull:
                        n = min(2, nfull - i)
                        batches.append(dict(qg=qg, kbs=kbs[i:i + n], full=True, first=(i == 0), last=False))
                        i += n
                    for kb_ in range(nfull, 8 * qg + 8):
                        batches.append(dict(qg=qg, kbs=[kb_], full=False, first=False, last=(kb_ == 8 * qg + 7)))

                def lmin_of(qg, kb_):
                    return max(0, (kb_ - 8 * qg - 1 + 1) // 2)

                def emit_S(bt):
                    st_, kst = st_next()
                    bt['st'] = (st_, kst)
                    qg = bt['qg']
                    for i, kb_ in enumerate(bt['kbs']):
                        lm = lmin_of(qg, kb_)
                        kb.mm(st_[:, i * 512 + lm * 128:(i + 1) * 512], KhT[0:97, kb_ * 128:(kb_ + 1) * 128],
                              QhT[0:97, qg * 512 + lm * 128:(qg + 1) * 512], True, True, KALL + ['QhT', 'QhT1'], [kst])

                def emit_PV(bt):
                    st_, kst = bt['st']
                    qg = bt['qg']
                    pt, kpt = pt_r.next()
                    pvb, kpv = pvt[qg % 2], ('psV', qg % 2)
                    if bt['first']:
                        kb.memset('dve', pvb[:, :], 0.0, [kpv])
                    if bt['full']:
                        n = len(bt['kbs'])
                        kb.act(pt[:, 0:n, :].rearrange("p a b -> p (a b)"), st_[:, 0:n * 512], AF.Exp, [kst], [kpt])
                    else:
                        kb_ = bt['kbs'][0]
                        lm = lmin_of(qg, kb_)
                        kb.act(pt[:, 0, lm * 128:512], st_[:, lm * 128:512], AF.Exp, [kst], [kpt])
                    for i, kb_ in enumerate(bt['kbs']):
                        lm = lmin_of(qg, kb_)
                        if kb_ == 8 * qg + 2 * lm + 1:
                            kb.tt('pool', pt[:, i, lm * 128:(lm + 1) * 128], pt[:, i, lm * 128:(lm + 1) * 128], attm[:], ALU.mult, [kpt, 'attm'], [kpt])
                    for i, kb_ in enumerate(bt['kbs']):
                        lm = lmin_of(qg, kb_)
                        for l in range(lm, 4):
                            kb.mm(pvb[:, l * 128:l * 128 + 65], pt[:, i, l * 128:(l + 1) * 128], Vh[:, kb_, 0:65], False, False,
                                  [kpt, 'Vh', 'Vh1'], [kpv], skip=True)
                    if bt['last']:
                        pv3 = pvb[:, :].rearrange("p (l c) -> p l c", c=128)
                        kb.P.add('dve', lambda e: e.reciprocal(out=rs[:], in_=pv3[:, :, 64]), [kpv], ['rs'])
                        kb.tt('dve', attnTok[:, 4 * qg:4 * qg + 4, h * 64:(h + 1) * 64], pv3[:, :, 0:64], bc(rs[:].unsqueeze(2), [128, 4, 64]), ALU.mult,
                              [kpv, 'rs'], [('attnTok', qg)])

                for i, bt in enumerate(batches):
                    if i == 0:
                        emit_S(bt)
                        if len(batches) > 1:
                            emit_S(batches[1])
                    if i + 2 < len(batches):
                        emit_S(batches[i + 2])
                    emit_PV(bt)
            if dbg:
                o = dbg_out('d_attn', [128, NOB * 512], BF16)
                kb.dma(o, attnTok[:].rearrange("p a b -> p (a b)"), r=[('attnTok', qg) for qg in range(NQG)], w=['d_attn'])
        P.barrier()
        if stop_after == 'B':
            P.add('sp', None, reads=list(dbgo.keys()) if False else [k for k in dbgo.keys() if k != 'd_mlo'] + [('d_mlo', gob) for gob in range(NOB)])
            P.emit(top)
            return nc, D, dbgo

        iota32_d = D['iota32']
        route = sb('route', [128, NOB, 2], F32)
        desti = sb('desti', [128, NOB, 2], I32)
        xv_b = xv.rearrange("(n p) d -> n p d", p=128)

        def rms_rinv(stk, xin, kxin, junk, kjunk, pool_pow=False):
            kb.act(junk[:], xin[:], AF.Square, [kxin], [kjunk, stk[1]], accum=stk[0][:, 0:1])
            if pool_pow:
                kb.ts('dve', stk[0][:, 1:2], stk[0][:, 0:1], 1.0 / 1024, EPS, ALU.mult, ALU.add, [stk[1]], [stk[1]])
                kb.tt('pool', stk[0][:, 2:3], stk[0][:, 1:2], mhalf[:], ALU.pow, [stk[1], 'mhalf'], [stk[1]])
                return stk[0][:, 2:3]
            kb.act(stk[0][:, 1:2], stk[0][:, 0:1], AF.Ln, [stk[1], 'epsc'], [stk[1]], bias=epsc[:], scale=1.0 / 1024)
            kb.act(stk[0][:, 2:3], stk[0][:, 1:2], AF.Exp, [stk[1]], [stk[1]], scale=-0.5)
            return stk[0][:, 2:3]

        with ExitStack() as sc_:
            sbc = lambda name, shape, dt: kb.sb(sc_, name, shape, dt)
            kb.ps = [sc_.enter_context(nc.psum_tensor('psC%d' % k, [128, 512], F32)) for k in range(8)]
            wo = sbc('wo', [128, 8, 1024], BF16)
            kb.dma(wo[:], w_o_d.rearrange("(c p) n -> p c n", p=128), w=['wo'], q='pool')
            gffn = sbc('gffn', [128, 1024], F32); wr = sbc('wr', [128, 8, 36], F32); brt = sbc('brt', [128, 36], F32)
            ustrF = sbc('ustrF', [128, 128], F32); ustrB = sbc('ustrB', [128, 128], BF16); onesB = sbc('onesB', [128, 128], BF16)
            iota32 = sbc('iota32', [128, 32], F32)
            tot = sbc('tot', [128, 32], F32)
            kb.dma(gffn[:], gffn_d, w=['gffn']); kb.dma(wr[:], w_rt_d.rearrange("(c p) n -> p c n", p=128), w=['wr'])
            kb.dma(brt[:], b_rt_d, w=['brt']); kb.dma(ustrF[:], ustr_d, w=['ustrF']); kb.dma(iota32[:], iota32_d, w=['iota32'])
            kb.copy('dve', ustrB[:], ustrF[:], ['ustrF'], ['ustrB'])
            kb.memset('pool', onesB[:], 1.0, ['onesB'])
            kb.memset('pool', tot[:], 0.0, ['tot'])
            mix_r = Ring(kb, sc_, 'mixT', [128, 8, 128], BF16, 4)
            xt_r = Ring(kb, sc_, 'xtc', [128, 1024], F32, 4)
            x1_r = Ring(kb, sc_, 'x1t', [128, 1024], F32, 3)
            hn_r = Ring(kb, sc_, 'hnF', [128, 1024], F32, 2)
            hb_r = Ring(kb, sc_, 'hnb', [128, 1024], BF16, 10)
            jk_r = Ring(kb, sc_, 'jkc', [128, 1024], BF16, 2)
            hnT_r = Ring(kb, sc_, 'hnT', [128, 8, 128], F32, 2)
            st_r = Ring(kb, sc_, 'stc', [128, 4], F32, 4)
            L4 = sbc('L4', [128, 4, 36], F32); sm4 = sbc('sm4', [128, 4, 16], F32)
            oh4 = sbc('oh4', [128, 4, 4], F32); oh1 = sbc('oh1', [128, 4, 8], F32); oh2 = sbc('oh2', [128, 4, 8], F32)
            t48 = sbc('t48', [128, 4, 4, 8], F32); ein = sbc('ein', [128, 4, 8], F32); ein2 = sbc('ein2', [128, 4, 8], F32)
            OH1 = sbc('OH1', [128, 4, 4, 8], F32); OH2 = sbc('OH2', [128, 4, 4, 8], F32); cntB = sbc('cntB', [128, 4, 32], BF16)
            pre = sbc('pre', [128, 4, 32], F32); t32 = sbc('t32', [128, 4, 32], F32); e4 = sbc('e4', [128, 4, 4], F32)
            fl32 = lambda t: t[:].rearrange("p g j -> p (g j)")
            cst = {}

            def c_load(ob):
                vb = 2 * ob + 1
                mixT, kmix = mix_r.next(); xt, kx = xt_r.next()
                cst[ob] = dict(mixT=mixT, kmix=kmix, xt=xt, kx=kx)
                kb.dma(mixT[:, 4:8, :], mlo_s[:, :, ob * 128:(ob + 1) * 128].rearrange("h p t -> p h t"), r=[('mlo_s', ob)], w=[(kmix, 'ml')])
                kb.dma(xt[:], xv_b[vb], w=[kx])

            def c_part1(ob):
                d_ = cst[ob]
                mixT, kmix, xt, kx = d_['mixT'], d_['kmix'], d_['xt'], d_['kx']
                x1t, kx1 = x1_r.next()
                pst, kp = kb.psum()
                pstb = pst[:].bitcast(BF16)
                for c in range(4):
                    kb.tr(pstb[:, c * 128:(c + 1) * 128], attnTok[:, ob, c * 128:(c + 1) * 128], identB[:], [('attnTok', ob // 4), 'identB'], [kp])
                kb.copy('act', mixT[:, 0:4, :].rearrange("p c t -> p (c t)"), pstb[:, 0:512], [kp], [(kmix, 'at')])
                for half in range(2):
                    ps, kp = kb.psum()
                    for c in range(8):
                        kb.mm(ps[:, :], mixT[:, c, :], wo[:, c, half * 512:(half + 1) * 512], c == 0, c == 7, [(kmix, 'at'), (kmix, 'ml'), 'wo'], [kp])
                    kb.tt('dve', x1t[:, half * 512:(half + 1) * 512], ps[:, :], xt[:, half * 512:(half + 1) * 512], ALU.add, [kp, kx], [kx1])
                kb.dma(x1_s[ob * 128:(ob + 1) * 128, :], x1t[:], r=[kx1], w=[('x1_s', ob)])
                jk, kjk = jk_r.next(); stk = st_r.next()
                rinv = rms_rinv(stk, x1t, kx1, jk, kjk)
                d_.update(x1t=x1t, kx1=kx1, stk=stk, rinv=rinv)

            def c_part2(ob):
                d_ = cst[ob]
                x1t, kx1, stk, rinv = d_['x1t'], d_['kx1'], d_['stk'], d_['rinv']
                hnF, khn = hn_r.next(); hnb, khb = hb_r.next()
                d_.update(hnb=hnb, khb=khb)
                kb.stt(hnF[:], x1t[:], rinv, gffn[:], ALU.mult, ALU.mult, [kx1, stk[1], 'gffn'], [khn])
                kb.copy('act', hnb[:], hnF[:], [khn], [khb])
                hnT, khT_ = hnT_r.next()
                for q4 in range(2):
                    pst, kp = kb.psum()
                    for c in range(4):
                        kb.tr(pst[:, c * 128:(c + 1) * 128], hnF[:, (q4 * 4 + c) * 128:(q4 * 4 + c + 1) * 128], identF[:], [khn, 'identF'], [kp])
                    kb.copy('act' if q4 == 0 else 'dve', hnT[:, q4 * 4:q4 * 4 + 4, :].rearrange("p c t -> p (c t)"), pst[:, :], [kp], [khT_])
                psl, kpl = kb.psum()
                for c in range(8):
                    kb.mm(psl[:, 0:36], hnT[:, c, :], wr[:, c, :], c == 0, c == 7, [khT_, 'wr'], [kpl])
                kb.tt('dve', L4[:, ob % 4, :], psl[:, 0:36], brt[:], ALU.add, [kpl, 'brt'], [('L4', ob % 4)])

            def c_route(qd):
                LK = [('L4', i) for i in range(4)]
                A = lambda i: sm4[:, :, i]
                Ab = lambda i, n: bc(sm4[:, :, i:i + 1], [128, 4, n])
                red = lambda out, in_, op, r, w: kb.P.add('dve', lambda e: e.tensor_reduce(out=out, in_=in_, axis=AX.X, op=op), r, w)
                red(A(0), L4[:, :, 0:4], ALU.max, LK, ['s0'])
                kb.tt('dve', oh4[:], L4[:, :, 0:4], Ab(0, 4), ALU.is_equal, LK + ['s0'], ['oh4'])
                kb.tt('dve', e4[:], L4[:, :, 0:4], Ab(0, 4), ALU.subtract, LK + ['s0'], ['e4'])
                kb.act(e4[:], e4[:], AF.Exp, ['e4'], ['e4'])
                red(A(2), e4[:], ALU.add, ['e4'], ['s2'])
                kb.P.add('dve', lambda e: e.reciprocal(out=A(3), in_=A(2)), ['s2'], ['s3'])
                L48 = L4[:, :, 4:36].rearrange("p b (g j) -> p b g j", j=8)
                kb.tt('dve', t48[:], L48, bc(oh4[:].unsqueeze(3), [128, 4, 4, 8]), ALU.mult, LK + ['oh4'], ['t48'])
                red(ein[:], t48[:].rearrange("p b g j -> p b j g"), ALU.add, ['t48'], ['ein'])
                red(A(4), ein[:], ALU.max, ['ein'], ['s4'])
                kb.tt('dve', oh1[:], ein[:], Ab(4, 8), ALU.is_equal, ['ein', 's4'], ['oh1'])
                kb.stt(ein2[:], oh1[:], -1e30, ein[:], ALU.mult, ALU.add, ['oh1', 'ein'], ['ein2'])
                red(A(5), ein2[:], ALU.max, ['ein2'], ['s5'])
                kb.tt('dve', oh2[:], ein2[:], Ab(5, 8), ALU.is_equal, ['ein2', 's5'], ['oh2'])
                kb.tt('dve', A(6), A(5), A(4), ALU.subtract, ['s5', 's4'], ['s6'])
                kb.act(A(6), A(6), AF.Exp, ['s6'], ['s6'])
                kb.ts('dve', A(7), A(6), 1.0, None, ALU.add, None, ['s6'], ['s7'])
                kb.P.add('dve', lambda e: e.reciprocal(out=A(7), in_=A(7)), ['s7'], ['s7'])
                kb.tt('dve', A(8), A(7), A(6), ALU.mult, ['s7', 's6'], ['s8'])
                rk_ = [('route', qd * 4 + i) for i in range(4)]
                kb.tt('dve', route[:, qd * 4:qd * 4 + 4, 0], A(7), A(3), ALU.mult, ['s7', 's3'], rk_)
                kb.tt('dve', route[:, qd * 4:qd * 4 + 4, 1], A(8), A(3), ALU.mult, ['s8', 's3'], rk_)
                o4 = bc(oh4[:].unsqueeze(3), [128, 4, 4, 8])
                kb.tt('dve', OH1[:], o4, bc(oh1[:].unsqueeze(2), [128, 4, 4, 8]), ALU.mult, ['oh4', 'oh1'], ['OH1'])
                kb.tt('dve', OH2[:], o4, bc(oh2[:].unsqueeze(2), [128, 4, 4, 8]), ALU.mult, ['oh4', 'oh2'], ['OH2'])
                f3 = lambda t: t[:].rearrange("p b g j -> p b (g j)")
                kb.tt('dve', cntB[:], f3(OH1), f3(OH2), ALU.add, ['OH1', 'OH2'], ['cntB'])
                psp, kpp = kb.psum()
                for b_ in range(4):
                    kb.mm(psp[:, b_ * 32:(b_ + 1) * 32], ustrB[:], cntB[:, b_, :], True, b_ == 0, ['ustrB', 'cntB'], [kpp])
                    for b2 in range(b_):
                        kb.mm(psp[:, b_ * 32:(b_ + 1) * 32], onesB[:], cntB[:, b2, :], False, b2 == b_ - 1, ['onesB', 'cntB'], [kpp])
                kb.tt('dve', pre[:], psp[:, 0:128].rearrange("p (b e) -> p b e", e=32), bc(tot[:].unsqueeze(1), [128, 4, 32]), ALU.add, [kpp, 'tot'], ['pre'])
                pst2, kpt2 = kb.psum()
                for b_ in range(4):
                    kb.mm(pst2[:, 0:32], onesB[:], cntB[:, b_, :], b_ == 0, b_ == 3, ['onesB', 'cntB'], [kpt2])
                kb.tt('dve', tot[:], tot[:], pst2[:, 0:32], ALU.add, ['tot', kpt2], ['tot'])
                dk_ = [('desti', qd * 4 + i) for i in range(4)]
                for k2, OHk in ((0, OH1), (1, OH2)):
                    kb.tt('dve', t32[:], f3(OHk), pre[:], ALU.mult, ['OH1', 'OH2', 'pre'], ['t32'])
                    red(A(9), t32[:], ALU.add, ['t32'], ['s9'])
                    kb.tt('dve', t32[:], f3(OHk), bc(iota32[:].unsqueeze(1), [128, 4, 32]), ALU.mult, ['OH1', 'OH2', 'iota32'], ['t32'])
                    red(A(10), t32[:], ALU.add, ['t32'], ['s10'])
                    kb.stt(A(11), A(10), float(CAP), A(9), ALU.mult, ALU.add, ['s10', 's9'], ['s11'])
                    kb.ts('dve', A(11), A(11), float(NEXP * CAP - 1), None, ALU.min, None, ['s11'], ['s11'])
                    kb.copy('dve', desti[:, qd * 4:qd * 4 + 4, k2], A(11), ['s11'], dk_)

            def c_scatter(ob):
                d_ = cst.pop(ob)
                hnb, khb = d_['hnb'], d_['khb']
                for k2 in range(2):
                    kb.P.add('pool', (lambda ob, k2, hnb: lambda e: e.indirect_dma_start(
                        out=xs_s[:, :], out_offset=bass.IndirectOffsetOnAxis(ap=desti[:, ob, k2:k2 + 1], axis=0),
                        in_=hnb[:], in_offset=None))(ob, k2, hnb),
                        [khb, ('desti', ob)], ['xs_s'], dma=True)

            c_load(0)
            if NOB > 1:
                c_load(1)
            c_part1(0)
            pend_sc = []
            for ob in range(NOB):
                if ob + 2 < NOB:
                    c_load(ob + 2)
                if ob + 1 < NOB:
                    c_part1(ob + 1)
                c_part2(ob)
                if pend_sc:
                    c_scatter(pend_sc.pop(0))
                if ob % 4 == 3:
                    c_route(ob // 4)
                    pend_sc.extend(range(ob - 3, ob + 1))
            while pend_sc:
                c_scatter(pend_sc.pop(0))
            if dbg:
                o = dbg_out('d_route', [128, NOB * 2], F32)
                kb.dma(o, route[:].rearrange("p a b -> p (a b)"), r=[('route', ob) for ob in range(NOB)], w=['d_route'])
                o = dbg_out('d_desti', [128, NOB * 2], I32)
                kb.dma(o, desti[:].rearrange("p a b -> p (a b)"), r=[('desti', ob) for ob in range(NOB)], w=['d_desti'])
                o = dbg_out('d_x1', [OT, 1024], F32)
                kb.dma(o, x1_s, r=[('x1_s', ob) for ob in range(NOB)], w=['d_x1'])
        P.barrier()
        if stop_after == 'C':
            P.add('sp', None, reads=[k for k in dbgo.keys() if k != 'd_mlo'] + [('d_mlo', gob) for gob in range(NOB)] + ['xs_s'])
            P.emit(top)
            return nc, D, dbgo

        NBLK = CAP // 128
        with ExitStack() as sd_:
            sbd = lambda name, shape, dt: kb.sb(sd_, name, shape, dt)
            kb.ps = [sd_.enter_context(nc.psum_tensor('psD%d' % k, [128, 512], F32)) for k in range(8)]
            wg_r = Ring(kb, sd_, 'wg', [128, 8, 256], BF16, 3); wu_r = Ring(kb, sd_, 'wu', [128, 8, 256], BF16, 3)
            wd_r = Ring(kb, sd_, 'wd', [128, 2, 1024], BF16, 3)
            xsb_r = Ring(kb, sd_, 'xsb', [128, 1024], BF16, 3 * NBLK)
            xsT_r = Ring(kb, sd_, 'xsT', [128, 8, CAP], BF16, 2)
            sg_r = Ring(kb, sd_, 'sg', [128, CAP], F32, 2)
            aT_r = Ring(kb, sd_, 'actT', [128, 2, CAP], BF16, 2)
            yb_r = Ring(kb, sd_, 'yb', [128, 1024], F32, 3)
            dst = {}

            def d_load(e_):
                wg, kwg = wg_r.next(); wu, kwu = wu_r.next(); wd, kwd = wd_r.next()
                kb.dma(wg[:], w_ge_d[e_].rearrange("(c p) f -> p c f", p=128), w=[kwg], q='pool')
                kb.dma(wu[:], w_ue_d[e_].rearrange("(c p) f -> p c f", p=128), w=[kwu], q='pool')
                kb.dma(wd[:], w_de_d[e_].rearrange("(c p) n -> p c n", p=128), w=[kwd], q='pool')
                xs_l = []
                for blk in range(NBLK):
                    xsb, kxb = xsb_r.next()
                    r0 = e_ * CAP + blk * 128
                    kb.dma(xsb[:], xs_s[r0:r0 + 128, :], r=['xs_s'], w=[kxb])
                    xs_l.append((xsb, kxb))
                dst[e_] = (wg, kwg, wu, kwu, wd, kwd, xs_l)

            dst2 = {}

            def d_tr(e_):
                wg, kwg, wu, kwu, wd, kwd, xs_l = dst.pop(e_)
                xsT, kxT = xsT_r.next()
                for blk in range(NBLK):
                    xsb, kxb = xs_l[blk]
                    pst, kp = kb.psum()
                    pstb = pst[:].bitcast(BF16)
                    for c in range(8):
                        kb.tr(pstb[:, c * 128:(c + 1) * 128], xsb[:, c * 128:(c + 1) * 128], identB[:], [kxb, 'identB'], [kp])
                    kb.copy('dve' if blk % 2 == 0 else 'act', xsT[:, :, blk * 128:(blk + 1) * 128], pstb.rearrange("p (c t) -> p c t", c=8), [kp], [kxT])
                dst2[e_] = (wg, kwg, wu, kwu, wd, kwd, xsT, kxT)

            def d_gateup(e_):
                wg, kwg, wu, kwu, wd, kwd, xsT, kxT = dst2[e_]
                aT, kaT = aT_r.next()
                dst2[e_] = (wd, kwd, aT, kaT)
                for ft in range(2):
                    psg_, kpg_ = kb.psum(); psu_, kpu_ = kb.psum()
                    for c in range(8):
                        kb.mm(psg_[:, 0:CAP], wg[:, c, ft * 128:(ft + 1) * 128], xsT[:, c, :], c == 0, c == 7, [kwg, kxT], [kpg_])
                    for c in range(8):
                        kb.mm(psu_[:, 0:CAP], wu[:, c, ft * 128:(ft + 1) * 128], xsT[:, c, :], c == 0, c == 7, [kwu, kxT], [kpu_])
                    sg, ksg = sg_r.next()
                    kb.act(sg[:], psg_[:, 0:CAP], AF.Silu, [kpg_], [ksg])
                    kb.tt('dve', aT[:, ft, :], sg[:], psu_[:, 0:CAP], ALU.mult, [ksg, kpu_], [kaT])

            def d_down(e_):
                wd, kwd, aT, kaT = dst2.pop(e_)
                for blk in range(NBLK):
                    yb, kyb = yb_r.next()
                    for half in range(2):
                        psy, kpy = kb.psum()
                        for ft in range(2):
                            kb.mm(psy[:, :], aT[:, ft, blk * 128:(blk + 1) * 128], wd[:, ft, half * 512:(half + 1) * 512], ft == 0, ft == 1, [kaT, kwd], [kpy])
                        kb.copy('act' if half == 0 else 'dve', yb[:, half * 512:(half + 1) * 512], psy[:, :], [kpy], [kyb])
                    r0 = e_ * CAP + blk * 128
                    kb.dma(ys_s[r0:r0 + 128, :], yb[:], r=[kyb], w=['ys_s'])

            d_load(0)
            d_load(1)
            d_tr(0)
            for e_ in range(NEXP):
                if e_ + 2 < NEXP:
                    d_load(e_ + 2)
                d_gateup(e_)
                if e_ + 1 < NEXP:
                    d_tr(e_ + 1)
                d_down(e_)
        P.barrier()

        with ExitStack() as se_:
            sbe = lambda name, shape, dt: kb.sb(se_, name, shape, dt)
            kb.ps = [se_.enter_context(nc.psum_tensor('psE%d' % k, [128, 512], F32)) for k in range(8)]
            wpg = sbe('wpg', [128, 8, 1024], BF16); wple = sbe('wple', [128, 2, 1024], BF16)
            kb.dma(wpg[:], w_pg_d.rearrange("(c p) n -> p c n", p=128), w=['wpg'], q='pool')
            kb.dma(wple[:], w_ple_d.rearrange("(c p) n -> p c n", p=128), w=['wple'], q='pool')
            gple = sbe('gple', [128, 1024], F32); gfin = sbe('gfin', [128, 1024], F32)
            kb.dma(gple[:], gple_d, w=['gple']); kb.dma(gfin[:], gfin_d, w=['gfin'])
            x1_r = Ring(kb, se_, 'x1e', [128, 1024], F32, 4)
            y_r = Ring(kb, se_, 'ye', [128, 1024], F32, 8)
            x2_r = Ring(kb, se_, 'x2e', [128, 1024], F32, 3)
            hp_r = Ring(kb, se_, 'hpe', [128, 1024], BF16, 3)
            hpT_r = Ring(kb, se_, 'hpT', [128, 8, 128], BF16, 3)
            jk_r = Ring(kb, se_, 'jke', [128, 1024], BF16, 2)
            st_r = Ring(kb, se_, 'ste', [128, 4], F32, 4)
            sgm_r = Ring(kb, se_, 'sgm', [128, 1024], F32, 2)
            pt_r2 = Ring(kb, se_, 'pte', [128, 256], F32, 4)
            pb_r = Ring(kb, se_, 'pbe', [128, 256], BF16, 2)
            pT_r = Ring(kb, se_, 'pTe', [128, 2, 128], BF16, 3)
            x3_r = Ring(kb, se_, 'x3e', [128, 1024], F32, 2)
            o_r = Ring(kb, se_, 'oe', [128, 1024], F32, 2)
            est = {}

            def e_load(ob):
                x1t, kx1 = x1_r.next(); y1, ky1 = y_r.next(); y2, ky2 = y_r.next(); ptile, kpt_ = pt_r2.next()
                kb.dma(x1t[:], x1_s[ob * 128:(ob + 1) * 128, :], r=[('x1_s', ob)], w=[kx1])
                for k2, (yt, kyt) in enumerate(((y1, ky1), (y2, ky2))):
                    kb.P.add('pool', (lambda ob, k2, yt: lambda e: e.indirect_dma_start(
                        out=yt[:], out_offset=None, in_=ys_s[:, :],
                        in_offset=bass.IndirectOffsetOnAxis(ap=desti[:, ob, k2:k2 + 1], axis=0)))(ob, k2, yt), ['ys_s', ('desti', ob)], [kyt], dma=True)
                kb.dma(ptile[:], pvd[ob * 128:(ob + 1) * 128, :], w=[kpt_])
                est[ob] = (x1t, kx1, y1, ky1, y2, ky2, ptile, kpt_)

            def e_compute(ob):
                x1t, kx1, y1, ky1, y2, ky2, ptile, kpt_ = est.pop(ob)
                x2, kx2 = x2_r.next()
                kb.stt(x2[:], y1[:], route[:, ob, 0:1], x1t[:], ALU.mult, ALU.add, [ky1, ('route', ob), kx1], [kx2])
                kb.stt(x2[:], y2[:], route[:, ob, 1:2], x2[:], ALU.mult, ALU.add, [ky2, ('route', ob), kx2], [kx2])
                jk, kjk = jk_r.next(); stk = st_r.next()
                rinv = rms_rinv(stk, x2, kx2, jk, kjk, pool_pow=True)
                hp, khp = hp_r.next()
                kb.stt(hp[:], x2[:], rinv, gple[:], ALU.mult, ALU.mult, [kx2, stk[1], 'gple'], [khp])
                est1[ob] = (x2, kx2, hp, khp, ptile, kpt_)

            def e_compute1b(ob):
                x2, kx2, hp, khp, ptile, kpt_ = est1.pop(ob)
                hpT, khpT = hpT_r.next()
                pst, kp = kb.psum()
                pstb = pst[:].bitcast(BF16)
                for c in range(8):
                    kb.tr(pstb[:, c * 128:(c + 1) * 128], hp[:, c * 128:(c + 1) * 128], identB[:], [khp, 'identB'], [kp])
                kb.copy('act', hpT[:].rearrange("p c t -> p (c t)"), pstb, [kp], [khpT])
                pbt, kpb = pb_r.next(); pT, kpT = pT_r.next()
                kb.copy('act', pbt[:], ptile[:], [kpt_], [kpb])
                pst, kp = kb.psum()
                pstb = pst[:].bitcast(BF16)
                for c in range(2):
                    kb.tr(pstb[:, c * 128:(c + 1) * 128], pbt[:, c * 128:(c + 1) * 128], identB[:], [kpb, 'identB'], [kp])
                kb.copy('act', pT[:].rearrange("p c t -> p (c t)"), pstb[:, 0:256], [kp], [kpT])
                est2[ob] = (x2, kx2, hpT, khpT, pT, kpT)

            def e_compute2(ob):
                x2, kx2, hpT, khpT, pT, kpT = est2.pop(ob)
                sgm, ksg = sgm_r.next(); x3, kx3 = x3_r.next()
                for half in range(2):
                    hs = slice(half * 512, (half + 1) * 512)
                    psg_, kpg_ = kb.psum()
                    for c in range(8):
                        kb.mm(psg_[:, :], hpT[:, c, :], wpg[:, c, hs], c == 0, c == 7, [khpT, 'wpg'], [kpg_])
                    kb.act(sgm[:, hs], psg_[:, :], AF.Sigmoid, [kpg_], [ksg])
                    psp, kpp = kb.psum()
                    for c in range(2):
                        kb.mm(psp[:, :], pT[:, c, :], wple[:, c, hs], c == 0, c == 1, [kpT, 'wple'], [kpp])
                    kb.tt('dve', x3[:, hs], psp[:, :], sgm[:, hs], ALU.mult, [kpp, ksg], [kx3])
                kb.tt('pool', x3[:], x3[:], x2[:], ALU.add, [kx3, kx2], [kx3])
                jk, kjk = jk_r.next(); stk = st_r.next()
                rinv = rms_rinv(stk, x3, kx3, jk, kjk, pool_pow=True)
                ot, kot = o_r.next()
                kb.stt(ot[:], x3[:], rinv, gfin[:], ALU.mult, ALU.mult, [kx3, stk[1], 'gfin'], [kot])
                kb.dma(out_d[ob * 128:(ob + 1) * 128, :], ot[:], r=[kot], w=['out'])

            est2 = {}
            est1 = {}
            e_load(0)
            if NOB > 1:
                e_load(1)
            e_compute(0)
            e_compute1b(0)
            for ob in range(NOB):
                if ob + 2 < NOB:
                    e_load(ob + 2)
                if ob + 1 < NOB:
                    e_compute(ob + 1)
                e_compute2(ob)
                if ob + 1 < NOB:
                    e_compute1b(ob + 1)

        P.add('sp', None, reads=['out'] + [k for k in dbgo.keys() if k != 'd_mlo'] + ([('d_mlo', gob) for gob in range(NOB)] if dbg else []))
        P.emit(top)
    return nc, D, dbgo


def _consts():
    c = {}
    c['ident'] = np.eye(128, dtype=np.float32)
    i = np.arange(128)
    c['trilT'] = (i[:, None] <= i[None, :]).astype(np.float32)
    c['attmask'] = ((i[:, None] // 64) <= (i[None, :] // 64)).astype(np.float32)
    pb = np.zeros((96, 96), np.float32)
    for j in range(16):
        pb[64 + j + 16, 64 + j] = -1.0
        pb[64 + j, 64 + j + 16] = 1.0
    c['permB'] = pb
    inv = (np.float32(1.0) / (np.float32(10000.0) ** (np.arange(0, 32, 2, dtype=np.float32) / np.float32(32)))).astype(np.float32)
    f = np.zeros((96, 1), np.float32)
    f[64:80, 0] = inv
    f[80:96, 0] = inv
    c['invfreq'] = f
    dm = np.zeros((4, 4, GB), np.float32)
    for k in range(4):
        dm[k, k, :] = 1.0
    c['dmask'] = dm.reshape(4, 4 * GB)
    c['ustrict'] = (i[:, None] < i[None, :]).astype(np.float32)
    c['iota32'] = np.broadcast_to(np.arange(32, dtype=np.float32), (128, 32)).copy()
    return c


def _prep_weights(I):
    f = lambda k: np.asarray(I[k], dtype=np.float32)
    w = {}
    w_in = f('w_in')[0]
    offs = np.cumsum([0, 256, 128, 32, 512, 512, 4, 4])
    cq, ckv, kr, xm, z, gi, gf = [w_in[:, offs[i]:offs[i + 1]] for i in range(7)]
    w['w_in_r'] = np.ascontiguousarray(np.concatenate(
        [ckv, xm, np.zeros((1024, 64), np.float32), kr, gi, gf, cq, z], axis=1))
    assert w['w_in_r'].shape[1] == NCOL
    w['gmix_col'] = np.ascontiguousarray(f('norm_mix_g')[0].reshape(8, 128).T)
    w['gq_col'] = np.ascontiguousarray(f('q_norm_g')[0].reshape(2, 128).T)
    w['gkv_col'] = np.ascontiguousarray(f('kv_norm_g')[0].reshape(128, 1))
    w['w_uq'] = f('w_uq')[0]
    w['w_ukv'] = f('w_ukv')[0]
    cw = f('conv_w')[0]
    w['convw_col'] = np.ascontiguousarray(cw.reshape(4, 4, 128).transpose(2, 1, 0).reshape(128, 16))
    w['convb_col'] = np.ascontiguousarray(f('conv_b')[0].reshape(4, 128).T)
    w['w_mq'] = f('w_mq')[0]; w['w_mk'] = f('w_mk')[0]; w['w_mv'] = f('w_mv')[0]
    w['bi_col'] = f('b_igate')[0].reshape(4, 1); w['bf_col'] = f('b_fgate')[0].reshape(4, 1)
    w['gmh_col'] = np.ascontiguousarray(f('mh_norm_g')[0].reshape(4, 128).T)
    w['skip_col'] = np.ascontiguousarray(f('ml_skip')[0].reshape(4, 128).T)
    w['w_o'] = f('w_o')[0]
    rep = lambda v: np.ascontiguousarray(np.broadcast_to(v.reshape(1, -1), (128, v.size)))
    w['gffn_rep'] = rep(f('norm_ffn_g')[0])
    w['w_router'] = np.ascontiguousarray(np.concatenate([f('w_router_group')[0], f('w_router_expert')[0]], axis=1))
    w['b_router_rep'] = rep(np.concatenate([f('b_router_group')[0], f('b_router_expert')[0]]))
    w['w_gate_e'] = f('w_gate_e')[0]; w['w_up_e'] = f('w_up_e')[0]; w['w_down_e'] = f('w_down_e')[0]
    w['gple_rep'] = rep(f('norm_ple_g')[0])
    w['w_ple'] = f('w_ple')[0]; w['w_ple_gate'] = f('w_ple_gate')[0]
    w['gfin_rep'] = rep(f('final_norm_g'))
    return w


def _prep_core(I, b, r, NVB):
    S = NVB * 128
    x = np.asarray(I['x'], dtype=np.float32)[b]
    pos = np.asarray(I['positions'])[b].astype(np.int32)
    p = np.asarray(I['p'], dtype=np.float32)[0, b]
    m = {}
    if r == 1:
        xv = x[:S]; posv = pos[:S]; valid = np.ones(S, np.float32); first_real = 0
    else:
        xv = np.concatenate([np.zeros((128, 1024), np.float32), x[:S - 128]], 0)
        posv = np.concatenate([np.zeros(128, np.int32), pos[:S - 128]])
        valid = np.concatenate([np.zeros(128, np.float32), np.ones(S - 128, np.float32)])
    own = np.zeros(S, bool)
    for vb in range(1, NVB, 2):
        own[vb * 128:(vb + 1) * 128] = True
    real_idx = np.arange(S) - (0 if r == 1 else 128)
    m['xv'] = np.ascontiguousarray(xv)
    m['posk'] = np.ascontiguousarray(np.broadcast_to(posv[None, :], (32, S)))
    m['posq'] = np.ascontiguousarray(np.broadcast_to(posv[own][None, :], (32, S // 2)))
    m['pv'] = np.ascontiguousarray(p[real_idx[own]])
    m['valid4'] = np.ascontiguousarray(np.broadcast_to(valid[None, :], (4, S)))
    m['ibias4'] = np.ascontiguousarray(np.broadcast_to(((valid - 1.0) * 1e4)[None, :], (4, S))).astype(np.float32)
    m['kbias'] = ((valid - 1.0) * 3e4).reshape(1, S).astype(np.float32)
    return m, real_idx[own]


_CACHE = {}


def kernel(**inputs):
    NVB = 64
    if 'nc' not in _CACHE:
        _CACHE['nc'] = build(NVB)
    nc, D, _ = _CACHE['nc']
    consts = _consts()
    w = _prep_weights(inputs)
    in_maps = []
    idx = []
    for core in range(8):
        b, r = core // 2, core % 2
        m, ridx = _prep_core(inputs, b, r, NVB)
        m.update(consts)
        m.update(w)
        in_maps.append({k: v for k, v in m.items() if k in D})
        idx.append((b, ridx))
    res = run_bass_kernel_spmd(nc, in_maps, core_ids=list(range(8)))
    out = np.zeros((4, 8192, 1024), np.float32)
    for core in range(8):
        b, ridx = idx[core]
        out[b, ridx] = np.asarray(res.results[core]['out'], dtype=np.float32)
    return out
```

```python
import numpy as np
from contextlib import ExitStack
import concourse.bass as bass
import concourse.mybir as mybir
from concourse.bass_utils import run_bass_kernel_spmd

F32 = mybir.dt.float32
BF16 = mybir.dt.bfloat16
I32 = mybir.dt.int32
ALU = mybir.AluOpType
AF = mybir.ActivationFunctionType
AX = mybir.AxisListType

NDS = 24
EPS = 1e-6
CAP = 384
NEXP = 32
GB = 4
C_CKV, C_XM, C_MISC, C_GI, C_GF, C_CQ, C_Z, NCOL = 0, 128, 640, 736, 740, 744, 1000, 1512
QSCALE = 1.0 / np.sqrt(96.0)


class Prog:
    def __init__(self, nc):
        self.nc = nc
        self.ops = []
        self.last_w = {}
        self.readers = {}
        self.dma_count = {}
        self.dma_ops = {}

    def add(self, eng, fn, reads=(), writes=(), dma=False):
        i = len(self.ops)
        deps = set()
        ex = [r for r in reads if isinstance(r, tuple) and isinstance(r[0], str) and r[0].startswith('ps')]
        if ex:
            reads = [r for r in reads if r not in ex]
            writes = list(writes) + ex
        for r in reads:
            if r in self.last_w:
                deps.add(self.last_w[r])
        for w in writes:
            if w in self.last_w:
                deps.add(self.last_w[w])
            deps.update(self.readers.get(w, ()))
        for r in reads:
            self.readers.setdefault(r, []).append(i)
        for w in writes:
            self.last_w[w] = i
            self.readers[w] = []
        deps.discard(i)
        op = dict(eng=eng, fn=fn, deps=deps, dma=dma, sig=None)
        if dma:
            lst = self.dma_ops.setdefault(eng, [])
            op['dk'] = len(lst)
            if len(lst) >= NDS:
                deps.add(lst[len(lst) - NDS])
            lst.append(i)
        self.ops.append(op)
        return i

    def barrier(self):
        engs = ['pe', 'act', 'dve', 'pool', 'sp']
        last = []
        for e in engs:
            for j in range(len(self.ops) - 1, -1, -1):
                o = self.ops[j]
                if o['eng'] == e and not o['dma'] and o['fn'] is not None:
                    last.append(j)
                    break
        for e, lst in self.dma_ops.items():
            last.extend(lst[-NDS:])
        for e in engs:
            i = len(self.ops)
            self.ops.append(dict(eng=e, fn=None, deps=set(last), dma=False, sig=None))

    def emit(self, stack):
        nc = self.nc
        ops = self.ops
        engs = ['pe', 'act', 'dve', 'pool', 'sp']
        csem = {e: stack.enter_context(nc.semaphore('c_' + e)) for e in engs}
        dsem = {e: [stack.enter_context(nc.semaphore('d_%s_%d' % (e, k))) for k in range(NDS)]
                for e in self.dma_ops}
        need = [False] * len(ops)
        for o in ops:
            for d in o['deps']:
                p = ops[d]
                if (not p['dma']) and (not o['dma']) and p['eng'] == 'pe' and o['eng'] == 'pe' and o['fn'] is not None:
                    continue
                need[d] = True
        cnt = {e: 0 for e in engs}
        for i, o in enumerate(ops):
            if o['dma']:
                k = o['dk']
                o['sig'] = (dsem[o['eng']][k % NDS], 16 * (k // NDS + 1), 16)
            elif need[i] and o['fn'] is not None:
                cnt[o['eng']] += 1
                o['sig'] = (csem[o['eng']], cnt[o['eng']], 1)
        per = {e: [o for o in ops if o['eng'] == e] for e in engs}

        def run(ename, eobj):
            waited = {}
            for o in per[ename]:
                for d in sorted(o['deps']):
                    p = ops[d]
                    if p['sig'] is None:
                        continue
                    if (not p['dma']) and (not o['dma']) and p['eng'] == 'pe' and ename == 'pe' and o['fn'] is not None:
                        continue
                    sem, val, _ = p['sig']
                    if waited.get(sem.name, 0) < val:
                        eobj.wait_ge(sem, val)
                        waited[sem.name] = val
                if o['fn'] is None:
                    continue
                ins = o['fn'](eobj)
                if o['sig'] is not None:
                    sem, val, inc = o['sig']
                    ins.then_inc(sem, inc)

        with nc.Block() as block:
            @block.tensor
            def _(e):
                run('pe', e)

            @block.scalar
            def _(e):
                run('act', e)

            @block.vector
            def _(e):
                run('dve', e)

            @block.gpsimd
            def _(e):
                run('pool', e)

            @block.sync
            def _(e):
                run('sp', e)


class Ring:
    def __init__(self, kb, stack, name, shape, dtype, n):
        self.t = [stack.enter_context(kb.nc.sbuf_tensor('s_%s_%d' % (name, k), shape, dtype)) for k in range(n)]
        self.name = name
        self.i = 0

    def next(self):
        k = self.i % len(self.t)
        self.i += 1
        return self.t[k], (self.name, k)


class KB:
    def __init__(self, nc):
        self.nc = nc
        self.P = Prog(nc)
        self.psi = 0
        self.ps = None

    def sb(self, st, name, shape, dt):
        return st.enter_context(self.nc.sbuf_tensor('s_' + name, shape, dt))

    def psum(self):
        k = self.psi % 8
        self.psi += 1
        return self.ps[k], ('ps', k)

    def dma(self, out, in_, r=(), w=(), q='sp'):
        self.P.add(q, lambda e: e.dma_start(out=out, in_=in_), r, w, dma=True)

    def act(self, out, in_, func, r, w, bias=None, scale=None, accum=None):
        kw = {}
        if bias is not None:
            kw['bias'] = bias
        if scale is not None:
            kw['scale'] = scale
        if accum is not None:
            kw['accum_out'] = accum
        self.P.add('act', lambda e: e.activation(out=out, in_=in_, func=func, **kw), r, w)

    def tt(self, eng, out, in0, in1, op, r, w):
        self.P.add(eng, lambda e: e.tensor_tensor(out=out, in0=in0, in1=in1, op=op), r, w)

    def ts(self, eng, out, in0, s1, s2, op0, op1, r, w):
        if s2 is None:
            self.P.add(eng, lambda e: e.tensor_scalar(out=out, in0=in0, scalar1=s1, scalar2=None, op0=op0), r, w)
        else:
            self.P.add(eng, lambda e: e.tensor_scalar(out=out, in0=in0, scalar1=s1, scalar2=s2, op0=op0, op1=op1), r, w)

    def stt(self, out, in0, scalar, in1, op0, op1, r, w):
        self.P.add('dve', lambda e: e.scalar_tensor_tensor(out=out, in0=in0, scalar=scalar, in1=in1, op0=op0, op1=op1), r, w)

    def copy(self, eng, out, in_, r, w):
        if eng == 'act':
            self.P.add('act', lambda e: e.copy(out=out, in_=in_), r, w)
        else:
            self.P.add(eng, lambda e: e.tensor_copy(out=out, in_=in_), r, w)

    def memset(self, eng, ap, val, w):
        self.P.add(eng, lambda e: e.memset(ap, val), (), w)

    def mm(self, out, lhsT, rhs, start, stop, r, w, skip=False):
        if skip:
            self.P.add('pe', lambda e: e.matmul(out, lhsT, rhs, start=start, stop=stop, skip_group_check=True), r, w)
        else:
            self.P.add('pe', lambda e: e.matmul(out, lhsT, rhs, start=start, stop=stop), r, w)

    def tr(self, out, in_, ident, r, w):
        self.P.add('pe', lambda e: e.transpose(out=out, in_=in_, identity=ident), r, w)

    def scan(self, out, d0, d1, init, op0, op1, r, w):
        self.P.add('dve', lambda e: e.tensor_tensor_scan(out=out, data0=d0, data1=d1, initial=init, op0=op0, op1=op1), r, w)


def bc(ap, shape):
    return ap.broadcast_to(shape)


def build(NVB, stop_after=None, dbg=False):
    VT = NVB * 128
    NOB = NVB // 2
    OT = NOB * 128
    NG = NVB // GB
    GT = GB * 128
    GO = GT // 2
    nc = bass.Bass("TRN2", target_bir_lowering=False)
    D = {}

    def din(name, shape, dt=F32):
        D[name] = nc.dram_tensor(name, list(shape), dt, kind="ExternalInput").ap()
        return D[name]

    def dscr(name, shape, dt):
        D[name] = nc.dram_tensor(name, list(shape), dt, kind="Internal").ap()
        return D[name]

    def dout(name, shape, dt=F32):
        D[name] = nc.dram_tensor(name, list(shape), dt, kind="ExternalOutput").ap()
        return D[name]

    xv = din('xv', [VT, 1024]); posk = din('posk', [32, VT], I32); posq = din('posq', [32, OT], I32)
    pvd = din('pv', [OT, 256])
    valid4 = din('valid4', [4, VT]); ibias4 = din('ibias4', [4, VT]); kbias = din('kbias', [1, VT])
    ident_d = din('ident', [128, 128]); tril_d = din('trilT', [128, 128]); attm_d = din('attmask', [128, 128])
    perm_d = din('permB', [96, 96]); invf_d = din('invfreq', [96, 1]); dmask_d = din('dmask', [4, 4 * GB])
    ustr_d = din('ustrict', [128, 128]); din('iota32', [128, 32])
    w_in_d = din('w_in_r', [1024, NCOL]); gmix_d = din('gmix_col', [128, 8])
    gq_d = din('gq_col', [128, 2]); gkv_d = din('gkv_col', [128, 1])
    w_uq_d = din('w_uq', [256, 768]); w_ukv_d = din('w_ukv', [128, 1024])
    convw_d = din('convw_col', [128, 16]); convb_d = din('convb_col', [128, 4])
    w_mq_d = din('w_mq', [4, 128, 128]); w_mk_d = din('w_mk', [4, 128, 128]); w_mv_d = din('w_mv', [4, 128, 128])
    bi_d = din('bi_col', [4, 1]); bf_d = din('bf_col', [4, 1])
    gmh_d = din('gmh_col', [128, 4]); skip_d = din('skip_col', [128, 4])
    w_o_d = din('w_o', [1024, 1024]); gffn_d = din('gffn_rep', [128, 1024])
    w_rt_d = din('w_router', [1024, 36]); b_rt_d = din('b_router_rep', [128, 36])
    w_ge_d = din('w_gate_e', [NEXP, 1024, 256]); w_ue_d = din('w_up_e', [NEXP, 1024, 256]); w_de_d = din('w_down_e', [NEXP, 256, 1024])
    gple_d = din('gple_rep', [128, 1024]); w_ple_d = din('w_ple', [256, 1024]); w_pg_d = din('w_ple_gate', [1024, 1024])
    gfin_d = din('gfin_rep', [128, 1024])
    out_d = dout('out', [OT, 1024])
    mlo_s = dscr('mlo_s', [4, 128, OT], BF16)
    x1_s = dscr('x1_s', [OT, 1024], F32)
    xs_s = dscr('xs_s', [NEXP * CAP, 1024], BF16)
    ys_s = dscr('ys_s', [NEXP * CAP, 1024], F32)
    cq_s = dscr('cq_s', [2, 128, OT], BF16); ckv_s = dscr('ckv_s', [128, VT], BF16)
    kr_s = dscr('kr_s', [32, VT], BF16); tab_s = dscr('tab_s', [2, 32, OT], F32)
    dbgo = {}

    def dbg_out(name, shape, dt=F32):
        dbgo[name] = dout(name, shape, dt)
        return dbgo[name]

    dbg_mlo = dbg_out('d_mlo', [4, 128, OT], BF16) if dbg else None
    kb = KB(nc)
    P = kb.P
    with ExitStack() as top:
        sb = lambda name, shape, dt: kb.sb(top, name, shape, dt)
        identF = sb('identF', [128, 128], F32); identB = sb('identB', [128, 128], BF16)
        trilT = sb('trilT', [128, 128], F32); attm = sb('attm', [128, 128], BF16)
        permB = sb('permB', [96, 96], F32); invf = sb('invf', [96, 1], F32)
        dmask = sb('dmask', [4, 4 * GB], F32); onesF = sb('onesF', [128, 128], F32)
        mhalf = sb('mhalf', [128, 1], F32); epsc = sb('epsc', [128, 1], F32)
        for t, d in ((identF, ident_d), (trilT, tril_d), (permB, perm_d), (invf, invf_d), (dmask, dmask_d)):
            kb.dma(t[:], d, w=[t.name[2:]])
        kb.dma(attm[:], attm_d, w=['attm'], q='pool')
        kb.copy('dve', identB[:], identF[:], ['identF'], ['identB'])
        kb.memset('pool', onesF[:], 1.0, ['onesF'])
        kb.memset('pool', mhalf[:], -0.5, ['mhalf'])
        kb.memset('pool', epsc[:], EPS, ['epsc'])
        zt = sb('zt', [128, 2, 1024], BF16)
        kb.memset('pool', zt[:], 0.0, ['zt'])
        C1 = 6.28125
        C2 = 2.0 * np.pi - 6.28125
        MAGIC = 12582912.0

        with ExitStack() as sa:
            sba = lambda name, shape, dt: kb.sb(sa, name, shape, dt)
            kb.ps = [sa.enter_context(nc.psum_tensor('psA%d' % k, [128, 512], F32)) for k in range(8)]
            win = sba('win', [128, 8, NCOL], BF16)
            gmix = sba('gmix', [128, 8], F32); gq = sba('gq', [128, 2], F32); gkv = sba('gkv', [128, 1], F32)
            convw = sba('convw', [128, 16], F32); convb = sba('convb', [128, 4], F32)
            cdiag = sba('cdiag', [128, 16, 128], F32)
            wmq = sba('wmq', [128, 4, 128], BF16); wmk = sba('wmk', [128, 4, 128], BF16); wmv = sba('wmv', [128, 4, 128], BF16)
            bi = sba('bi', [4, 1], F32); bfc = sba('bfc', [4, 1], F32); nbf = sba('nbf', [4, 1], F32)
            gmh = sba('gmh', [128, 4], F32); skipc = sba('skipc', [128, 4], F32)
            ones4 = sba('ones4', [4, 128], F32); onesr = sba('onesr', [4, GT], F32)
            for t, d in ((gmix, gmix_d), (gq, gq_d), (gkv, gkv_d), (convw, convw_d), (convb, convb_d),
                         (bi, bi_d), (bfc, bf_d), (gmh, gmh_d), (skipc, skip_d)):
                kb.dma(t[:], d, w=[t.name[2:]])
            kb.ts('pool', nbf[:], bfc[:], -1.0, 0.0, ALU.mult, ALU.add, ['bfc'], ['nbf'])
            kb.memset('pool', ones4[:], 1.0, ['ones4'])
            kb.memset('pool', onesr[:], 1.0, ['onesr'])
            for t, d in ((wmq, w_mq_d), (wmk, w_mk_d), (wmv, w_mv_d)):
                kb.dma(t[:], d.rearrange("h d e -> d h e"), w=[t.name[2:]], q='pool')
            with ExitStack() as sw:
                stg = Ring(kb, sw, 'wstg', [128, NCOL], F32, 8)
                w_in_v = w_in_d.rearrange("(c p) n -> p c n", p=128)
                for c in range(8):
                    t, k = stg.next()
                    kb.dma(t[:], w_in_v[:, c, :], w=[k])
                    kb.ts('dve', win[:, c, :], t[:], gmix[:, c:c + 1], None, ALU.mult, None, [k, 'gmix'], ['win'])
            P.barrier()
            zf_todo = list(range(0, NEXP * CAP, 256))

            def zero_fill(n):
                for _ in range(n):
                    if zf_todo:
                        r0 = zf_todo.pop(0)
                        kb.dma(xs_s[r0:r0 + 256, :].rearrange('(p k) d -> p k d', k=2), zt[:], r=['zt'], w=['xs_s'], q='pool')
            for j in range(16):
                kb.ts('pool', cdiag[:, j, :], identF[:], convw[:, j:j + 1], 0.0, ALU.mult, ALU.add, ['identF', 'convw'], ['cdiag'])

            xt_r = Ring(kb, sa, 'xt', [128, 1024], F32, 4)
            hb_r = Ring(kb, sa, 'hb', [128, 1024], BF16, 3)
            st_r = Ring(kb, sa, 'st', [128, 4], F32, 4)
            hT_r = Ring(kb, sa, 'hT', [128, 8, GT], BF16, 1)
            sq_r = Ring(kb, sa, 'sq', [128, GT], F32, 2)
            rv_r = Ring(kb, sa, 'rv', [128, GT], F32, 2)
            lat_r = Ring(kb, sa, 'lat', [128, GT], BF16, 2)
            xmF = sba('xmF', [128, 4, 4 + GT], F32)
            krF = sba('krF', [96, GT], F32)
            rt1 = sba('rt1', [96, GT], F32); rt2 = sba('rt2', [96, GT], F32); krB = sba('krB', [96, GT], BF16)
            tab_r = Ring(kb, sa, 'tab', [96, 2, GT], F32, 2)
            posi = sba('posi', [96, GT], I32); angf = sba('angf', [96, GT], F32); rtmp = sba('rtmp', [96, GT], F32); rk = sba('rk', [96, GT], F32)
            gU = sba('gU', [4, GT], F32); gS = sba('gS', [4, GT], F32); gNB = sba('gNB', [4, GT], F32)
            gG = sba('gG', [4, GT], F32); gW = sba('gW', [4, GT], F32); gE = sba('gE', [4, GT], F32)
            gval = sba('gval', [4, GT], F32); gib = sba('gib', [4, GT], F32)
            Rall = sba('Rall', [4, GB + 1], F32); dec4 = sba('dec4', [4, GB], F32); Rm = sba('Rm', [4, 4, GB], F32)
            cNB = sba('cNB', [4, 1], F32); cG = sba('cG', [4, 1], F32)
            xmB_r = Ring(kb, sa, 'xmB', [128, 4, GT], BF16, 2)
            xcB_r = Ring(kb, sa, 'xcB', [128, 4, GT], BF16, 2)
            xcFo_r = Ring(kb, sa, 'xcFo', [128, 4, GO], F32, 2)
            zsF_r = Ring(kb, sa, 'zsF', [128, 4, GO], F32, 2)
            wcol_r = Ring(kb, sa, 'wcol', [128, GB, 8], F32, 2)
            dcol_r = Ring(kb, sa, 'dcol', [128, 4, GB], F32, 2)
            qT_r = Ring(kb, sa, 'qT', [128, 4, GO], BF16, 2)
            kT_r = Ring(kb, sa, 'kT', [128, 4, GO], BF16, 2)
            ktm_r = Ring(kb, sa, 'ktm', [128, 4, 128], BF16, 4)
            wv_r = Ring(kb, sa, 'wv', [128, 4, 132], BF16, 4)
            a0_r = Ring(kb, sa, 'a0', [128, 4, 128], BF16, 3)
            Cst = sba('Cst', [128, 4, 132], F32); Cd32 = sba('Cd32', [128, 4, 132], F32); Cd16 = sba('Cd16', [128, 4, 132], BF16)
            hN_r = Ring(kb, sa, 'hN', [128, 4, 128], F32, 2); hc_r = Ring(kb, sa, 'hc', [128, 4, 128], F32, 2)
            bst = sba('bst', [128, 4, 6], F32); bmv = sba('bmv', [128, 4, 2], F32); rstd = sba('rstd', [128, 4], F32)
            dcl = sba('dcl', [128, 4], F32); rden = sba('rden', [128, 4], F32)
            f1_r = Ring(kb, sa, 'f1', [128, 4, 128], F32, 2); f2_r = Ring(kb, sa, 'f2', [128, 4, 128], F32, 2)
            mlo_r = Ring(kb, sa, 'mlo', [128, 4, 128], BF16, 2)
            kb.memset('pool', xmF[:], 0.0, ['xmF'])
            kb.memset('pool', Cst[:], 0.0, ['Cst'])
            kb.memset('pool', cNB[:], 0.0, ['cNB'])
            kb.memset('pool', cG[:], 0.0, ['cG'])
            xv_b = xv.rearrange("(n p) d -> n p d", p=128)
            rows = slice(64, 96)
            v3 = lambda t: t[:].rearrange("p (b t) -> p b t", t=128)
            GS = {}

            def tables(g):
                tab, ktab = tab_r.next()
                T0 = g * GT
                kb.dma(posi[rows, :], posk[:, T0:T0 + GT], w=['posi'])
                kb.copy('dve', angf[rows, :], posi[rows, :], ['posi'], ['angf'])
                kb.ts('dve', angf[rows, :], angf[rows, :], invf[rows, 0:1], None, ALU.mult, None, ['angf', 'invf'], ['angf'])
                kb.ts('dve', rk[rows, :], angf[rows, :], 1.0 / (2 * np.pi), MAGIC, ALU.mult, ALU.add, ['angf'], ['rk'])
                kb.ts('dve', rk[rows, :], rk[rows, :], -MAGIC, None, ALU.add, None, ['rk'], ['rk'])
                kb.stt(rtmp[rows, :], rk[rows, :], -C1, angf[rows, :], ALU.mult, ALU.add, ['rk', 'angf'], ['rtmp'])
                kb.stt(rtmp[rows, :], rk[rows, :], -C2, rtmp[rows, :], ALU.mult, ALU.add, ['rk', 'rtmp'], ['rtmp'])
                kb.ts('dve', rtmp[rows, :], rtmp[rows, :], np.pi, -np.pi, ALU.min, ALU.max, ['rtmp'], ['rtmp'])
                kb.act(tab[rows, 1, :], rtmp[rows, :], AF.Sin, ['rtmp'], [ktab])
                kb.act(rtmp[rows, :], rtmp[rows, :], AF.Abs, ['rtmp'], ['rtmp'])
                kb.act(tab[rows, 0, :], rtmp[rows, :], AF.Sin, ['rtmp', 'halfpi'], [ktab], bias=halfpi[rows, 0:1], scale=-1.0)
                for ti in range(2):
                    kb.dma(tab_s[ti, :, g * GO:(g + 1) * GO].rearrange("p (b t) -> p b t", t=128),
                           tab[rows, ti, :].rearrange("p (b t) -> p b t", t=128)[:, 1::2, :], r=[ktab], w=[('tab_s', g)])
                return tab, ktab

            halfpi = sba('halfpi', [96, 1], F32)
            kb.memset('pool', halfpi[:], float(np.pi / 2), ['halfpi'])

            def P_part(g, part):
                T0 = g * GT
                O0 = g * GO
                if part == 0:
                    GS[g] = dict(tab=GS.pop(('tab', g)))
                    hT, khT = hT_r.next()
                    GS[g].update(hT=hT, khT=khT, xts=[])
                    for j in range(GB):
                        xt, kx = xt_r.next()
                        kb.dma(xt[:], xv_b[g * GB + j], w=[kx])
                        GS[g]['xts'].append((xt, kx))
                st = GS[g]
                hT, khT = st['hT'], st['khT']
                def a1_act(js):
                    for j in js:
                        xt, kx = st['xts'][j]
                        hb, kh = hb_r.next(); stt_, ks = st_r.next()
                        kb.act(hb[:], xt[:], AF.Square, [kx], [kh, ks], accum=stt_[:, 0:1])
                        kb.act(stt_[:, 1:2], stt_[:, 0:1], AF.Ln, [ks, 'epsc'], [ks], bias=epsc[:], scale=1.0 / 1024)
                        kb.act(stt_[:, 2:3], stt_[:, 1:2], AF.Exp, [ks], [ks], scale=-0.5)
                        kb.act(hb[:], xt[:], AF.Copy, [kx, ks], [kh], scale=stt_[:, 2:3])
                        st.setdefault('hbs', {})[j] = (hb, kh)

                def a1_pe(js):
                    for j in js:
                        hb, kh = st['hbs'].pop(j)
                        pst, kp = kb.psum()
                        pstb = pst[:].bitcast(BF16)
                        for c in range(8):
                            kb.tr(pstb[:, c * 128:(c + 1) * 128], hb[:, c * 128:(c + 1) * 128], identB[:], [kh, 'identB'], [kp])
                        kb.copy('dve', hT[:, :, j * 128:(j + 1) * 128], pstb.rearrange("p (c t) -> p c t", c=8), [kp], [khT])

                if part == 0:
                    a1_act((0, 1))
                    return
                if part == 1:
                    a1_pe((0, 1))
                    a1_act((2, 3))
                    return
                if part == 2:
                    a1_pe((2, 3))
                rhs_all = lambda c: hT[:, c, :]
                hT_own = lambda c: hT[:, c, :].rearrange("p (b t) -> p b t", t=128)[:, 1::2, :]
                if part == 2:
                    tab, ktab = st['tab']
                    xmB, kxmB = xmB_r.next(); zsF, kzs = zsF_r.next()
                    st.update(xmB=xmB, kxmB=kxmB, zsF=zsF, kzs=kzs)
                    for h in range(4):
                        ps, kp = kb.psum()
                        for c in range(8):
                            kb.mm(ps[:, :], win[:, c, C_XM + h * 128:C_XM + (h + 1) * 128], rhs_all(c), c == 0, c == 7, ['win', khT], [kp])
                        kb.copy('act', xmF[:, h, 4:4 + GT], ps[:, :], [kp], ['xmF'])
                        kb.copy('dve', xmB[:, h, :], ps[:, :], [kp], [kxmB])
                    psi_, kpi = kb.psum()
                    for c in range(8):
                        kb.mm(psi_[0:4, :], win[:, c, C_GI:C_GI + 4], rhs_all(c), c == 0, c == 7, ['win', khT], [kpi])
                    psf_, kpf = kb.psum()
                    for c in range(8):
                        kb.mm(psf_[0:4, :], win[:, c, C_GF:C_GF + 4], rhs_all(c), c == 0, c == 7, ['win', khT], [kpf])
                    kb.act(gU[:], psi_[0:4, :], AF.Identity, [kpi, 'bi'], ['gU'], bias=bi[:])
                    kb.act(gS[:], psf_[0:4, :], AF.Exp, [kpf, 'nbf'], ['gS'], bias=nbf[:], scale=-1.0)
                    kb.act(gS[:], gS[:], AF.Ln, ['gS'], ['gS'], bias=1.0)
                    ps, kp = kb.psum()
                    for c in range(8):
                        kb.mm(ps[:, :], win[:, c, C_CKV:C_CKV + 128], rhs_all(c), c == 0, c == 7, ['win', khT], [kp])
                    sq, ksq = sq_r.next(); rv, krv = rv_r.next(); lat, klat = lat_r.next()
                    kb.act(sq[:], ps[:, :], AF.Square, [kp], [ksq])
                    ps2, kp2 = kb.psum()
                    kb.mm(ps2[:, :], onesF[:], sq[:], True, True, ['onesF', ksq], [kp2])
                    kb.act(rv[:], ps2[:, :], AF.Ln, [kp2, 'epsc'], [krv], bias=epsc[:], scale=1.0 / 128)
                    kb.act(rv[:], rv[:], AF.Exp, [krv], [krv], scale=-0.5)
                    kb.stt(lat[:], ps[:, :], gkv[:, 0:1], rv[:], ALU.mult, ALU.mult, [kp, 'gkv', krv], [klat])
                    kb.dma(ckv_s[:, T0:T0 + GT], lat[:], r=[klat], w=[('ckv_s', g)])
                    ps, kp = kb.psum()
                    for c in range(8):
                        kb.mm(ps[0:96, :], win[:, c, C_MISC:C_MISC + 96], rhs_all(c), c == 0, c == 7, ['win', khT], [kp])
                    kb.copy('act', krF[rows, :], ps[rows, :], [kp], ['krF'])
                    ps2, kp2 = kb.psum()
                    kb.mm(ps2[0:96, :], permB[rows, :], krF[rows, :], True, True, ['permB', 'krF'], [kp2])
                    kb.tt('dve', rt1[rows, :], krF[rows, :], tab[rows, 0, :], ALU.mult, ['krF', ktab], ['rt1'])
                    kb.tt('dve', rt2[rows, :], ps2[rows, :], tab[rows, 1, :], ALU.mult, [kp2, ktab], ['rt2'])
                    kb.tt('dve', krB[rows, :], rt1[rows, :], rt2[rows, :], ALU.add, ['rt1', 'rt2'], ['krB'])
                    kb.dma(kr_s[:, T0:T0 + GT], krB[rows, :], r=['krB'], w=[('kr_s', g)])
                    psq = []
                    for q_ in range(2):
                        ps, kp = kb.psum()
                        for c in range(8):
                            kb.mm(ps[:, 0:GO], win[:, c, C_CQ + q_ * 128:C_CQ + (q_ + 1) * 128], hT_own(c), c == 0, c == 7, ['win', khT], [kp])
                        psq.append((ps, kp))
                    sq, ksq = sq_r.next(); rv, krv = rv_r.next(); lat, klat = lat_r.next()
                    for q_ in range(2):
                        kb.act(sq[:, q_ * GO:(q_ + 1) * GO], psq[q_][0][:, 0:GO], AF.Square, [psq[q_][1]], [ksq])
                    ps2, kp2 = kb.psum()
                    for q_ in range(2):
                        kb.mm(ps2[:, 0:GO], onesF[:], sq[:, q_ * GO:(q_ + 1) * GO], q_ == 0, q_ == 1, ['onesF', ksq], [kp2])
                    kb.act(rv[:, 0:GO], ps2[:, 0:GO], AF.Ln, [kp2, 'epsc'], [krv], bias=epsc[:], scale=1.0 / 256)
                    kb.act(rv[:, 0:GO], rv[:, 0:GO], AF.Exp, [krv], [krv], scale=-0.5)
                    for q_ in range(2):
                        kb.stt(lat[:, q_ * GO:(q_ + 1) * GO], psq[q_][0][:, 0:GO], gq[:, q_:q_ + 1], rv[:, 0:GO], ALU.mult, ALU.mult,
                               [psq[q_][1], 'gq', krv], [klat])
                    kb.dma(cq_s[:, :, O0:O0 + GO].rearrange("q p t -> p q t"), lat[:].rearrange("p (q t) -> p q t", q=2), r=[klat], w=[('cq_s', g)])
                    for h in range(4):
                        ps, kp = kb.psum()
                        for c in range(8):
                            kb.mm(ps[:, 0:GO], win[:, c, C_Z + h * 128:C_Z + (h + 1) * 128], hT_own(c), c == 0, c == 7, ['win', khT], [kp])
                        kb.act(zsF[:, h, :], ps[:, 0:GO], AF.Silu, [kp], [kzs])
                    return
                xcB, kxcB = xcB_r.next(); xcFo, kxcFo = xcFo_r.next()
                wcol, kwcol = wcol_r.next(); dcol, kdcol = dcol_r.next()
                qT, kqT = qT_r.next(); kT, kkT = kT_r.next()
                st.update(xcB=xcB, kxcB=kxcB, xcFo=xcFo, kxcFo=kxcFo, wcol=wcol, kwcol=kwcol, dcol=dcol, kdcol=kdcol, qT=qT, kqT=kqT, kT=kT, kkT=kkT)
                for h in range(4):
                    ps, kp = kb.psum()
                    for k in range(4):
                        kb.mm(ps[:, :], cdiag[:, h * 4 + k, :], xmF[:, h, 1 + k:1 + k + GT], k == 0, k == 3, ['cdiag', 'xmF'], [kp])
                    kb.act(xcB[:, h, :], ps[:, :], AF.Silu, [kp, 'convb'], [kxcB], bias=convb[:, h:h + 1])
                    kb.act(xcFo[:, h, :].rearrange("p (b t) -> p b t", t=128), ps[:, :].rearrange("p (b t) -> p b t", t=128)[:, 1::2, :],
                           AF.Silu, [kp, 'convb'], [kxcFo], bias=convb[:, h:h + 1])
                kb.copy('pool', xmF[:, :, 1:4], xmF[:, :, 1 + GT:4 + GT], ['xmF'], ['xmF'])
                kb.dma(gval[:], valid4[:, T0:T0 + GT], w=['gval'])
                kb.dma(gib[:], ibias4[:, T0:T0 + GT], w=['gib'])
                kb.tt('dve', gS[:], gS[:], gval[:], ALU.mult, ['gS', 'gval'], ['gS'])
                kb.scan(gNB[:], onesr[:], gS[:], cNB[:, 0:1], ALU.mult, ALU.add, ['onesr', 'gS', 'cNB'], ['gNB'])
                kb.tt('dve', gU[:], gU[:], gNB[:], ALU.add, ['gU', 'gNB'], ['gU'])
                kb.tt('dve', gU[:], gU[:], gib[:], ALU.add, ['gU', 'gib'], ['gU'])
                kb.scan(gG[:], onesr[:], gU[:], cG[:, 0:1], ALU.mult, ALU.max, ['onesr', 'gU', 'cG'], ['gG'])
                kb.copy('dve', Rall[:, 0:1], cG[:], ['cG'], ['Rall'])
                kb.copy('dve', Rall[:, 1:GB + 1], gG[:].rearrange("p (b t) -> p b t", t=128)[:, :, 127], ['gG'], ['Rall'])
                kb.copy('dve', cNB[:], gNB[:, GT - 1:GT], ['gNB'], ['cNB'])
                kb.copy('dve', cG[:], gG[:, GT - 1:GT], ['gG'], ['cG'])
                Rb = bc(Rall[:, 1:GB + 1].unsqueeze(2), [4, GB, 128])
                kb.tt('dve', v3(gW), v3(gU), Rb, ALU.subtract, ['gU', 'Rall'], ['gW'])
                kb.tt('dve', v3(gE), v3(gNB), Rb, ALU.subtract, ['gNB', 'Rall'], ['gE'])
                kb.tt('dve', dec4[:], Rall[:, 0:GB], Rall[:, 1:GB + 1], ALU.subtract, ['Rall'], ['dec4'])
                kb.act(dec4[:], dec4[:], AF.Exp, ['dec4'], ['dec4'])
                kb.act(gW[:], gW[:], AF.Exp, ['gW'], ['gW'])
                kb.act(gE[:], gE[:], AF.Exp, ['gE'], ['gE'])
                kb.tt('dve', Rm[:], bc(dec4[:].unsqueeze(1), [4, 4, GB]), dmask[:].rearrange("p (h b) -> p h b", b=GB), ALU.mult,
                      ['dec4', 'dmask'], ['Rm'])
                psg, kpg = kb.psum()
                for b_ in range(GB):
                    kb.tr(psg[:, b_ * 8:b_ * 8 + 4], gW[:, b_ * 128:(b_ + 1) * 128], identF[0:4, 0:4], ['gW', 'identF'], [kpg])
                    kb.tr(psg[:, b_ * 8 + 4:b_ * 8 + 8], gE[:, b_ * 128:(b_ + 1) * 128], identF[0:4, 0:4], ['gE', 'identF'], [kpg])
                kb.copy('dve', wcol[:].rearrange("p b e -> p (b e)"), psg[:, 0:GB * 8], [kpg], [kwcol])
                psd, kpd = kb.psum()
                kb.mm(psd[:, 0:4 * GB], ones4[:], Rm[:].rearrange("p h b -> p (h b)"), True, True, ['ones4', 'Rm'], [kpd])
                kb.copy('dve', dcol[:].rearrange("p h b -> p (h b)"), psd[:, 0:4 * GB], [kpd], [kdcol])
                xc_own = lambda h: xcB[:, h, :].rearrange("p (b t) -> p b t", t=128)[:, 1::2, :]
                for hp in range(2):
                    psq_, kpq = kb.psum()
                    psk_, kpk = kb.psum()
                    for hh in range(2):
                        h = hp * 2 + hh
                        kb.mm(psq_[:, hh * GO:(hh + 1) * GO], wmq[:, h, :], xc_own(h), True, True, ['wmq', kxcB], [kpq])
                        kb.mm(psk_[:, hh * GO:(hh + 1) * GO], wmk[:, h, :], xc_own(h), True, True, ['wmk', kxcB], [kpk])
                    kb.copy('act', qT[:, hp * 2:hp * 2 + 2, :].rearrange("p h t -> p (h t)"), psq_[:, 0:2 * GO], [kpq], [kqT])
                    kb.act(kT[:, hp * 2:hp * 2 + 2, :].rearrange("p h t -> p (h t)"), psk_[:, 0:2 * GO], AF.Copy, [kpk], [kkT], scale=float(128 ** -0.5))
                if g + 1 < NG:
                    GS[('tab', g + 1)] = tables(g + 1)

            def M_pre(g, j):
                st = GS[g]
                xmB, kxmB = st['xmB'], st['kxmB']
                xcB, kxcB = st['xcB'], st['kxcB']
                wcol, kwcol = st['wcol'], st['kwcol']
                qT, kqT, kT, kkT = st['qT'], st['kqT'], st['kT'], st['kkT']
                owned = (j % 2 == 1)
                ob = j // 2
                bsl = slice(j * 128, (j + 1) * 128)
                ktm, kkt = ktm_r.next(); wv, kwv = wv_r.next()
                psk_, kpk = kb.psum()
                psv_, kpv = kb.psum()
                for h in range(4):
                    kb.mm(psk_[:, h * 128:(h + 1) * 128], xcB[:, h, bsl], wmk[:, h, :], True, True, [kxcB, 'wmk'], [kpk])
                for h in range(4):
                    kb.mm(psv_[:, h * 128:(h + 1) * 128], xmB[:, h, bsl], wmv[:, h, :], True, True, [kxmB, 'wmv'], [kpv])
                kb.act(ktm[:].rearrange("p h d -> p (h d)"), psk_[:, :], AF.Copy, [kpk], [kkt], scale=float(128 ** -0.5))
                wsl = wcol[:, j, 0:4]
                kb.tt('dve', wv[:, :, 0:128], psv_[:, :].rearrange("p (h d) -> p h d", d=128), bc(wsl.unsqueeze(2), [128, 4, 128]), ALU.mult,
                      [kpv, kwcol], [kwv])
                kb.copy('pool', wv[:, :, 128:129], wsl.unsqueeze(2), [kwcol], [kwv])
                a0, ka0 = None, None
                if owned:
                    osl = slice(ob * 128, (ob + 1) * 128)
                    pss, kps = kb.psum()
                    for h in range(4):
                        kb.mm(pss[:, h * 128:(h + 1) * 128], kT[:, h, osl], qT[:, h, osl], True, True, [kkT, kqT], [kps])
                    a0, ka0 = a0_r.next()
                    kb.tt('dve', a0[:], pss[:, :].rearrange("p (h t) -> p h t", t=128), bc(trilT[:].unsqueeze(1), [128, 4, 128]), ALU.mult,
                          [kps, 'trilT'], [ka0])
                st.setdefault('pre', {})[j] = (ktm, kkt, wv, kwv, a0, ka0)

            def M_block(g, j):
                st = GS[g]
                xmB, kxmB, zsF, kzs = st['xmB'], st['kxmB'], st['zsF'], st['kzs']
                xcB, kxcB, xcFo, kxcFo = st['xcB'], st['kxcB'], st['xcFo'], st['kxcFo']
                wcol, kwcol, dcol, kdcol = st['wcol'], st['kwcol'], st['dcol'], st['kdcol']
                qT, kqT, kT, kkT = st['qT'], st['kqT'], st['kT'], st['kkT']
                owned = (j % 2 == 1)
                ob = j // 2
                bsl = slice(j * 128, (j + 1) * 128)
                ktm, kkt, wv, kwv, a0, ka0 = st['pre'].pop(j)
                dsl = dcol[:, :, j]
                kb.tt('dve', Cd32[:, :, 0:129], Cst[:, :, 0:129], bc(dsl.unsqueeze(2), [128, 4, 129]), ALU.mult, ['Cst', kdcol], ['Cd32'])
                if owned:
                    kb.copy('act', Cd16[:, :, 0:129], Cd32[:, :, 0:129], ['Cd32'], ['Cd16'])
                    osl = slice(ob * 128, (ob + 1) * 128)
                    pn = []
                    for hp in range(2):
                        psn, kpn = kb.psum()
                        for hh in range(2):
                            h = hp * 2 + hh
                            kb.mm(psn[:, hh * 256:hh * 256 + 129], a0[:, h, :], wv[:, h, 0:129], True, False, [ka0, kwv], [kpn])
                            kb.mm(psn[:, hh * 256:hh * 256 + 129], qT[:, h, osl], Cd16[:, h, 0:129], False, True, [kqT, 'Cd16'], [kpn])
                        pn.append((psn, kpn))
                for hp in range(2):
                    psu, kpu = kb.psum()
                    for hh in range(2):
                        h = hp * 2 + hh
                        kb.mm(psu[:, hh * 256:hh * 256 + 129], ktm[:, h, :], wv[:, h, 0:129], True, True, [kkt, kwv], [kpu])
                    kb.tt('dve', Cst[:, hp * 2:hp * 2 + 2, 0:129], Cd32[:, hp * 2:hp * 2 + 2, 0:129],
                          psu[:, :].rearrange("p (h c) -> p h c", c=256)[:, :, 0:129], ALU.add, ['Cd32', kpu], ['Cst'])
                if owned:
                    hN, khN = hN_r.next(); hc, khc = hc_r.next(); f1, kf1 = f1_r.next(); f2, kf2 = f2_r.next(); mlo, kmlo = mlo_r.next()
                    ecl = wcol[:, j, 4:8]
                    for hp in range(2):
                        psn, kpn = pn[hp]
                        pv3 = psn[:, :].rearrange("p (h c) -> p h c", c=256)
                        kb.stt(rden[:, hp * 2:hp * 2 + 2], pv3[:, :, 128], -1.0, ecl[:, hp * 2:hp * 2 + 2], ALU.mult, ALU.max, [kpn, kwcol], ['rden'])
                        kb.tt('dve', dcl[:, hp * 2:hp * 2 + 2], pv3[:, :, 128], rden[:, hp * 2:hp * 2 + 2], ALU.max, [kpn, 'rden'], ['dcl'])
                    kb.P.add('dve', lambda e: e.reciprocal(out=rden[:], in_=dcl[:]), ['dcl'], ['rden'])
                    for hp in range(2):
                        psn, kpn = pn[hp]
                        pv3 = psn[:, :].rearrange("p (h c) -> p h c", c=256)
                        kb.tt('dve', hN[:, hp * 2:hp * 2 + 2, :], pv3[:, :, 0:128], bc(rden[:, hp * 2:hp * 2 + 2].unsqueeze(2), [128, 2, 128]), ALU.mult,
                              [kpn, 'rden'], [khN])
                    for h in range(4):
                        kb.P.add('dve', (lambda h: lambda e: e.bn_stats(out=bst[:, h, :], in_=hN[:, h, :]))(h), [khN], ['bst'])
                    for h in range(4):
                        kb.P.add('dve', (lambda h: lambda e: e.bn_aggr(out=bmv[:, h, :], in_=bst[:, h, :]))(h), ['bst'], ['bmv'])
                    kb.ts('dve', rstd[:], bmv[:, :, 1], EPS, None, ALU.add, None, ['bmv'], ['rstd'])
                    kb.tt('pool', rstd[:], rstd[:], bc(mhalf[:], [128, 4]), ALU.pow, ['rstd', 'mhalf'], ['rstd'])
                    for h in range(4):
                        kb.ts('dve', hc[:, h, :], hN[:, h, :], bmv[:, h, 0:1], rstd[:, h:h + 1], ALU.subtract, ALU.mult, [khN, 'bmv', 'rstd'], [khc])
                    pst, kpt = kb.psum()
                    for h in range(4):
                        kb.tr(pst[:, h * 128:(h + 1) * 128], hc[:, h, :], identF[:], [khc, 'identF'], [kpt])
                    kb.tt('dve', f1[:], pst[:, :].rearrange("p (h t) -> p h t", t=128), bc(gmh[:].unsqueeze(2), [128, 4, 128]), ALU.mult,
                          [kpt, 'gmh'], [kf1])
                    kb.tt('pool', f2[:], xcFo[:, :, osl], bc(skipc[:].unsqueeze(2), [128, 4, 128]), ALU.mult, [kxcFo, 'skipc'], [kf2])
                    kb.tt('pool', f1[:], f1[:], f2[:], ALU.add, [kf1, kf2], [kf1])
                    kb.tt('pool', mlo[:], f1[:], zsF[:, :, osl], ALU.mult, [kf1, kzs], [kmlo])
                    gob = g * (GB // 2) + ob
                    kb.dma(mlo_s[:, :, gob * 128:(gob + 1) * 128].rearrange("h p t -> p h t"), mlo[:], r=[kmlo], w=[('mlo_s', gob)])
                    if dbg:
                        kb.dma(dbg_mlo[:, :, gob * 128:(gob + 1) * 128].rearrange("h p t -> p h t"), mlo[:], r=[kmlo], w=[('d_mlo', gob)])

            steps = [lambda: GS.__setitem__(('tab', 0), tables(0))]
            for part in range(4):
                steps.append((lambda part: lambda: P_part(0, part))(part))
            pq = [(g_, p_) for g_ in range(1, NG) for p_ in range(4)]
            for _ in range(2):
                if pq:
                    steps.append((lambda gp: lambda: P_part(*gp))(pq.pop(0)))
            for g in range(NG):
                steps.append((lambda g: lambda: M_pre(g, 0))(g))
                steps.append((lambda g: lambda: M_pre(g, 1))(g))
                steps.append((lambda g: lambda: M_pre(g, 2))(g))
                for j in range(GB):
                    steps.append((lambda g, j: lambda: M_block(g, j))(g, j))
                    if j + 3 < GB:
                        steps.append((lambda g, j: lambda: M_pre(g, j + 3))(g, j))
                    if pq:
                        steps.append((lambda gp: lambda: P_part(*gp))(pq.pop(0)))
            for i_, f_ in enumerate(steps):
                f_()
                if i_ % 3 == 2:
                    zero_fill(1)
            zero_fill(len(zf_todo))
        P.barrier()
        if stop_after == 'A':
            fin_r = [('mlo_s', gob) for gob in range(NOB)] + [('d_mlo', gob) for gob in range(NOB)] + [('ckv_s', g) for g in range(NG)] + \
                [('cq_s', g) for g in range(NG)] + [('kr_s', g) for g in range(NG)] + [('tab_s', g) for g in range(NG)]
            P.add('sp', None, reads=[k for k in fin_r if k in P.last_w])
            P.emit(top)
            return nc, D, dbgo

        attnTok = sb('attnTok', [128, NOB, 512], BF16)
        NQG = NOB // 4
        with ExitStack() as sbk:
            sbb = lambda name, shape, dt: kb.sb(sbk, name, shape, dt)
            stt2 = [sbk.enter_context(nc.psum_tensor('psS%d' % k, [128, 1024], F32)) for k in range(3)]
            pvt = [sbk.enter_context(nc.psum_tensor('psV%d' % k, [128, 512], F32)) for k in range(2)]
            wuq = sbb('wuq', [128, 2, 768], BF16); wukv = sbb('wukv', [128, 1024], BF16)
            kb.dma(wuq[:], w_uq_d.rearrange("(c p) n -> p c n", p=128), w=['wuq'], q='pool')
            kb.dma(wukv[:], w_ukv_d, w=['wukv'], q='pool')
            cosq = sbb('cosq', [96, OT], F32); sinq = sbb('sinq', [96, OT], F32)
            cqnT = sbb('cqnT', [128, 2, OT], BF16); ckvnT = sbb('ckvnT', [128, VT], BF16)
            KhT = sbb('KhT', [97, VT], BF16)
            allA = [('ckv_s', g_) for g_ in range(NG)] + [('cq_s', g_) for g_ in range(NG)] + [('kr_s', g_) for g_ in range(NG)] + [('tab_s', g_) for g_ in range(NG)]
            kb.dma(ckvnT[:], ckv_s, r=allA, w=['ckvnT'])
            kb.dma(cqnT[:], cq_s.rearrange("q p t -> p q t"), r=allA, w=['cqnT'])
            kb.dma(KhT[64:96, :], kr_s, r=allA, w=['KhT_r'])
            kb.dma(KhT[96:97, :], kbias, w=['KhT_b'], q='pool')
            kb.dma(cosq[64:96, :], tab_s[0], r=allA, w=['tabQ'])
            kb.dma(sinq[64:96, :], tab_s[1], r=allA, w=['tabQ'])
            kb.ts('dve', cosq[64:96, :], cosq[64:96, :], float(QSCALE), None, ALU.mult, None, ['tabQ'], ['tabQ'])
            kb.ts('dve', sinq[64:96, :], sinq[64:96, :], float(QSCALE), None, ALU.mult, None, ['tabQ'], ['tabQ'])
            Vh = sbb('Vh', [128, NVB, 65], BF16)
            QhT = sbb('QhT', [97, OT], BF16)
            kb.memset('pool', Vh[:, :, 64:65], 1.0, ['Vh1'])
            kb.memset('pool', QhT[96:97, :], 1.0, ['QhT1'])
            qraw = sbb('qraw', [96, 512], F32); qt1 = sbb('qt1', [96, 512], F32); qt2 = sbb('qt2', [96, 512], F32)
            pt_r = Ring(kb, sbk, 'pt', [128, 2, 512], BF16, 4)
            rs = sbb('rs', [128, 4], F32)
            KR = ['KhT_r', 'KhT_b']
            CKV = ['ckvnT']
            CQN = ['cqnT']
            sti = [0]

            def st_next():
                k = sti[0] % 3
                sti[0] += 1
                return stt2[k], ('psS', k)

            for h in range(8):
                for ch in range(VT // 512):
                    st_, kst = st_next()
                    kb.mm(st_[0:64, 0:512], wukv[:, h * 128:h * 128 + 64], ckvnT[:, ch * 512:(ch + 1) * 512], True, True, ['wukv'] + CKV, [kst])
                    kb.copy('act' if ch % 2 == 0 else 'dve', KhT[0:64, ch * 512:(ch + 1) * 512], st_[0:64, 0:512], [kst], ['KhT_n'])
                for v8 in range(NVB // 8):
                    st_, kst = st_next()
                    for bb in range(8):
                        blk = v8 * 8 + bb
                        kb.mm(st_[:, bb * 64:(bb + 1) * 64], ckvnT[:, blk * 128:(blk + 1) * 128], wukv[:, h * 128 + 64:h * 128 + 128], True, True,
                              ['wukv'] + CKV, [kst])
                    kb.copy('dve' if v8 % 2 == 0 else 'act', Vh[:, v8 * 8:(v8 + 1) * 8, 0:64], st_[:, 0:512].rearrange("p (b d) -> p b d", d=64), [kst], ['Vh'])
                for qc in range(OT // 512):
                    cs = slice(qc * 512, (qc + 1) * 512)
                    st_, kst = st_next()
                    for c in range(2):
                        kb.mm(st_[0:96, 0:512], wuq[:, c, h * 96:(h + 1) * 96], cqnT[:, c, cs], c == 0, c == 1, ['wuq'] + CQN, [kst])
                    kb.act(QhT[0:64, cs], st_[0:64, 0:512], AF.Copy, [kst], ['QhT'], scale=float(QSCALE))
                    kb.copy('act', qraw[64:96, :], st_[64:96, 0:512], [kst], ['qraw'])
                    st2_, kst2 = st_next()
                    kb.mm(st2_[0:96, 0:512], permB[64:96, :], qraw[64:96, :], True, True, ['permB', 'qraw'], [kst2])
                    kb.tt('pool', qt1[64:96, :], qraw[64:96, :], cosq[64:96, cs], ALU.mult, ['qraw', 'tabQ'], ['qt1'])
                    kb.tt('dve', qt2[64:96, :], st2_[64:96, 0:512], sinq[64:96, cs], ALU.mult, [kst2, 'tabQ'], ['qt2'])
                    kb.tt('pool', QhT[64:96, cs], qt1[64:96, :], qt2[64:96, :], ALU.add, ['qt1', 'qt2'], ['QhT'])
                KALL = ['KhT_n'] + KR
                batches = []
                for qg in range(NQG):
                    kbs = list(range(8 * qg + 8))
                    nfull = 8 * qg + 2
                    i = 0
                    while i < nfull:
                        n = min(2, nfull - i)
                        batches.append(dict(qg=qg, kbs=kbs[i:i + n], full=True, first=(i == 0), last=False))
                        i += n
                    for kb_ in range(nfull, 8 * qg + 8):
                        batches.append(dict(qg=qg, kbs=[kb_], full=False, first=False, last=(kb_ == 8 * qg + 7)))

                def lmin_of(qg, kb_):
                    return max(0, (kb_ - 8 * qg - 1 + 1) // 2)

                def emit_S(bt):
                    st_, kst = st_next()
                    bt['st'] = (st_, kst)
                    qg = bt['qg']
                    for i, kb_ in enumerate(bt['kbs']):
                        lm = lmin_of(qg, kb_)
                        kb.mm(st_[:, i * 512 + lm * 128:(i + 1) * 512], KhT[0:97, kb_ * 128:(kb_ + 1) * 128],
                              QhT[0:97, qg * 512 + lm * 128:(qg + 1) * 512], True, True, KALL + ['QhT', 'QhT1'], [kst])

                def emit_PV(bt):
                    st_, kst = bt['st']
                    qg = bt['qg']
                    pt, kpt = pt_r.next()
                    pvb, kpv = pvt[qg % 2], ('psV', qg % 2)
                    if bt['first']:
                        kb.memset('dve', pvb[:, :], 0.0, [kpv])
                    if bt['full']:
                        n = len(bt['kbs'])
                        kb.act(pt[:, 0:n, :].rearrange("p a b -> p (a b)"), st_[:, 0:n * 512], AF.Exp, [kst], [kpt])
                    else:
                        kb_ = bt['kbs'][0]
                        lm = lmin_of(qg, kb_)
                        kb.act(pt[:, 0, lm * 128:512], st_[:, lm * 128:512], AF.Exp, [kst], [kpt])
                    for i, kb_ in enumerate(bt['kbs']):
                        lm = lmin_of(qg, kb_)
                        if kb_ == 8 * qg + 2 * lm + 1:
                            kb.tt('pool', pt[:, i, lm * 128:(lm + 1) * 128], pt[:, i, lm * 128:(lm + 1) * 128], attm[:], ALU.mult, [kpt, 'attm'], [kpt])
                    for i, kb_ in enumerate(bt['kbs']):
                        lm = lmin_of(qg, kb_)
                        for l in range(lm, 4):
                            kb.mm(pvb[:, l * 128:l * 128 + 65], pt[:, i, l * 128:(l + 1) * 128], Vh[:, kb_, 0:65], False, False,
                                  [kpt, 'Vh', 'Vh1'], [kpv], skip=True)
                    if bt['last']:
                        pv3 = pvb[:, :].rearrange("p (l c) -> p l c", c=128)
                        kb.P.add('dve', lambda e: e.reciprocal(out=rs[:], in_=pv3[:, :, 64]), [kpv], ['rs'])
                        kb.tt('dve', attnTok[:, 4 * qg:4 * qg + 4, h * 64:(h + 1) * 64], pv3[:, :, 0:64], bc(rs[:].unsqueeze(2), [128, 4, 64]), ALU.mult,
                              [kpv, 'rs'], [('attnTok', qg)])

                for i, bt in enumerate(batches):
                    if i == 0:
                        emit_S(bt)
                        if len(batches) > 1:
                            emit_S(batches[1])
                    if i + 2 < len(batches):
                        emit_S(batches[i + 2])
                    emit_PV(bt)
            if dbg:
                o = dbg_out('d_attn', [128, NOB * 512], BF16)
                kb.dma(o, attnTok[:].rearrange("p a b -> p (a b)"), r=[('attnTok', qg) for qg in range(NQG)], w=['d_attn'])
        P.barrier()
        if stop_after == 'B':
            P.add('sp', None, reads=list(dbgo.keys()) if False else [k for k in dbgo.keys() if k != 'd_mlo'] + [('d_mlo', gob) for gob in range(NOB)])
            P.emit(top)
            return nc, D, dbgo

        iota32_d = D['iota32']
        route = sb('route', [128, NOB, 2], F32)
        desti = sb('desti', [128, NOB, 2], I32)
        xv_b = xv.rearrange("(n p) d -> n p d", p=128)

        def rms_rinv(stk, xin, kxin, junk, kjunk, pool_pow=False):
            kb.act(junk[:], xin[:], AF.Square, [kxin], [kjunk, stk[1]], accum=stk[0][:, 0:1])
            if pool_pow:
                kb.ts('dve', stk[0][:, 1:2], stk[0][:, 0:1], 1.0 / 1024, EPS, ALU.mult, ALU.add, [stk[1]], [stk[1]])
                kb.tt('pool', stk[0][:, 2:3], stk[0][:, 1:2], mhalf[:], ALU.pow, [stk[1], 'mhalf'], [stk[1]])
                return stk[0][:, 2:3]
            kb.act(stk[0][:, 1:2], stk[0][:, 0:1], AF.Ln, [stk[1], 'epsc'], [stk[1]], bias=epsc[:], scale=1.0 / 1024)
            kb.act(stk[0][:, 2:3], stk[0][:, 1:2], AF.Exp, [stk[1]], [stk[1]], scale=-0.5)
            return stk[0][:, 2:3]

        with ExitStack() as sc_:
            sbc = lambda name, shape, dt: kb.sb(sc_, name, shape, dt)
            kb.ps = [sc_.enter_context(nc.psum_tensor('psC%d' % k, [128, 512], F32)) for k in range(8)]
            wo = sbc('wo', [128, 8, 1024], BF16)
            kb.dma(wo[:], w_o_d.rearrange("(c p) n -> p c n", p=128), w=['wo'], q='pool')
            gffn = sbc('gffn', [128, 1024], F32); wr = sbc('wr', [128, 8, 36], F32); brt = sbc('brt', [128, 36], F32)
            ustrF = sbc('ustrF', [128, 128], F32); ustrB = sbc('ustrB', [128, 128], BF16); onesB = sbc('onesB', [128, 128], BF16)
            iota32 = sbc('iota32', [128, 32], F32)
            tot = sbc('tot', [128, 32], F32)
            kb.dma(gffn[:], gffn_d, w=['gffn']); kb.dma(wr[:], w_rt_d.rearrange("(c p) n -> p c n", p=128), w=['wr'])
            kb.dma(brt[:], b_rt_d, w=['brt']); kb.dma(ustrF[:], ustr_d, w=['ustrF']); kb.dma(iota32[:], iota32_d, w=['iota32'])
            kb.copy('dve', ustrB[:], ustrF[:], ['ustrF'], ['ustrB'])
            kb.memset('pool', onesB[:], 1.0, ['onesB'])
            kb.memset('pool', tot[:], 0.0, ['tot'])
            mix_r = Ring(kb, sc_, 'mixT', [128, 8, 128], BF16, 4)
            xt_r = Ring(kb, sc_, 'xtc', [128, 1024], F32, 4)
            x1_r = Ring(kb, sc_, 'x1t', [128, 1024], F32, 3)
            hn_r = Ring(kb, sc_, 'hnF', [128, 1024], F32, 2)
            hb_r = Ring(kb, sc_, 'hnb', [128, 1024], BF16, 10)
            jk_r = Ring(kb, sc_, 'jkc', [128, 1024], BF16, 2)
            hnT_r = Ring(kb, sc_, 'hnT', [128, 8, 128], F32, 2)
            st_r = Ring(kb, sc_, 'stc', [128, 4], F32, 4)
            L4 = sbc('L4', [128, 4, 36], F32); sm4 = sbc('sm4', [128, 4, 16], F32)
            oh4 = sbc('oh4', [128, 4, 4], F32); oh1 = sbc('oh1', [128, 4, 8], F32); oh2 = sbc('oh2', [128, 4, 8], F32)
            t48 = sbc('t48', [128, 4, 4, 8], F32); ein = sbc('ein', [128, 4, 8], F32); ein2 = sbc('ein2', [128, 4, 8], F32)
            OH1 = sbc('OH1', [128, 4, 4, 8], F32); OH2 = sbc('OH2', [128, 4, 4, 8], F32); cntB = sbc('cntB', [128, 4, 32], BF16)
            pre = sbc('pre', [128, 4, 32], F32); t32 = sbc('t32', [128, 4, 32], F32); e4 = sbc('e4', [128, 4, 4], F32)
            fl32 = lambda t: t[:].rearrange("p g j -> p (g j)")
            cst = {}

            def c_load(ob):
                vb = 2 * ob + 1
                mixT, kmix = mix_r.next(); xt, kx = xt_r.next()
                cst[ob] = dict(mixT=mixT, kmix=kmix, xt=xt, kx=kx)
                kb.dma(mixT[:, 4:8, :], mlo_s[:, :, ob * 128:(ob + 1) * 128].rearrange("h p t -> p h t"), r=[('mlo_s', ob)], w=[(kmix, 'ml')])
                kb.dma(xt[:], xv_b[vb], w=[kx])

            def c_part1(ob):
                d_ = cst[ob]
                mixT, kmix, xt, kx = d_['mixT'], d_['kmix'], d_['xt'], d_['kx']
                x1t, kx1 = x1_r.next()
                pst, kp = kb.psum()
                pstb = pst[:].bitcast(BF16)
                for c in range(4):
                    kb.tr(pstb[:, c * 128:(c + 1) * 128], attnTok[:, ob, c * 128:(c + 1) * 128], identB[:], [('attnTok', ob // 4), 'identB'], [kp])
                kb.copy('act', mixT[:, 0:4, :].rearrange("p c t -> p (c t)"), pstb[:, 0:512], [kp], [(kmix, 'at')])
                for half in range(2):
                    ps, kp = kb.psum()
                    for c in range(8):
                        kb.mm(ps[:, :], mixT[:, c, :], wo[:, c, half * 512:(half + 1) * 512], c == 0, c == 7, [(kmix, 'at'), (kmix, 'ml'), 'wo'], [kp])
                    kb.tt('dve', x1t[:, half * 512:(half + 1) * 512], ps[:, :], xt[:, half * 512:(half + 1) * 512], ALU.add, [kp, kx], [kx1])
                kb.dma(x1_s[ob * 128:(ob + 1) * 128, :], x1t[:], r=[kx1], w=[('x1_s', ob)])
                jk, kjk = jk_r.next(); stk = st_r.next()
                rinv = rms_rinv(stk, x1t, kx1, jk, kjk)
                d_.update(x1t=x1t, kx1=kx1, stk=stk, rinv=rinv)

            def c_part2(ob):
                d_ = cst[ob]
                x1t, kx1, stk, rinv = d_['x1t'], d_['kx1'], d_['stk'], d_['rinv']
                hnF, khn = hn_r.next(); hnb, khb = hb_r.next()
                d_.update(hnb=hnb, khb=khb)
                kb.stt(hnF[:], x1t[:], rinv, gffn[:], ALU.mult, ALU.mult, [kx1, stk[1], 'gffn'], [khn])
                kb.copy('act', hnb[:], hnF[:], [khn], [khb])
                hnT, khT_ = hnT_r.next()
                for q4 in range(2):
                    pst, kp = kb.psum()
                    for c in range(4):
                        kb.tr(pst[:, c * 128:(c + 1) * 128], hnF[:, (q4 * 4 + c) * 128:(q4 * 4 + c + 1) * 128], identF[:], [khn, 'identF'], [kp])
                    kb.copy('act' if q4 == 0 else 'dve', hnT[:, q4 * 4:q4 * 4 + 4, :].rearrange("p c t -> p (c t)"), pst[:, :], [kp], [khT_])
                psl, kpl = kb.psum()
                for c in range(8):
                    kb.mm(psl[:, 0:36], hnT[:, c, :], wr[:, c, :], c == 0, c == 7, [khT_, 'wr'], [kpl])
                kb.tt('dve', L4[:, ob % 4, :], psl[:, 0:36], brt[:], ALU.add, [kpl, 'brt'], [('L4', ob % 4)])

            def c_route(qd):
                LK = [('L4', i) for i in range(4)]
                A = lambda i: sm4[:, :, i]
                Ab = lambda i, n: bc(sm4[:, :, i:i + 1], [128, 4, n])
                red = lambda out, in_, op, r, w: kb.P.add('dve', lambda e: e.tensor_reduce(out=out, in_=in_, axis=AX.X, op=op), r, w)
                red(A(0), L4[:, :, 0:4], ALU.max, LK, ['s0'])
                kb.tt('dve', oh4[:], L4[:, :, 0:4], Ab(0, 4), ALU.is_equal, LK + ['s0'], ['oh4'])
                kb.tt('dve', e4[:], L4[:, :, 0:4], Ab(0, 4), ALU.subtract, LK + ['s0'], ['e4'])
                kb.act(e4[:], e4[:], AF.Exp, ['e4'], ['e4'])
                red(A(2), e4[:], ALU.add, ['e4'], ['s2'])
                kb.P.add('dve', lambda e: e.reciprocal(out=A(3), in_=A(2)), ['s2'], ['s3'])
                L48 = L4[:, :, 4:36].rearrange("p b (g j) -> p b g j", j=8)
                kb.tt('dve', t48[:], L48, bc(oh4[:].unsqueeze(3), [128, 4, 4, 8]), ALU.mult, LK + ['oh4'], ['t48'])
                red(ein[:], t48[:].rearrange("p b g j -> p b j g"), ALU.add, ['t48'], ['ein'])
                red(A(4), ein[:], ALU.max, ['ein'], ['s4'])
                kb.tt('dve', oh1[:], ein[:], Ab(4, 8), ALU.is_equal, ['ein', 's4'], ['oh1'])
                kb.stt(ein2[:], oh1[:], -1e30, ein[:], ALU.mult, ALU.add, ['oh1', 'ein'], ['ein2'])
                red(A(5), ein2[:], ALU.max, ['ein2'], ['s5'])
                kb.tt('dve', oh2[:], ein2[:], Ab(5, 8), ALU.is_equal, ['ein2', 's5'], ['oh2'])
                kb.tt('dve', A(6), A(5), A(4), ALU.subtract, ['s5', 's4'], ['s6'])
                kb.act(A(6), A(6), AF.Exp, ['s6'], ['s6'])
                kb.ts('dve', A(7), A(6), 1.0, None, ALU.add, None, ['s6'], ['s7'])
                kb.P.add('dve', lambda e: e.reciprocal(out=A(7), in_=A(7)), ['s7'], ['s7'])
                kb.tt('dve', A(8), A(7), A(6), ALU.mult, ['s7', 's6'], ['s8'])
                rk_ = [('route', qd * 4 + i) for i in range(4)]
                kb.tt('dve', route[:, qd * 4:qd * 4 + 4, 0], A(7), A(3), ALU.mult, ['s7', 's3'], rk_)
                kb.tt('dve', route[:, qd * 4:qd * 4 + 4, 1], A(8), A(3), ALU.mult, ['s8', 's3'], rk_)
                o4 = bc(oh4[:].unsqueeze(3), [128, 4, 4, 8])
                kb.tt('dve', OH1[:], o4, bc(oh1[:].unsqueeze(2), [128, 4, 4, 8]), ALU.mult, ['oh4', 'oh1'], ['OH1'])
                kb.tt('dve', OH2[:], o4, bc(oh2[:].unsqueeze(2), [128, 4, 4, 8]), ALU.mult, ['oh4', 'oh2'], ['OH2'])
                f3 = lambda t: t[:].rearrange("p b g j -> p b (g j)")
                kb.tt('dve', cntB[:], f3(OH1), f3(OH2), ALU.add, ['OH1', 'OH2'], ['cntB'])
                psp, kpp = kb.psum()
                for b_ in range(4):
                    kb.mm(psp[:, b_ * 32:(b_ + 1) * 32], ustrB[:], cntB[:, b_, :], True, b_ == 0, ['ustrB', 'cntB'], [kpp])
                    for b2 in range(b_):
                        kb.mm(psp[:, b_ * 32:(b_ + 1) * 32], onesB[:], cntB[:, b2, :], False, b2 == b_ - 1, ['onesB', 'cntB'], [kpp])
                kb.tt('dve', pre[:], psp[:, 0:128].rearrange("p (b e) -> p b e", e=32), bc(tot[:].unsqueeze(1), [128, 4, 32]), ALU.add, [kpp, 'tot'], ['pre'])
                pst2, kpt2 = kb.psum()
                for b_ in range(4):
                    kb.mm(pst2[:, 0:32], onesB[:], cntB[:, b_, :], b_ == 0, b_ == 3, ['onesB', 'cntB'], [kpt2])
                kb.tt('dve', tot[:], tot[:], pst2[:, 0:32], ALU.add, ['tot', kpt2], ['tot'])
                dk_ = [('desti', qd * 4 + i) for i in range(4)]
                for k2, OHk in ((0, OH1), (1, OH2)):
                    kb.tt('dve', t32[:], f3(OHk), pre[:], ALU.mult, ['OH1', 'OH2', 'pre'], ['t32'])
                    red(A(9), t32[:], ALU.add, ['t32'], ['s9'])
                    kb.tt('dve', t32[:], f3(OHk), bc(iota32[:].unsqueeze(1), [128, 4, 32]), ALU.mult, ['OH1', 'OH2', 'iota32'], ['t32'])
                    red(A(10), t32[:], ALU.add, ['t32'], ['s10'])
                    kb.stt(A(11), A(10), float(CAP), A(9), ALU.mult, ALU.add, ['s10', 's9'], ['s11'])
                    kb.ts('dve', A(11), A(11), float(NEXP * CAP - 1), None, ALU.min, None, ['s11'], ['s11'])
                    kb.copy('dve', desti[:, qd * 4:qd * 4 + 4, k2], A(11), ['s11'], dk_)

            def c_scatter(ob):
                d_ = cst.pop(ob)
                hnb, khb = d_['hnb'], d_['khb']
                for k2 in range(2):
                    kb.P.add('pool', (lambda ob, k2, hnb: lambda e: e.indirect_dma_start(
                        out=xs_s[:, :], out_offset=bass.IndirectOffsetOnAxis(ap=desti[:, ob, k2:k2 + 1], axis=0),
                        in_=hnb[:], in_offset=None))(ob, k2, hnb),
                        [khb, ('desti', ob)], ['xs_s'], dma=True)

            c_load(0)
            if NOB > 1:
                c_load(1)
            c_part1(0)
            pend_sc = []
            for ob in range(NOB):
                if ob + 2 < NOB:
                    c_load(ob + 2)
                if ob + 1 < NOB:
                    c_part1(ob + 1)
                c_part2(ob)
                if pend_sc:
                    c_scatter(pend_sc.pop(0))
                if ob % 4 == 3:
                    c_route(ob // 4)
                    pend_sc.extend(range(ob - 3, ob + 1))
            while pend_sc:
                c_scatter(pend_sc.pop(0))
            if dbg:
                o = dbg_out('d_route', [128, NOB * 2], F32)
                kb.dma(o, route[:].rearrange("p a b -> p (a b)"), r=[('route', ob) for ob in range(NOB)], w=['d_route'])
                o = dbg_out('d_desti', [128, NOB * 2], I32)
                kb.dma(o, desti[:].rearrange("p a b -> p (a b)"), r=[('desti', ob) for ob in range(NOB)], w=['d_desti'])
                o = dbg_out('d_x1', [OT, 1024], F32)
                kb.dma(o, x1_s, r=[('x1_s', ob) for ob in range(NOB)], w=['d_x1'])
        P.barrier()
        if stop_after == 'C':
            P.add('sp', None, reads=[k for k in dbgo.keys() if k != 'd_mlo'] + [('d_mlo', gob) for gob in range(NOB)] + ['xs_s'])
            P.emit(top)
            return nc, D, dbgo

        NBLK = CAP // 128
        with ExitStack() as sd_:
            sbd = lambda name, shape, dt: kb.sb(sd_, name, shape, dt)
            kb.ps = [sd_.enter_context(nc.psum_tensor('psD%d' % k, [128, 512], F32)) for k in range(8)]
            wg_r = Ring(kb, sd_, 'wg', [128, 8, 256], BF16, 3); wu_r = Ring(kb, sd_, 'wu', [128, 8, 256], BF16, 3)
            wd_r = Ring(kb, sd_, 'wd', [128, 2, 1024], BF16, 3)
            xsb_r = Ring(kb, sd_, 'xsb', [128, 1024], BF16, 3 * NBLK)
            xsT_r = Ring(kb, sd_, 'xsT', [128, 8, CAP], BF16, 2)
            sg_r = Ring(kb, sd_, 'sg', [128, CAP], F32, 2)
            aT_r = Ring(kb, sd_, 'actT', [128, 2, CAP], BF16, 2)
            yb_r = Ring(kb, sd_, 'yb', [128, 1024], F32, 3)
            dst = {}

            def d_load(e_):
                wg, kwg = wg_r.next(); wu, kwu = wu_r.next(); wd, kwd = wd_r.next()
                kb.dma(wg[:], w_ge_d[e_].rearrange("(c p) f -> p c f", p=128), w=[kwg], q='pool')
                kb.dma(wu[:], w_ue_d[e_].rearrange("(c p) f -> p c f", p=128), w=[kwu], q='pool')
                kb.dma(wd[:], w_de_d[e_].rearrange("(c p) n -> p c n", p=128), w=[kwd], q='pool')
                xs_l = []
                for blk in range(NBLK):
                    xsb, kxb = xsb_r.next()
                    r0 = e_ * CAP + blk * 128
                    kb.dma(xsb[:], xs_s[r0:r0 + 128, :], r=['xs_s'], w=[kxb])
                    xs_l.append((xsb, kxb))
                dst[e_] = (wg, kwg, wu, kwu, wd, kwd, xs_l)

            dst2 = {}

            def d_tr(e_):
                wg, kwg, wu, kwu, wd, kwd, xs_l = dst.pop(e_)
                xsT, kxT = xsT_r.next()
                for blk in range(NBLK):
                    xsb, kxb = xs_l[blk]
                    pst, kp = kb.psum()
                    pstb = pst[:].bitcast(BF16)
                    for c in range(8):
                        kb.tr(pstb[:, c * 128:(c + 1) * 128], xsb[:, c * 128:(c + 1) * 128], identB[:], [kxb, 'identB'], [kp])
                    kb.copy('dve' if blk % 2 == 0 else 'act', xsT[:, :, blk * 128:(blk + 1) * 128], pstb.rearrange("p (c t) -> p c t", c=8), [kp], [kxT])
                dst2[e_] = (wg, kwg, wu, kwu, wd, kwd, xsT, kxT)

            def d_gateup(e_):
                wg, kwg, wu, kwu, wd, kwd, xsT, kxT = dst2[e_]
                aT, kaT = aT_r.next()
                dst2[e_] = (wd, kwd, aT, kaT)
                for ft in range(2):
                    psg_, kpg_ = kb.psum(); psu_, kpu_ = kb.psum()
                    for c in range(8):
                        kb.mm(psg_[:, 0:CAP], wg[:, c, ft * 128:(ft + 1) * 128], xsT[:, c, :], c == 0, c == 7, [kwg, kxT], [kpg_])
                    for c in range(8):
                        kb.mm(psu_[:, 0:CAP], wu[:, c, ft * 128:(ft + 1) * 128], xsT[:, c, :], c == 0, c == 7, [kwu, kxT], [kpu_])
                    sg, ksg = sg_r.next()
                    kb.act(sg[:], psg_[:, 0:CAP], AF.Silu, [kpg_], [ksg])
                    kb.tt('dve', aT[:, ft, :], sg[:], psu_[:, 0:CAP], ALU.mult, [ksg, kpu_], [kaT])

            def d_down(e_):
                wd, kwd, aT, kaT = dst2.pop(e_)
                for blk in range(NBLK):
                    yb, kyb = yb_r.next()
                    for half in range(2):
                        psy, kpy = kb.psum()
                        for ft in range(2):
                            kb.mm(psy[:, :], aT[:, ft, blk * 128:(blk + 1) * 128], wd[:, ft, half * 512:(half + 1) * 512], ft == 0, ft == 1, [kaT, kwd], [kpy])
                        kb.copy('act' if half == 0 else 'dve', yb[:, half * 512:(half + 1) * 512], psy[:, :], [kpy], [kyb])
                    r0 = e_ * CAP + blk * 128
                    kb.dma(ys_s[r0:r0 + 128, :], yb[:], r=[kyb], w=['ys_s'])

            d_load(0)
            d_load(1)
            d_tr(0)
            for e_ in range(NEXP):
                if e_ + 2 < NEXP:
                    d_load(e_ + 2)
                d_gateup(e_)
                if e_ + 1 < NEXP:
                    d_tr(e_ + 1)
                d_down(e_)
        P.barrier()

        with ExitStack() as se_:
            sbe = lambda name, shape, dt: kb.sb(se_, name, shape, dt)
            kb.ps = [se_.enter_context(nc.psum_tensor('psE%d' % k, [128, 512], F32)) for k in range(8)]
            wpg = sbe('wpg', [128, 8, 1024], BF16); wple = sbe('wple', [128, 2, 1024], BF16)
            kb.dma(wpg[:], w_pg_d.rearrange("(c p) n -> p c n", p=128), w=['wpg'], q='pool')
            kb.dma(wple[:], w_ple_d.rearrange("(c p) n -> p c n", p=128), w=['wple'], q='pool')
            gple = sbe('gple', [128, 1024], F32); gfin = sbe('gfin', [128, 1024], F32)
            kb.dma(gple[:], gple_d, w=['gple']); kb.dma(gfin[:], gfin_d, w=['gfin'])
            x1_r = Ring(kb, se_, 'x1e', [128, 1024], F32, 4)
            y_r = Ring(kb, se_, 'ye', [128, 1024], F32, 8)
            x2_r = Ring(kb, se_, 'x2e', [128, 1024], F32, 3)
            hp_r = Ring(kb, se_, 'hpe', [128, 1024], BF16, 3)
            hpT_r = Ring(kb, se_, 'hpT', [128, 8, 128], BF16, 3)
            jk_r = Ring(kb, se_, 'jke', [128, 1024], BF16, 2)
            st_r = Ring(kb, se_, 'ste', [128, 4], F32, 4)
            sgm_r = Ring(kb, se_, 'sgm', [128, 1024], F32, 2)
            pt_r2 = Ring(kb, se_, 'pte', [128, 256], F32, 4)
            pb_r = Ring(kb, se_, 'pbe', [128, 256], BF16, 2)
            pT_r = Ring(kb, se_, 'pTe', [128, 2, 128], BF16, 3)
            x3_r = Ring(kb, se_, 'x3e', [128, 1024], F32, 2)
            o_r = Ring(kb, se_, 'oe', [128, 1024], F32, 2)
            est = {}

            def e_load(ob):
                x1t, kx1 = x1_r.next(); y1, ky1 = y_r.next(); y2, ky2 = y_r.next(); ptile, kpt_ = pt_r2.next()
                kb.dma(x1t[:], x1_s[ob * 128:(ob + 1) * 128, :], r=[('x1_s', ob)], w=[kx1])
                for k2, (yt, kyt) in enumerate(((y1, ky1), (y2, ky2))):
                    kb.P.add('pool', (lambda ob, k2, yt: lambda e: e.indirect_dma_start(
                        out=yt[:], out_offset=None, in_=ys_s[:, :],
                        in_offset=bass.IndirectOffsetOnAxis(ap=desti[:, ob, k2:k2 + 1], axis=0)))(ob, k2, yt), ['ys_s', ('desti', ob)], [kyt], dma=True)
                kb.dma(ptile[:], pvd[ob * 128:(ob + 1) * 128, :], w=[kpt_])
                est[ob] = (x1t, kx1, y1, ky1, y2, ky2, ptile, kpt_)

            def e_compute(ob):
                x1t, kx1, y1, ky1, y2, ky2, ptile, kpt_ = est.pop(ob)
                x2, kx2 = x2_r.next()
                kb.stt(x2[:], y1[:], route[:, ob, 0:1], x1t[:], ALU.mult, ALU.add, [ky1, ('route', ob), kx1], [kx2])
                kb.stt(x2[:], y2[:], route[:, ob, 1:2], x2[:], ALU.mult, ALU.add, [ky2, ('route', ob), kx2], [kx2])
                jk, kjk = jk_r.next(); stk = st_r.next()
                rinv = rms_rinv(stk, x2, kx2, jk, kjk, pool_pow=True)
                hp, khp = hp_r.next()
                kb.stt(hp[:], x2[:], rinv, gple[:], ALU.mult, ALU.mult, [kx2, stk[1], 'gple'], [khp])
                est1[ob] = (x2, kx2, hp, khp, ptile, kpt_)

            def e_compute1b(ob):
                x2, kx2, hp, khp, ptile, kpt_ = est1.pop(ob)
                hpT, khpT = hpT_r.next()
                pst, kp = kb.psum()
                pstb = pst[:].bitcast(BF16)
                for c in range(8):
                    kb.tr(pstb[:, c * 128:(c + 1) * 128], hp[:, c * 128:(c + 1) * 128], identB[:], [khp, 'identB'], [kp])
                kb.copy('act', hpT[:].rearrange("p c t -> p (c t)"), pstb, [kp], [khpT])
                pbt, kpb = pb_r.next(); pT, kpT = pT_r.next()
                kb.copy('act', pbt[:], ptile[:], [kpt_], [kpb])
                pst, kp = kb.psum()
                pstb = pst[:].bitcast(BF16)
                for c in range(2):
                    kb.tr(pstb[:, c * 128:(c + 1) * 128], pbt[:, c * 128:(c + 1) * 128], identB[:], [kpb, 'identB'], [kp])
                kb.copy('dve', pT[:].rearrange("p c t -> p (c t)"), pstb[:, 0:256], [kp], [kpT])
                est2[ob] = (x2, kx2, hpT, khpT, pT, kpT)

            def e_compute2(ob):
                x2, kx2, hpT, khpT, pT, kpT = est2.pop(ob)
                sgm, ksg = sgm_r.next(); x3, kx3 = x3_r.next()
                for half in range(2):
                    hs = slice(half * 512, (half + 1) * 512)
                    psg_, kpg_ = kb.psum()
                    for c in range(8):
                        kb.mm(psg_[:, :], hpT[:, c, :], wpg[:, c, hs], c == 0, c == 7, [khpT, 'wpg'], [kpg_])
                    kb.act(sgm[:, hs], psg_[:, :], AF.Sigmoid, [kpg_], [ksg])
                    psp, kpp = kb.psum()
                    for c in range(2):
                        kb.mm(psp[:, :], pT[:, c, :], wple[:, c, hs], c == 0, c == 1, [kpT, 'wple'], [kpp])
                    kb.tt('dve', x3[:, hs], psp[:, :], sgm[:, hs], ALU.mult, [kpp, ksg], [kx3])
                kb.tt('dve', x3[:], x3[:], x2[:], ALU.add, [kx3, kx2], [kx3])
                jk, kjk = jk_r.next(); stk = st_r.next()
                rinv = rms_rinv(stk, x3, kx3, jk, kjk, pool_pow=True)
                ot, kot = o_r.next()
                kb.stt(ot[:], x3[:], rinv, gfin[:], ALU.mult, ALU.mult, [kx3, stk[1], 'gfin'], [kot])
                kb.dma(out_d[ob * 128:(ob + 1) * 128, :], ot[:], r=[kot], w=['out'])

            est2 = {}
            est1 = {}
            e_load(0)
            if NOB > 1:
                e_load(1)
            e_compute(0)
            e_compute1b(0)
            for ob in range(NOB):
                if ob + 2 < NOB:
                    e_load(ob + 2)
                if ob + 1 < NOB:
                    e_compute(ob + 1)
                e_compute2(ob)
                if ob + 1 < NOB:
                    e_compute1b(ob + 1)

        P.add('sp', None, reads=['out'] + [k for k in dbgo.keys() if k != 'd_mlo'] + ([('d_mlo', gob) for gob in range(NOB)] if dbg else []))
        P.emit(top)
    return nc, D, dbgo


def _consts():
    c = {}
    c['ident'] = np.eye(128, dtype=np.float32)
    i = np.arange(128)
    c['trilT'] = (i[:, None] <= i[None, :]).astype(np.float32)
    c['attmask'] = ((i[:, None] // 64) <= (i[None, :] // 64)).astype(np.float32)
    pb = np.zeros((96, 96), np.float32)
    for j in range(16):
        pb[64 + j + 16, 64 + j] = -1.0
        pb[64 + j, 64 + j + 16] = 1.0
    c['permB'] = pb
    inv = (np.float32(1.0) / (np.float32(10000.0) ** (np.arange(0, 32, 2, dtype=np.float32) / np.float32(32)))).astype(np.float32)
    f = np.zeros((96, 1), np.float32)
    f[64:80, 0] = inv
    f[80:96, 0] = inv
    c['invfreq'] = f
    dm = np.zeros((4, 4, GB), np.float32)
    for k in range(4):
        dm[k, k, :] = 1.0
    c['dmask'] = dm.reshape(4, 4 * GB)
    c['ustrict'] = (i[:, None] < i[None, :]).astype(np.float32)
    c['iota32'] = np.broadcast_to(np.arange(32, dtype=np.float32), (128, 32)).copy()
    return c


def _prep_weights(I):
    f = lambda k: np.asarray(I[k], dtype=np.float32)
    w = {}
    w_in = f('w_in')[0]
    offs = np.cumsum([0, 256, 128, 32, 512, 512, 4, 4])
    cq, ckv, kr, xm, z, gi, gf = [w_in[:, offs[i]:offs[i + 1]] for i in range(7)]
    w['w_in_r'] = np.ascontiguousarray(np.concatenate(
        [ckv, xm, np.zeros((1024, 64), np.float32), kr, gi, gf, cq, z], axis=1))
    assert w['w_in_r'].shape[1] == NCOL
    w['gmix_col'] = np.ascontiguousarray(f('norm_mix_g')[0].reshape(8, 128).T)
    w['gq_col'] = np.ascontiguousarray(f('q_norm_g')[0].reshape(2, 128).T)
    w['gkv_col'] = np.ascontiguousarray(f('kv_norm_g')[0].reshape(128, 1))
    w['w_uq'] = f('w_uq')[0]
    w['w_ukv'] = f('w_ukv')[0]
    cw = f('conv_w')[0]
    w['convw_col'] = np.ascontiguousarray(cw.reshape(4, 4, 128).transpose(2, 1, 0).reshape(128, 16))
    w['convb_col'] = np.ascontiguousarray(f('conv_b')[0].reshape(4, 128).T)
    w['w_mq'] = f('w_mq')[0]; w['w_mk'] = f('w_mk')[0]; w['w_mv'] = f('w_mv')[0]
    w['bi_col'] = f('b_igate')[0].reshape(4, 1); w['bf_col'] = f('b_fgate')[0].reshape(4, 1)
    w['gmh_col'] = np.ascontiguousarray(f('mh_norm_g')[0].reshape(4, 128).T)
    w['skip_col'] = np.ascontiguousarray(f('ml_skip')[0].reshape(4, 128).T)
    w['w_o'] = f('w_o')[0]
    rep = lambda v: np.ascontiguousarray(np.broadcast_to(v.reshape(1, -1), (128, v.size)))
    w['gffn_rep'] = rep(f('norm_ffn_g')[0])
    w['w_router'] = np.ascontiguousarray(np.concatenate([f('w_router_group')[0], f('w_router_expert')[0]], axis=1))
    w['b_router_rep'] = rep(np.concatenate([f('b_router_group')[0], f('b_router_expert')[0]]))
    w['w_gate_e'] = f('w_gate_e')[0]; w['w_up_e'] = f('w_up_e')[0]; w['w_down_e'] = f('w_down_e')[0]
    w['gple_rep'] = rep(f('norm_ple_g')[0])
    w['w_ple'] = f('w_ple')[0]; w['w_ple_gate'] = f('w_ple_gate')[0]
    w['gfin_rep'] = rep(f('final_norm_g'))
    return w


def _prep_core(I, b, r, NVB):
    S = NVB * 128
    x = np.asarray(I['x'], dtype=np.float32)[b]
    pos = np.asarray(I['positions'])[b].astype(np.int32)
    p = np.asarray(I['p'], dtype=np.float32)[0, b]
    m = {}
    if r == 1:
        xv = x[:S]; posv = pos[:S]; valid = np.ones(S, np.float32); first_real = 0
    else:
        xv = np.concatenate([np.zeros((128, 1024), np.float32), x[:S - 128]], 0)
        posv = np.concatenate([np.zeros(128, np.int32), pos[:S - 128]])
        valid = np.concatenate([np.zeros(128, np.float32), np.ones(S - 128, np.float32)])
    own = np.zeros(S, bool)
    for vb in range(1, NVB, 2):
        own[vb * 128:(vb + 1) * 128] = True
    real_idx = np.arange(S) - (0 if r == 1 else 128)
    m['xv'] = np.ascontiguousarray(xv)
    m['posk'] = np.ascontiguousarray(np.broadcast_to(posv[None, :], (32, S)))
    m['posq'] = np.ascontiguousarray(np.broadcast_to(posv[own][None, :], (32, S // 2)))
    m['pv'] = np.ascontiguousarray(p[real_idx[own]])
    m['valid4'] = np.ascontiguousarray(np.broadcast_to(valid[None, :], (4, S)))
    m['ibias4'] = np.ascontiguousarray(np.broadcast_to(((valid - 1.0) * 1e4)[None, :], (4, S))).astype(np.float32)
    m['kbias'] = ((valid - 1.0) * 3e4).reshape(1, S).astype(np.float32)
    return m, real_idx[own]


_CACHE = {}


def kernel(**inputs):
    NVB = 64
    if 'nc' not in _CACHE:
        _CACHE['nc'] = build(NVB)
    nc, D, _ = _CACHE['nc']
    consts = _consts()
    w = _prep_weights(inputs)
    in_maps = []
    idx = []
    for core in range(8):
        b, r = core // 2, core % 2
        m, ridx = _prep_core(inputs, b, r, NVB)
        m.update(consts)
        m.update(w)
        in_maps.append({k: v for k, v in m.items() if k in D})
        idx.append((b, ridx))
    res = run_bass_kernel_spmd(nc, in_maps, core_ids=list(range(8)))
    out = np.zeros((4, 8192, 1024), np.float32)
    for core in range(8):
        b, ridx = idx[core]
        out[b, ridx] = np.asarray(res.results[core]['out'], dtype=np.float32)
    return out
```

```python
import numpy as np
from contextlib import ExitStack
import concourse.bass as bass
import concourse.mybir as mybir
from concourse.bass_utils import run_bass_kernel_spmd

F32 = mybir.dt.float32
BF16 = mybir.dt.bfloat16
I32 = mybir.dt.int32
ALU = mybir.AluOpType
AF = mybir.ActivationFunctionType
AX = mybir.AxisListType

NDS = 12
EPS = 1e-6
CAP = 384
NEXP = 32
GB = 4
C_CKV, C_XM, C_MISC, C_GI, C_GF, C_CQ, C_Z, NCOL = 0, 128, 640, 736, 740, 744, 1000, 1512
QSCALE = 1.0 / np.sqrt(96.0)


class Prog:
    def __init__(self, nc):
        self.nc = nc
        self.ops = []
        self.last_w = {}
        self.readers = {}
        self.dma_count = {}
        self.dma_ops = {}

    def add(self, eng, fn, reads=(), writes=(), dma=False):
        i = len(self.ops)
        deps = set()
        ex = [r for r in reads if isinstance(r, tuple) and isinstance(r[0], str) and r[0].startswith('ps')]
        if ex:
            reads = [r for r in reads if r not in ex]
            writes = list(writes) + ex
        for r in reads:
            if r in self.last_w:
                deps.add(self.last_w[r])
        for w in writes:
            if w in self.last_w:
                deps.add(self.last_w[w])
            deps.update(self.readers.get(w, ()))
        for r in reads:
            self.readers.setdefault(r, []).append(i)
        for w in writes:
            self.last_w[w] = i
            self.readers[w] = []
        deps.discard(i)
        op = dict(eng=eng, fn=fn, deps=deps, dma=dma, sig=None)
        if dma:
            lst = self.dma_ops.setdefault(eng, [])
            op['dk'] = len(lst)
            if len(lst) >= NDS:
                deps.add(lst[len(lst) - NDS])
            lst.append(i)
        self.ops.append(op)
        return i

    def barrier(self):
        engs = ['pe', 'act', 'dve', 'pool', 'sp']
        last = []
        for e in engs:
            for j in range(len(self.ops) - 1, -1, -1):
                o = self.ops[j]
                if o['eng'] == e and not o['dma'] and o['fn'] is not None:
                    last.append(j)
                    break
        for e, lst in self.dma_ops.items():
            last.extend(lst[-NDS:])
        for e in engs:
            i = len(self.ops)
            self.ops.append(dict(eng=e, fn=None, deps=set(last), dma=False, sig=None))

    def emit(self, stack):
        nc = self.nc
        ops = self.ops
        engs = ['pe', 'act', 'dve', 'pool', 'sp']
        csem = {e: stack.enter_context(nc.semaphore('c_' + e)) for e in engs}
        dsem = {e: [stack.enter_context(nc.semaphore('d_%s_%d' % (e, k))) for k in range(NDS)]
                for e in self.dma_ops}
        need = [False] * len(ops)
        for o in ops:
            for d in o['deps']:
                p = ops[d]
                if (not p['dma']) and (not o['dma']) and p['eng'] == 'pe' and o['eng'] == 'pe' and o['fn'] is not None:
                    continue
                need[d] = True
        cnt = {e: 0 for e in engs}
        for i, o in enumerate(ops):
            if o['dma']:
                k = o['dk']
                o['sig'] = (dsem[o['eng']][k % NDS], 16 * (k // NDS + 1), 16)
            elif need[i] and o['fn'] is not None:
                cnt[o['eng']] += 1
                o['sig'] = (csem[o['eng']], cnt[o['eng']], 1)
        per = {e: [o for o in ops if o['eng'] == e] for e in engs}

        def run(ename, eobj):
            waited = {}
            for o in per[ename]:
                for d in sorted(o['deps']):
                    p = ops[d]
                    if p['sig'] is None:
                        continue
                    if (not p['dma']) and (not o['dma']) and p['eng'] == 'pe' and ename == 'pe' and o['fn'] is not None:
                        continue
                    sem, val, _ = p['sig']
                    if waited.get(sem.name, 0) < val:
                        eobj.wait_ge(sem, val)
                        waited[sem.name] = val
                if o['fn'] is None:
                    continue
                ins = o['fn'](eobj)
                if o['sig'] is not None:
                    sem, val, inc = o['sig']
                    ins.then_inc(sem, inc)

        with nc.Block() as block:
            @block.tensor
            def _(e):
                run('pe', e)

            @block.scalar
            def _(e):
                run('act', e)

            @block.vector
            def _(e):
                run('dve', e)

            @block.gpsimd
            def _(e):
                run('pool', e)

            @block.sync
            def _(e):
                run('sp', e)


class Ring:
    def __init__(self, kb, stack, name, shape, dtype, n):
        self.t = [stack.enter_context(kb.nc.sbuf_tensor('s_%s_%d' % (name, k), shape, dtype)) for k in range(n)]
        self.name = name
        self.i = 0

    def next(self):
        k = self.i % len(self.t)
        self.i += 1
        return self.t[k], (self.name, k)


class KB:
    def __init__(self, nc):
        self.nc = nc
        self.P = Prog(nc)
        self.psi = 0
        self.ps = None

    def sb(self, st, name, shape, dt):
        return st.enter_context(self.nc.sbuf_tensor('s_' + name, shape, dt))

    def psum(self):
        k = self.psi % 8
        self.psi += 1
        return self.ps[k], ('ps', k)

    def dma(self, out, in_, r=(), w=(), q='sp'):
        self.P.add(q, lambda e: e.dma_start(out=out, in_=in_), r, w, dma=True)

    def act(self, out, in_, func, r, w, bias=None, scale=None, accum=None):
        kw = {}
        if bias is not None:
            kw['bias'] = bias
        if scale is not None:
            kw['scale'] = scale
        if accum is not None:
            kw['accum_out'] = accum
        self.P.add('act', lambda e: e.activation(out=out, in_=in_, func=func, **kw), r, w)

    def tt(self, eng, out, in0, in1, op, r, w):
        self.P.add(eng, lambda e: e.tensor_tensor(out=out, in0=in0, in1=in1, op=op), r, w)

    def ts(self, eng, out, in0, s1, s2, op0, op1, r, w):
        if s2 is None:
            self.P.add(eng, lambda e: e.tensor_scalar(out=out, in0=in0, scalar1=s1, scalar2=None, op0=op0), r, w)
        else:
            self.P.add(eng, lambda e: e.tensor_scalar(out=out, in0=in0, scalar1=s1, scalar2=s2, op0=op0, op1=op1), r, w)

    def stt(self, out, in0, scalar, in1, op0, op1, r, w):
        self.P.add('dve', lambda e: e.scalar_tensor_tensor(out=out, in0=in0, scalar=scalar, in1=in1, op0=op0, op1=op1), r, w)

    def copy(self, eng, out, in_, r, w):
        if eng == 'act':
            self.P.add('act', lambda e: e.copy(out=out, in_=in_), r, w)
        else:
            self.P.add(eng, lambda e: e.tensor_copy(out=out, in_=in_), r, w)

    def memset(self, eng, ap, val, w):
        self.P.add(eng, lambda e: e.memset(ap, val), (), w)

    def mm(self, out, lhsT, rhs, start, stop, r, w, skip=False):
        if skip:
            self.P.add('pe', lambda e: e.matmul(out, lhsT, rhs, start=start, stop=stop, skip_group_check=True), r, w)
        else:
            self.P.add('pe', lambda e: e.matmul(out, lhsT, rhs, start=start, stop=stop), r, w)

    def tr(self, out, in_, ident, r, w):
        self.P.add('pe', lambda e: e.transpose(out=out, in_=in_, identity=ident), r, w)

    def scan(self, out, d0, d1, init, op0, op1, r, w):
        self.P.add('dve', lambda e: e.tensor_tensor_scan(out=out, data0=d0, data1=d1, initial=init, op0=op0, op1=op1), r, w)


def bc(ap, shape):
    return ap.broadcast_to(shape)


def build(NVB, stop_after=None, dbg=False):
    VT = NVB * 128
    NOB = NVB // 2
    OT = NOB * 128
    NG = NVB // GB
    GT = GB * 128
    GO = GT // 2
    nc = bass.Bass("TRN2", target_bir_lowering=False)
    D = {}

    def din(name, shape, dt=F32):
        D[name] = nc.dram_tensor(name, list(shape), dt, kind="ExternalInput").ap()
        return D[name]

    def dscr(name, shape, dt):
        D[name] = nc.dram_tensor(name, list(shape), dt, kind="Internal").ap()
        return D[name]

    def dout(name, shape, dt=F32):
        D[name] = nc.dram_tensor(name, list(shape), dt, kind="ExternalOutput").ap()
        return D[name]

    xv = din('xv', [VT, 1024]); posk = din('posk', [32, VT], I32); posq = din('posq', [32, OT], I32)
    pvd = din('pv', [OT, 256])
    valid4 = din('valid4', [4, VT]); ibias4 = din('ibias4', [4, VT]); kbias = din('kbias', [1, VT])
    ident_d = din('ident', [128, 128]); tril_d = din('trilT', [128, 128]); attm_d = din('attmask', [128, 128])
    perm_d = din('permB', [96, 96]); invf_d = din('invfreq', [96, 1]); dmask_d = din('dmask', [4, 4 * GB])
    ustr_d = din('ustrict', [128, 128]); din('iota32', [128, 32])
    w_in_d = din('w_in_r', [1024, NCOL]); gmix_d = din('gmix_col', [128, 8])
    gq_d = din('gq_col', [128, 2]); gkv_d = din('gkv_col', [128, 1])
    w_uq_d = din('w_uq', [256, 768]); w_ukv_d = din('w_ukv', [128, 1024])
    convw_d = din('convw_col', [128, 16]); convb_d = din('convb_col', [128, 4])
    w_mq_d = din('w_mq', [4, 128, 128]); w_mk_d = din('w_mk', [4, 128, 128]); w_mv_d = din('w_mv', [4, 128, 128])
    bi_d = din('bi_col', [4, 1]); bf_d = din('bf_col', [4, 1])
    gmh_d = din('gmh_col', [128, 4]); skip_d = din('skip_col', [128, 4])
    w_o_d = din('w_o', [1024, 1024]); gffn_d = din('gffn_rep', [128, 1024])
    w_rt_d = din('w_router', [1024, 36]); b_rt_d = din('b_router_rep', [128, 36])
    w_ge_d = din('w_gate_e', [NEXP, 1024, 256]); w_ue_d = din('w_up_e', [NEXP, 1024, 256]); w_de_d = din('w_down_e', [NEXP, 256, 1024])
    gple_d = din('gple_rep', [128, 1024]); w_ple_d = din('w_ple', [256, 1024]); w_pg_d = din('w_ple_gate', [1024, 1024])
    gfin_d = din('gfin_rep', [128, 1024])
    out_d = dout('out', [OT, 1024])
    mlo_s = dscr('mlo_s', [4, 128, OT], BF16)
    x1_s = dscr('x1_s', [OT, 1024], F32)
    xs_s = dscr('xs_s', [NEXP * CAP, 1024], BF16)
    ys_s = dscr('ys_s', [NEXP * CAP, 1024], F32)
    cq_s = dscr('cq_s', [2, 128, OT], BF16); ckv_s = dscr('ckv_s', [128, VT], BF16)
    kr_s = dscr('kr_s', [32, VT], BF16); tab_s = dscr('tab_s', [2, 32, OT], F32)
    dbgo = {}

    def dbg_out(name, shape, dt=F32):
        dbgo[name] = dout(name, shape, dt)
        return dbgo[name]

    dbg_mlo = dbg_out('d_mlo', [4, 128, OT], BF16) if dbg else None
    kb = KB(nc)
    P = kb.P
    with ExitStack() as top:
        sb = lambda name, shape, dt: kb.sb(top, name, shape, dt)
        identF = sb('identF', [128, 128], F32); identB = sb('identB', [128, 128], BF16)
        trilT = sb('trilT', [128, 128], F32); attm = sb('attm', [128, 128], BF16)
        permB = sb('permB', [96, 96], F32); invf = sb('invf', [96, 1], F32)
        dmask = sb('dmask', [4, 4 * GB], F32); onesF = sb('onesF', [128, 128], F32)
        mhalf = sb('mhalf', [128, 1], F32); epsc = sb('epsc', [128, 1], F32)
        for t, d in ((identF, ident_d), (trilT, tril_d), (permB, perm_d), (invf, invf_d), (dmask, dmask_d)):
            kb.dma(t[:], d, w=[t.name[2:]])
        kb.dma(attm[:], attm_d, w=['attm'], q='pool')
        kb.copy('dve', identB[:], identF[:], ['identF'], ['identB'])
        kb.memset('pool', onesF[:], 1.0, ['onesF'])
        kb.memset('pool', mhalf[:], -0.5, ['mhalf'])
        kb.memset('pool', epsc[:], EPS, ['epsc'])
        zt = sb('zt', [128, 2, 1024], BF16)
        kb.memset('pool', zt[:], 0.0, ['zt'])
        C1 = 6.28125
        C2 = 2.0 * np.pi - 6.28125
        MAGIC = 12582912.0

        with ExitStack() as sa:
            sba = lambda name, shape, dt: kb.sb(sa, name, shape, dt)
            kb.ps = [sa.enter_context(nc.psum_tensor('psA%d' % k, [128, 512], F32)) for k in range(8)]
            win = sba('win', [128, 8, NCOL], BF16)
            gmix = sba('gmix', [128, 8], F32); gq = sba('gq', [128, 2], F32); gkv = sba('gkv', [128, 1], F32)
            convw = sba('convw', [128, 16], F32); convb = sba('convb', [128, 4], F32)
            cdiag = sba('cdiag', [128, 16, 128], F32)
            wmq = sba('wmq', [128, 4, 128], BF16); wmk = sba('wmk', [128, 4, 128], BF16); wmv = sba('wmv', [128, 4, 128], BF16)
            bi = sba('bi', [4, 1], F32); bfc = sba('bfc', [4, 1], F32); nbf = sba('nbf', [4, 1], F32)
            gmh = sba('gmh', [128, 4], F32); skipc = sba('skipc', [128, 4], F32)
            ones4 = sba('ones4', [4, 128], F32); onesr = sba('onesr', [4, GT], F32)
            for t, d in ((gmix, gmix_d), (gq, gq_d), (gkv, gkv_d), (convw, convw_d), (convb, convb_d),
                         (bi, bi_d), (bfc, bf_d), (gmh, gmh_d), (skipc, skip_d)):
                kb.dma(t[:], d, w=[t.name[2:]])
            kb.ts('pool', nbf[:], bfc[:], -1.0, 0.0, ALU.mult, ALU.add, ['bfc'], ['nbf'])
            kb.memset('pool', ones4[:], 1.0, ['ones4'])
            kb.memset('pool', onesr[:], 1.0, ['onesr'])
            for t, d in ((wmq, w_mq_d), (wmk, w_mk_d), (wmv, w_mv_d)):
                kb.dma(t[:], d.rearrange("h d e -> d h e"), w=[t.name[2:]], q='pool')
            with ExitStack() as sw:
                stg = Ring(kb, sw, 'wstg', [128, NCOL], F32, 8)
                w_in_v = w_in_d.rearrange("(c p) n -> p c n", p=128)
                for c in range(8):
                    t, k = stg.next()
                    kb.dma(t[:], w_in_v[:, c, :], w=[k])
                    kb.ts('dve', win[:, c, :], t[:], gmix[:, c:c + 1], None, ALU.mult, None, [k, 'gmix'], ['win'])
            P.barrier()
            zf_todo = list(range(0, NEXP * CAP, 256))

            def zero_fill(n):
                for _ in range(n):
                    if zf_todo:
                        r0 = zf_todo.pop(0)
                        kb.dma(xs_s[r0:r0 + 256, :].rearrange('(p k) d -> p k d', k=2), zt[:], r=['zt'], w=['xs_s'], q='pool')
            for j in range(16):
                kb.ts('pool', cdiag[:, j, :], identF[:], convw[:, j:j + 1], 0.0, ALU.mult, ALU.add, ['identF', 'convw'], ['cdiag'])

            xt_r = Ring(kb, sa, 'xt', [128, 1024], F32, 4)
            hb_r = Ring(kb, sa, 'hb', [128, 1024], BF16, 3)
            st_r = Ring(kb, sa, 'st', [128, 4], F32, 4)
            hT_r = Ring(kb, sa, 'hT', [128, 8, GT], BF16, 1)
            sq_r = Ring(kb, sa, 'sq', [128, GT], F32, 2)
            rv_r = Ring(kb, sa, 'rv', [128, GT], F32, 2)
            lat_r = Ring(kb, sa, 'lat', [128, GT], BF16, 2)
            xmF = sba('xmF', [128, 4, 4 + GT], F32)
            krF = sba('krF', [96, GT], F32)
            rt1 = sba('rt1', [96, GT], F32); rt2 = sba('rt2', [96, GT], F32); krB = sba('krB', [96, GT], BF16)
            tab_r = Ring(kb, sa, 'tab', [96, 2, GT], F32, 2)
            posi = sba('posi', [96, GT], I32); angf = sba('angf', [96, GT], F32); rtmp = sba('rtmp', [96, GT], F32); rk = sba('rk', [96, GT], F32)
            gU = sba('gU', [4, GT], F32); gS = sba('gS', [4, GT], F32); gNB = sba('gNB', [4, GT], F32)
            gG = sba('gG', [4, GT], F32); gW = sba('gW', [4, GT], F32); gE = sba('gE', [4, GT], F32)
            gval = sba('gval', [4, GT], F32); gib = sba('gib', [4, GT], F32)
            Rall = sba('Rall', [4, GB + 1], F32); dec4 = sba('dec4', [4, GB], F32); Rm = sba('Rm', [4, 4, GB], F32)
            cNB = sba('cNB', [4, 1], F32); cG = sba('cG', [4, 1], F32)
            xmB_r = Ring(kb, sa, 'xmB', [128, 4, GT], BF16, 2)
            xcB_r = Ring(kb, sa, 'xcB', [128, 4, GT], BF16, 2)
            xcFo_r = Ring(kb, sa, 'xcFo', [128, 4, GO], F32, 2)
            zsF_r = Ring(kb, sa, 'zsF', [128, 4, GO], F32, 2)
            wcol_r = Ring(kb, sa, 'wcol', [128, GB, 8], F32, 2)
            dcol_r = Ring(kb, sa, 'dcol', [128, 4, GB], F32, 2)
            qT_r = Ring(kb, sa, 'qT', [128, 4, GO], BF16, 2)
            kT_r = Ring(kb, sa, 'kT', [128, 4, GO], BF16, 2)
            ktm_r = Ring(kb, sa, 'ktm', [128, 4, 128], BF16, 4)
            wv_r = Ring(kb, sa, 'wv', [128, 4, 132], BF16, 4)
            a0_r = Ring(kb, sa, 'a0', [128, 4, 128], BF16, 3)
            Cst = sba('Cst', [128, 4, 132], F32); Cd32 = sba('Cd32', [128, 4, 132], F32); Cd16 = sba('Cd16', [128, 4, 132], BF16)
            hN_r = Ring(kb, sa, 'hN', [128, 4, 128], F32, 2); hc_r = Ring(kb, sa, 'hc', [128, 4, 128], F32, 2)
            bst = sba('bst', [128, 4, 6], F32); bmv = sba('bmv', [128, 4, 2], F32); rstd = sba('rstd', [128, 4], F32)
            dcl = sba('dcl', [128, 4], F32); rden = sba('rden', [128, 4], F32)
            f1_r = Ring(kb, sa, 'f1', [128, 4, 128], F32, 2); f2_r = Ring(kb, sa, 'f2', [128, 4, 128], F32, 2)
            mlo_r = Ring(kb, sa, 'mlo', [128, 4, 128], BF16, 2)
            kb.memset('pool', xmF[:], 0.0, ['xmF'])
            kb.memset('pool', Cst[:], 0.0, ['Cst'])
            kb.memset('pool', cNB[:], 0.0, ['cNB'])
            kb.memset('pool', cG[:], 0.0, ['cG'])
            xv_b = xv.rearrange("(n p) d -> n p d", p=128)
            rows = slice(64, 96)
            v3 = lambda t: t[:].rearrange("p (b t) -> p b t", t=128)
            GS = {}

            def tables(g):
                tab, ktab = tab_r.next()
                T0 = g * GT
                kb.dma(posi[rows, :], posk[:, T0:T0 + GT], w=['posi'])
                kb.copy('dve', angf[rows, :], posi[rows, :], ['posi'], ['angf'])
                kb.ts('dve', angf[rows, :], angf[rows, :], invf[rows, 0:1], None, ALU.mult, None, ['angf', 'invf'], ['angf'])
                kb.ts('dve', rk[rows, :], angf[rows, :], 1.0 / (2 * np.pi), MAGIC, ALU.mult, ALU.add, ['angf'], ['rk'])
                kb.ts('dve', rk[rows, :], rk[rows, :], -MAGIC, None, ALU.add, None, ['rk'], ['rk'])
                kb.stt(rtmp[rows, :], rk[rows, :], -C1, angf[rows, :], ALU.mult, ALU.add, ['rk', 'angf'], ['rtmp'])
                kb.stt(rtmp[rows, :], rk[rows, :], -C2, rtmp[rows, :], ALU.mult, ALU.add, ['rk', 'rtmp'], ['rtmp'])
                kb.ts('dve', rtmp[rows, :], rtmp[rows, :], np.pi, -np.pi, ALU.min, ALU.max, ['rtmp'], ['rtmp'])
                kb.act(tab[rows, 1, :], rtmp[rows, :], AF.Sin, ['rtmp'], [ktab])
                kb.act(rtmp[rows, :], rtmp[rows, :], AF.Abs, ['rtmp'], ['rtmp'])
                kb.act(tab[rows, 0, :], rtmp[rows, :], AF.Sin, ['rtmp', 'halfpi'], [ktab], bias=halfpi[rows, 0:1], scale=-1.0)
                for ti in range(2):
                    kb.dma(tab_s[ti, :, g * GO:(g + 1) * GO].rearrange("p (b t) -> p b t", t=128),
                           tab[rows, ti, :].rearrange("p (b t) -> p b t", t=128)[:, 1::2, :], r=[ktab], w=[('tab_s', g)])
                return tab, ktab

            halfpi = sba('halfpi', [96, 1], F32)
            kb.memset('pool', halfpi[:], float(np.pi / 2), ['halfpi'])

            def P_part(g, part):
                T0 = g * GT
                O0 = g * GO
                if part == 0:
                    GS[g] = dict(tab=GS.pop(('tab', g)))
                    hT, khT = hT_r.next()
                    GS[g].update(hT=hT, khT=khT, xts=[])
                    for j in range(GB):
                        xt, kx = xt_r.next()
                        kb.dma(xt[:], xv_b[g * GB + j], w=[kx])
                        GS[g]['xts'].append((xt, kx))
                st = GS[g]
                hT, khT = st['hT'], st['khT']
                def a1_act(js):
                    for j in js:
                        xt, kx = st['xts'][j]
                        hb, kh = hb_r.next(); stt_, ks = st_r.next()
                        kb.act(hb[:], xt[:], AF.Square, [kx], [kh, ks], accum=stt_[:, 0:1])
                        kb.act(stt_[:, 1:2], stt_[:, 0:1], AF.Ln, [ks, 'epsc'], [ks], bias=epsc[:], scale=1.0 / 1024)
                        kb.act(stt_[:, 2:3], stt_[:, 1:2], AF.Exp, [ks], [ks], scale=-0.5)
                        kb.act(hb[:], xt[:], AF.Copy, [kx, ks], [kh], scale=stt_[:, 2:3])
                        st.setdefault('hbs', {})[j] = (hb, kh)

                def a1_pe(js):
                    for j in js:
                        hb, kh = st['hbs'].pop(j)
                        pst, kp = kb.psum()
                        pstb = pst[:].bitcast(BF16)
                        for c in range(8):
                            kb.tr(pstb[:, c * 128:(c + 1) * 128], hb[:, c * 128:(c + 1) * 128], identB[:], [kh, 'identB'], [kp])
                        kb.copy('dve', hT[:, :, j * 128:(j + 1) * 128], pstb.rearrange("p (c t) -> p c t", c=8), [kp], [khT])

                if part == 0:
                    a1_act((0, 1))
                    return
                if part == 1:
                    a1_pe((0, 1))
                    a1_act((2, 3))
                    return
                if part == 2:
                    a1_pe((2, 3))
                rhs_all = lambda c: hT[:, c, :]
                hT_own = lambda c: hT[:, c, :].rearrange("p (b t) -> p b t", t=128)[:, 1::2, :]
                if part == 2:
                    tab, ktab = st['tab']
                    xmB, kxmB = xmB_r.next(); zsF, kzs = zsF_r.next()
                    st.update(xmB=xmB, kxmB=kxmB, zsF=zsF, kzs=kzs)
                    for h in range(4):
                        ps, kp = kb.psum()
                        for c in range(8):
                            kb.mm(ps[:, :], win[:, c, C_XM + h * 128:C_XM + (h + 1) * 128], rhs_all(c), c == 0, c == 7, ['win', khT], [kp])
                        kb.copy('act', xmF[:, h, 4:4 + GT], ps[:, :], [kp], ['xmF'])
                        kb.copy('dve', xmB[:, h, :], ps[:, :], [kp], [kxmB])
                    psi_, kpi = kb.psum()
                    for c in range(8):
                        kb.mm(psi_[0:4, :], win[:, c, C_GI:C_GI + 4], rhs_all(c), c == 0, c == 7, ['win', khT], [kpi])
                    psf_, kpf = kb.psum()
                    for c in range(8):
                        kb.mm(psf_[0:4, :], win[:, c, C_GF:C_GF + 4], rhs_all(c), c == 0, c == 7, ['win', khT], [kpf])
                    kb.act(gU[:], psi_[0:4, :], AF.Identity, [kpi, 'bi'], ['gU'], bias=bi[:])
                    kb.act(gS[:], psf_[0:4, :], AF.Exp, [kpf, 'nbf'], ['gS'], bias=nbf[:], scale=-1.0)
                    kb.act(gS[:], gS[:], AF.Ln, ['gS'], ['gS'], bias=1.0)
                    ps, kp = kb.psum()
                    for c in range(8):
                        kb.mm(ps[:, :], win[:, c, C_CKV:C_CKV + 128], rhs_all(c), c == 0, c == 7, ['win', khT], [kp])
                    sq, ksq = sq_r.next(); rv, krv = rv_r.next(); lat, klat = lat_r.next()
                    kb.act(sq[:], ps[:, :], AF.Square, [kp], [ksq])
                    ps2, kp2 = kb.psum()
                    kb.mm(ps2[:, :], onesF[:], sq[:], True, True, ['onesF', ksq], [kp2])
                    kb.act(rv[:], ps2[:, :], AF.Ln, [kp2, 'epsc'], [krv], bias=epsc[:], scale=1.0 / 128)
                    kb.act(rv[:], rv[:], AF.Exp, [krv], [krv], scale=-0.5)
                    kb.stt(lat[:], ps[:, :], gkv[:, 0:1], rv[:], ALU.mult, ALU.mult, [kp, 'gkv', krv], [klat])
                    kb.dma(ckv_s[:, T0:T0 + GT], lat[:], r=[klat], w=[('ckv_s', g)])
                    ps, kp = kb.psum()
                    for c in range(8):
                        kb.mm(ps[0:96, :], win[:, c, C_MISC:C_MISC + 96], rhs_all(c), c == 0, c == 7, ['win', khT], [kp])
                    kb.copy('act', krF[rows, :], ps[rows, :], [kp], ['krF'])
                    ps2, kp2 = kb.psum()
                    kb.mm(ps2[0:96, :], permB[rows, :], krF[rows, :], True, True, ['permB', 'krF'], [kp2])
                    kb.tt('dve', rt1[rows, :], krF[rows, :], tab[rows, 0, :], ALU.mult, ['krF', ktab], ['rt1'])
                    kb.tt('dve', rt2[rows, :], ps2[rows, :], tab[rows, 1, :], ALU.mult, [kp2, ktab], ['rt2'])
                    kb.tt('dve', krB[rows, :], rt1[rows, :], rt2[rows, :], ALU.add, ['rt1', 'rt2'], ['krB'])
                    kb.dma(kr_s[:, T0:T0 + GT], krB[rows, :], r=['krB'], w=[('kr_s', g)])
                    psq = []
                    for q_ in range(2):
                        ps, kp = kb.psum()
                        for c in range(8):
                            kb.mm(ps[:, 0:GO], win[:, c, C_CQ + q_ * 128:C_CQ + (q_ + 1) * 128], hT_own(c), c == 0, c == 7, ['win', khT], [kp])
                        psq.append((ps, kp))
                    sq, ksq = sq_r.next(); rv, krv = rv_r.next(); lat, klat = lat_r.next()
                    for q_ in range(2):
                        kb.act(sq[:, q_ * GO:(q_ + 1) * GO], psq[q_][0][:, 0:GO], AF.Square, [psq[q_][1]], [ksq])
                    ps2, kp2 = kb.psum()
                    for q_ in range(2):
                        kb.mm(ps2[:, 0:GO], onesF[:], sq[:, q_ * GO:(q_ + 1) * GO], q_ == 0, q_ == 1, ['onesF', ksq], [kp2])
                    kb.act(rv[:, 0:GO], ps2[:, 0:GO], AF.Ln, [kp2, 'epsc'], [krv], bias=epsc[:], scale=1.0 / 256)
                    kb.act(rv[:, 0:GO], rv[:, 0:GO], AF.Exp, [krv], [krv], scale=-0.5)
                    for q_ in range(2):
                        kb.stt(lat[:, q_ * GO:(q_ + 1) * GO], psq[q_][0][:, 0:GO], gq[:, q_:q_ + 1], rv[:, 0:GO], ALU.mult, ALU.mult,
                               [psq[q_][1], 'gq', krv], [klat])
                    kb.dma(cq_s[:, :, O0:O0 + GO].rearrange("q p t -> p q t"), lat[:].rearrange("p (q t) -> p q t", q=2), r=[klat], w=[('cq_s', g)])
                    for h in range(4):
                        ps, kp = kb.psum()
                        for c in range(8):
                            kb.mm(ps[:, 0:GO], win[:, c, C_Z + h * 128:C_Z + (h + 1) * 128], hT_own(c), c == 0, c == 7, ['win', khT], [kp])
                        kb.act(zsF[:, h, :], ps[:, 0:GO], AF.Silu, [kp], [kzs])
                    return
                xcB, kxcB = xcB_r.next(); xcFo, kxcFo = xcFo_r.next()
                wcol, kwcol = wcol_r.next(); dcol, kdcol = dcol_r.next()
                qT, kqT = qT_r.next(); kT, kkT = kT_r.next()
                st.update(xcB=xcB, kxcB=kxcB, xcFo=xcFo, kxcFo=kxcFo, wcol=wcol, kwcol=kwcol, dcol=dcol, kdcol=kdcol, qT=qT, kqT=kqT, kT=kT, kkT=kkT)
                for h in range(4):
                    ps, kp = kb.psum()
                    for k in range(4):
                        kb.mm(ps[:, :], cdiag[:, h * 4 + k, :], xmF[:, h, 1 + k:1 + k + GT], k == 0, k == 3, ['cdiag', 'xmF'], [kp])
                    kb.act(xcB[:, h, :], ps[:, :], AF.Silu, [kp, 'convb'], [kxcB], bias=convb[:, h:h + 1])
                    kb.act(xcFo[:, h, :].rearrange("p (b t) -> p b t", t=128), ps[:, :].rearrange("p (b t) -> p b t", t=128)[:, 1::2, :],
                           AF.Silu, [kp, 'convb'], [kxcFo], bias=convb[:, h:h + 1])
                kb.copy('pool', xmF[:, :, 1:4], xmF[:, :, 1 + GT:4 + GT], ['xmF'], ['xmF'])
                kb.dma(gval[:], valid4[:, T0:T0 + GT], w=['gval'])
                kb.dma(gib[:], ibias4[:, T0:T0 + GT], w=['gib'])
                kb.tt('dve', gS[:], gS[:], gval[:], ALU.mult, ['gS', 'gval'], ['gS'])
                kb.scan(gNB[:], onesr[:], gS[:], cNB[:, 0:1], ALU.mult, ALU.add, ['onesr', 'gS', 'cNB'], ['gNB'])
                kb.tt('dve', gU[:], gU[:], gNB[:], ALU.add, ['gU', 'gNB'], ['gU'])
                kb.tt('dve', gU[:], gU[:], gib[:], ALU.add, ['gU', 'gib'], ['gU'])
                kb.scan(gG[:], onesr[:], gU[:], cG[:, 0:1], ALU.mult, ALU.max, ['onesr', 'gU', 'cG'], ['gG'])
                kb.copy('dve', Rall[:, 0:1], cG[:], ['cG'], ['Rall'])
                kb.copy('dve', Rall[:, 1:GB + 1], gG[:].rearrange("p (b t) -> p b t", t=128)[:, :, 127], ['gG'], ['Rall'])
                kb.copy('dve', cNB[:], gNB[:, GT - 1:GT], ['gNB'], ['cNB'])
                kb.copy('dve', cG[:], gG[:, GT - 1:GT], ['gG'], ['cG'])
                Rb = bc(Rall[:, 1:GB + 1].unsqueeze(2), [4, GB, 128])
                kb.tt('dve', v3(gW), v3(gU), Rb, ALU.subtract, ['gU', 'Rall'], ['gW'])
                kb.tt('dve', v3(gE), v3(gNB), Rb, ALU.subtract, ['gNB', 'Rall'], ['gE'])
                kb.tt('dve', dec4[:], Rall[:, 0:GB], Rall[:, 1:GB + 1], ALU.subtract, ['Rall'], ['dec4'])
                kb.act(dec4[:], dec4[:], AF.Exp, ['dec4'], ['dec4'])
                kb.act(gW[:], gW[:], AF.Exp, ['gW'], ['gW'])
                kb.act(gE[:], gE[:], AF.Exp, ['gE'], ['gE'])
                kb.tt('dve', Rm[:], bc(dec4[:].unsqueeze(1), [4, 4, GB]), dmask[:].rearrange("p (h b) -> p h b", b=GB), ALU.mult,
                      ['dec4', 'dmask'], ['Rm'])
                psg, kpg = kb.psum()
                for b_ in range(GB):
                    kb.tr(psg[:, b_ * 8:b_ * 8 + 4], gW[:, b_ * 128:(b_ + 1) * 128], identF[0:4, 0:4], ['gW', 'identF'], [kpg])
                    kb.tr(psg[:, b_ * 8 + 4:b_ * 8 + 8], gE[:, b_ * 128:(b_ + 1) * 128], identF[0:4, 0:4], ['gE', 'identF'], [kpg])
                kb.copy('dve', wcol[:].rearrange("p b e -> p (b e)"), psg[:, 0:GB * 8], [kpg], [kwcol])
                psd, kpd = kb.psum()
                kb.mm(psd[:, 0:4 * GB], ones4[:], Rm[:].rearrange("p h b -> p (h b)"), True, True, ['ones4', 'Rm'], [kpd])
                kb.copy('dve', dcol[:].rearrange("p h b -> p (h b)"), psd[:, 0:4 * GB], [kpd], [kdcol])
                xc_own = lambda h: xcB[:, h, :].rearrange("p (b t) -> p b t", t=128)[:, 1::2, :]
                for hp in range(2):
                    psq_, kpq = kb.psum()
                    psk_, kpk = kb.psum()
                    for hh in range(2):
                        h = hp * 2 + hh
                        kb.mm(psq_[:, hh * GO:(hh + 1) * GO], wmq[:, h, :], xc_own(h), True, True, ['wmq', kxcB], [kpq])
                        kb.mm(psk_[:, hh * GO:(hh + 1) * GO], wmk[:, h, :], xc_own(h), True, True, ['wmk', kxcB], [kpk])
                    kb.copy('act', qT[:, hp * 2:hp * 2 + 2, :].rearrange("p h t -> p (h t)"), psq_[:, 0:2 * GO], [kpq], [kqT])
                    kb.act(kT[:, hp * 2:hp * 2 + 2, :].rearrange("p h t -> p (h t)"), psk_[:, 0:2 * GO], AF.Copy, [kpk], [kkT], scale=float(128 ** -0.5))
                if g + 1 < NG:
                    GS[('tab', g + 1)] = tables(g + 1)

            def M_pre(g, j):
                st = GS[g]
                xmB, kxmB = st['xmB'], st['kxmB']
                xcB, kxcB = st['xcB'], st['kxcB']
                wcol, kwcol = st['wcol'], st['kwcol']
                qT, kqT, kT, kkT = st['qT'], st['kqT'], st['kT'], st['kkT']
                owned = (j % 2 == 1)
                ob = j // 2
                bsl = slice(j * 128, (j + 1) * 128)
                ktm, kkt = ktm_r.next(); wv, kwv = wv_r.next()
                psk_, kpk = kb.psum()
                psv_, kpv = kb.psum()
                for h in range(4):
                    kb.mm(psk_[:, h * 128:(h + 1) * 128], xcB[:, h, bsl], wmk[:, h, :], True, True, [kxcB, 'wmk'], [kpk])
                for h in range(4):
                    kb.mm(psv_[:, h * 128:(h + 1) * 128], xmB[:, h, bsl], wmv[:, h, :], True, True, [kxmB, 'wmv'], [kpv])
                kb.act(ktm[:].rearrange("p h d -> p (h d)"), psk_[:, :], AF.Copy, [kpk], [kkt], scale=float(128 ** -0.5))
                wsl = wcol[:, j, 0:4]
                kb.tt('dve', wv[:, :, 0:128], psv_[:, :].rearrange("p (h d) -> p h d", d=128), bc(wsl.unsqueeze(2), [128, 4, 128]), ALU.mult,
                      [kpv, kwcol], [kwv])
                kb.copy('pool', wv[:, :, 128:129], wsl.unsqueeze(2), [kwcol], [kwv])
                a0, ka0 = None, None
                if owned:
                    osl = slice(ob * 128, (ob + 1) * 128)
                    pss, kps = kb.psum()
                    for h in range(4):
                        kb.mm(pss[:, h * 128:(h + 1) * 128], kT[:, h, osl], qT[:, h, osl], True, True, [kkT, kqT], [kps])
                    a0, ka0 = a0_r.next()
                    kb.tt('dve', a0[:], pss[:, :].rearrange("p (h t) -> p h t", t=128), bc(trilT[:].unsqueeze(1), [128, 4, 128]), ALU.mult,
                          [kps, 'trilT'], [ka0])
                st.setdefault('pre', {})[j] = (ktm, kkt, wv, kwv, a0, ka0)

            def M_block(g, j):
                st = GS[g]
                xmB, kxmB, zsF, kzs = st['xmB'], st['kxmB'], st['zsF'], st['kzs']
                xcB, kxcB, xcFo, kxcFo = st['xcB'], st['kxcB'], st['xcFo'], st['kxcFo']
                wcol, kwcol, dcol, kdcol = st['wcol'], st['kwcol'], st['dcol'], st['kdcol']
                qT, kqT, kT, kkT = st['qT'], st['kqT'], st['kT'], st['kkT']
                owned = (j % 2 == 1)
                ob = j // 2
                bsl = slice(j * 128, (j + 1) * 128)
                ktm, kkt, wv, kwv, a0, ka0 = st['pre'].pop(j)
                dsl = dcol[:, :, j]
                kb.tt('dve', Cd32[:, :, 0:129], Cst[:, :, 0:129], bc(dsl.unsqueeze(2), [128, 4, 129]), ALU.mult, ['Cst', kdcol], ['Cd32'])
                if owned:
                    kb.copy('act', Cd16[:, :, 0:129], Cd32[:, :, 0:129], ['Cd32'], ['Cd16'])
                    osl = slice(ob * 128, (ob + 1) * 128)
                    pn = []
                    for hp in range(2):
                        psn, kpn = kb.psum()
                        for hh in range(2):
                            h = hp * 2 + hh
                            kb.mm(psn[:, hh * 256:hh * 256 + 129], a0[:, h, :], wv[:, h, 0:129], True, False, [ka0, kwv], [kpn])
                            kb.mm(psn[:, hh * 256:hh * 256 + 129], qT[:, h, osl], Cd16[:, h, 0:129], False, True, [kqT, 'Cd16'], [kpn])
                        pn.append((psn, kpn))
                for hp in range(2):
                    psu, kpu = kb.psum()
                    for hh in range(2):
                        h = hp * 2 + hh
                        kb.mm(psu[:, hh * 256:hh * 256 + 129], ktm[:, h, :], wv[:, h, 0:129], True, True, [kkt, kwv], [kpu])
                    kb.tt('dve', Cst[:, hp * 2:hp * 2 + 2, 0:129], Cd32[:, hp * 2:hp * 2 + 2, 0:129],
                          psu[:, :].rearrange("p (h c) -> p h c", c=256)[:, :, 0:129], ALU.add, ['Cd32', kpu], ['Cst'])
                if owned:
                    hN, khN = hN_r.next(); hc, khc = hc_r.next(); f1, kf1 = f1_r.next(); f2, kf2 = f2_r.next(); mlo, kmlo = mlo_r.next()
                    ecl = wcol[:, j, 4:8]
                    for hp in range(2):
                        psn, kpn = pn[hp]
                        pv3 = psn[:, :].rearrange("p (h c) -> p h c", c=256)
                        kb.stt(rden[:, hp * 2:hp * 2 + 2], pv3[:, :, 128], -1.0, ecl[:, hp * 2:hp * 2 + 2], ALU.mult, ALU.max, [kpn, kwcol], ['rden'])
                        kb.tt('dve', dcl[:, hp * 2:hp * 2 + 2], pv3[:, :, 128], rden[:, hp * 2:hp * 2 + 2], ALU.max, [kpn, 'rden'], ['dcl'])
                    kb.P.add('dve', lambda e: e.reciprocal(out=rden[:], in_=dcl[:]), ['dcl'], ['rden'])
                    for hp in range(2):
                        psn, kpn = pn[hp]
                        pv3 = psn[:, :].rearrange("p (h c) -> p h c", c=256)
                        kb.tt('dve', hN[:, hp * 2:hp * 2 + 2, :], pv3[:, :, 0:128], bc(rden[:, hp * 2:hp * 2 + 2].unsqueeze(2), [128, 2, 128]), ALU.mult,
                              [kpn, 'rden'], [khN])
                    for h in range(4):
                        kb.P.add('dve', (lambda h: lambda e: e.bn_stats(out=bst[:, h, :], in_=hN[:, h, :]))(h), [khN], ['bst'])
                    for h in range(4):
                        kb.P.add('dve', (lambda h: lambda e: e.bn_aggr(out=bmv[:, h, :], in_=bst[:, h, :]))(h), ['bst'], ['bmv'])
                    kb.ts('dve', rstd[:], bmv[:, :, 1], EPS, None, ALU.add, None, ['bmv'], ['rstd'])
                    kb.tt('pool', rstd[:], rstd[:], bc(mhalf[:], [128, 4]), ALU.pow, ['rstd', 'mhalf'], ['rstd'])
                    for h in range(4):
                        kb.ts('dve', hc[:, h, :], hN[:, h, :], bmv[:, h, 0:1], rstd[:, h:h + 1], ALU.subtract, ALU.mult, [khN, 'bmv', 'rstd'], [khc])
                    pst, kpt = kb.psum()
                    for h in range(4):
                        kb.tr(pst[:, h * 128:(h + 1) * 128], hc[:, h, :], identF[:], [khc, 'identF'], [kpt])
                    kb.tt('dve', f1[:], pst[:, :].rearrange("p (h t) -> p h t", t=128), bc(gmh[:].unsqueeze(2), [128, 4, 128]), ALU.mult,
                          [kpt, 'gmh'], [kf1])
                    kb.tt('pool', f2[:], xcFo[:, :, osl], bc(skipc[:].unsqueeze(2), [128, 4, 128]), ALU.mult, [kxcFo, 'skipc'], [kf2])
                    kb.tt('pool', f1[:], f1[:], f2[:], ALU.add, [kf1, kf2], [kf1])
                    kb.tt('pool', mlo[:], f1[:], zsF[:, :, osl], ALU.mult, [kf1, kzs], [kmlo])
                    gob = g * (GB // 2) + ob
                    kb.dma(mlo_s[:, :, gob * 128:(gob + 1) * 128].rearrange("h p t -> p h t"), mlo[:], r=[kmlo], w=[('mlo_s', gob)])
                    if dbg:
                        kb.dma(dbg_mlo[:, :, gob * 128:(gob + 1) * 128].rearrange("h p t -> p h t"), mlo[:], r=[kmlo], w=[('d_mlo', gob)])

            steps = [lambda: GS.__setitem__(('tab', 0), tables(0))]
            for part in range(4):
                steps.append((lambda part: lambda: P_part(0, part))(part))
            pq = [(g_, p_) for g_ in range(1, NG) for p_ in range(4)]
            for _ in range(2):
                if pq:
                    steps.append((lambda gp: lambda: P_part(*gp))(pq.pop(0)))
            for g in range(NG):
                steps.append((lambda g: lambda: M_pre(g, 0))(g))
                steps.append((lambda g: lambda: M_pre(g, 1))(g))
                steps.append((lambda g: lambda: M_pre(g, 2))(g))
                for j in range(GB):
                    steps.append((lambda g, j: lambda: M_block(g, j))(g, j))
                    if j + 3 < GB:
                        steps.append((lambda g, j: lambda: M_pre(g, j + 3))(g, j))
                    if pq:
                        steps.append((lambda gp: lambda: P_part(*gp))(pq.pop(0)))
            for i_, f_ in enumerate(steps):
                f_()
                if i_ % 3 == 2:
                    zero_fill(1)
            zero_fill(len(zf_todo))
        P.barrier()
        if stop_after == 'A':
            fin_r = [('mlo_s', gob) for gob in range(NOB)] + [('d_mlo', gob) for gob in range(NOB)] + [('ckv_s', g) for g in range(NG)] + \
                [('cq_s', g) for g in range(NG)] + [('kr_s', g) for g in range(NG)] + [('tab_s', g) for g in range(NG)]
            P.add('sp', None, reads=[k for k in fin_r if k in P.last_w])
            P.emit(top)
            return nc, D, dbgo

        attnTok = sb('attnTok', [128, NOB, 512], BF16)
        wo = sb('wo', [128, 8, 1024], BF16)
        kb.dma(wo[:], w_o_d.rearrange("(c p) n -> p c n", p=128), w=['wo'], q='pool')
        NQG = NOB // 4
        with ExitStack() as sbk:
            sbb = lambda name, shape, dt: kb.sb(sbk, name, shape, dt)
            stt2 = [sbk.enter_context(nc.psum_tensor('psS%d' % k, [128, 1024], F32)) for k in range(3)]
            pvt = [sbk.enter_context(nc.psum_tensor('psV%d' % k, [128, 512], F32)) for k in range(2)]
            wuq = sbb('wuq', [128, 2, 768], BF16); wukv = sbb('wukv', [128, 1024], BF16)
            kb.dma(wuq[:], w_uq_d.rearrange("(c p) n -> p c n", p=128), w=['wuq'], q='pool')
            kb.dma(wukv[:], w_ukv_d, w=['wukv'], q='pool')
            cosq = sbb('cosq', [96, OT], F32); sinq = sbb('sinq', [96, OT], F32)
            cqnT = sbb('cqnT', [128, 2, OT], BF16); ckvnT = sbb('ckvnT', [128, VT], BF16)
            KhT = sbb('KhT', [97, VT], BF16)
            allA = [('ckv_s', g_) for g_ in range(NG)] + [('cq_s', g_) for g_ in range(NG)] + [('kr_s', g_) for g_ in range(NG)] + [('tab_s', g_) for g_ in range(NG)]
            kb.dma(ckvnT[:], ckv_s, r=allA, w=['ckvnT'])
            kb.dma(cqnT[:], cq_s.rearrange("q p t -> p q t"), r=allA, w=['cqnT'])
            kb.dma(KhT[64:96, :], kr_s, r=allA, w=['KhT_r'])
            kb.dma(KhT[96:97, :], kbias, w=['KhT_b'], q='pool')
            kb.dma(cosq[64:96, :], tab_s[0], r=allA, w=['tabQ'])
            kb.dma(sinq[64:96, :], tab_s[1], r=allA, w=['tabQ'])
            kb.ts('dve', cosq[64:96, :], cosq[64:96, :], float(QSCALE), None, ALU.mult, None, ['tabQ'], ['tabQ'])
            kb.ts('dve', sinq[64:96, :], sinq[64:96, :], float(QSCALE), None, ALU.mult, None, ['tabQ'], ['tabQ'])
            Vh = sbb('Vh', [128, NVB, 65], BF16)
            QhT = sbb('QhT', [97, OT], BF16)
            kb.memset('pool', Vh[:, :, 64:65], 1.0, ['Vh1'])
            kb.memset('pool', QhT[96:97, :], 1.0, ['QhT1'])
            qraw = sbb('qraw', [96, 512], F32); qt1 = sbb('qt1', [96, 512], F32); qt2 = sbb('qt2', [96, 512], F32)
            pt_r = Ring(kb, sbk, 'pt', [128, 2, 512], BF16, 4)
            rs = sbb('rs', [128, 4], F32)
            KR = ['KhT_r', 'KhT_b']
            CKV = ['ckvnT']
            CQN = ['cqnT']
            sti = [0]

            def st_next():
                k = sti[0] % 3
                sti[0] += 1
                return stt2[k], ('psS', k)

            for h in range(8):
                for ch in range(VT // 512):
                    st_, kst = st_next()
                    kb.mm(st_[0:64, 0:512], wukv[:, h * 128:h * 128 + 64], ckvnT[:, ch * 512:(ch + 1) * 512], True, True, ['wukv'] + CKV, [kst])
                    kb.copy('act' if ch % 2 == 0 else 'dve', KhT[0:64, ch * 512:(ch + 1) * 512], st_[0:64, 0:512], [kst], ['KhT_n'])
                for v8 in range(NVB // 8):
                    st_, kst = st_next()
                    for bb in range(8):
                        blk = v8 * 8 + bb
                        kb.mm(st_[:, bb * 64:(bb + 1) * 64], ckvnT[:, blk * 128:(blk + 1) * 128], wukv[:, h * 128 + 64:h * 128 + 128], True, True,
                              ['wukv'] + CKV, [kst])
                    kb.copy('dve' if v8 % 2 == 0 else 'act', Vh[:, v8 * 8:(v8 + 1) * 8, 0:64], st_[:, 0:512].rearrange("p (b d) -> p b d", d=64), [kst], ['Vh'])
                for qc in range(OT // 512):
                    cs = slice(qc * 512, (qc + 1) * 512)
                    st_, kst = st_next()
                    for c in range(2):
                        kb.mm(st_[0:96, 0:512], wuq[:, c, h * 96:(h + 1) * 96], cqnT[:, c, cs], c == 0, c == 1, ['wuq'] + CQN, [kst])
                    kb.act(QhT[0:64, cs], st_[0:64, 0:512], AF.Copy, [kst], ['QhT'], scale=float(QSCALE))
                    kb.copy('act', qraw[64:96, :], st_[64:96, 0:512], [kst], ['qraw'])
                    st2_, kst2 = st_next()
                    kb.mm(st2_[0:96, 0:512], permB[64:96, :], qraw[64:96, :], True, True, ['permB', 'qraw'], [kst2])
                    kb.tt('pool', qt1[64:96, :], qraw[64:96, :], cosq[64:96, cs], ALU.mult, ['qraw', 'tabQ'], ['qt1'])
                    kb.tt('dve', qt2[64:96, :], st2_[64:96, 0:512], sinq[64:96, cs], ALU.mult, [kst2, 'tabQ'], ['qt2'])
                    kb.tt('pool', QhT[64:96, cs], qt1[64:96, :], qt2[64:96, :], ALU.add, ['qt1', 'qt2'], ['QhT'])
                KALL = ['KhT_n'] + KR
                batches = []
                for qg in range(NQG):
                    kbs = list(range(8 * qg + 8))
                    nfull = 8 * qg + 2
                    i = 0
                    while i < nfull:
                        n = min(2, nfull - i)
                        batches.append(dict(qg=qg, kbs=kbs[i:i + n], full=True, first=(i == 0), last=False))
                        i += n
                    for kb_ in range(nfull, 8 * qg + 8, 2):
                        batches.append(dict(qg=qg, kbs=[kb_, kb_ + 1], full=False, first=False, last=(kb_ + 1 == 8 * qg + 7)))

                def lmin_of(qg, kb_):
                    return max(0, (kb_ - 8 * qg - 1 + 1) // 2)

                def emit_S(bt):
                    st_, kst = st_next()
                    bt['st'] = (st_, kst)
                    qg = bt['qg']
                    for i, kb_ in enumerate(bt['kbs']):
                        lm = lmin_of(qg, kb_)
                        kb.mm(st_[:, i * 512 + lm * 128:(i + 1) * 512], KhT[0:97, kb_ * 128:(kb_ + 1) * 128],
                              QhT[0:97, qg * 512 + lm * 128:(qg + 1) * 512], True, True, KALL + ['QhT', 'QhT1'], [kst])

                def emit_PV(bt):
                    st_, kst = bt['st']
                    qg = bt['qg']
                    pt, kpt = pt_r.next()
                    pvb, kpv = pvt[qg % 2], ('psV', qg % 2)
                    if bt['first']:
                        kb.memset('dve', pvb[:, :], 0.0, [kpv])
                    if bt['full']:
                        n = len(bt['kbs'])
                        kb.act(pt[:, 0:n, :].rearrange("p a b -> p (a b)"), st_[:, 0:n * 512], AF.Exp, [kst], [kpt])
                    else:
                        lm = lmin_of(qg, bt['kbs'][0])
                        assert lmin_of(qg, bt['kbs'][1]) == lm
                        kb.act(pt[:, 0:2, lm * 128:512], st_[:, 0:1024].rearrange("p (a b) -> p a b", b=512)[:, :, lm * 128:512], AF.Exp, [kst], [kpt])
                    for i, kb_ in enumerate(bt['kbs']):
                        lm = lmin_of(qg, kb_)
                        if kb_ == 8 * qg + 2 * lm + 1:
                            kb.tt('pool', pt[:, i, lm * 128:(lm + 1) * 128], pt[:, i, lm * 128:(lm + 1) * 128], attm[:], ALU.mult, [kpt, 'attm'], [kpt])
                    for i, kb_ in enumerate(bt['kbs']):
                        lm = lmin_of(qg, kb_)
                        for l in range(lm, 4):
                            kb.mm(pvb[:, l * 128:l * 128 + 65], pt[:, i, l * 128:(l + 1) * 128], Vh[:, kb_, 0:65], False, False,
                                  [kpt, 'Vh', 'Vh1'], [kpv], skip=True)
                    if bt['last']:
                        pv3 = pvb[:, :].rearrange("p (l c) -> p l c", c=128)
                        kb.P.add('dve', lambda e: e.reciprocal(out=rs[:], in_=pv3[:, :, 64]), [kpv], ['rs'])
                        kb.tt('dve', attnTok[:, 4 * qg:4 * qg + 4, h * 64:(h + 1) * 64], pv3[:, :, 0:64], bc(rs[:].unsqueeze(2), [128, 4, 64]), ALU.mult,
                              [kpv, 'rs'], [('attnTok', qg)])

                for i, bt in enumerate(batches):
                    if i == 0:
                        emit_S(bt)
                        if len(batches) > 1:
                            emit_S(batches[1])
                    if i + 2 < len(batches):
                        emit_S(batches[i + 2])
                    emit_PV(bt)
            if dbg:
                o = dbg_out('d_attn', [128, NOB * 512], BF16)
                kb.dma(o, attnTok[:].rearrange("p a b -> p (a b)"), r=[('attnTok', qg) for qg in range(NQG)], w=['d_attn'])
        P.barrier()
        if stop_after == 'B':
            P.add('sp', None, reads=list(dbgo.keys()) if False else [k for k in dbgo.keys() if k != 'd_mlo'] + [('d_mlo', gob) for gob in range(NOB)])
            P.emit(top)
            return nc, D, dbgo

        iota32_d = D['iota32']
        route = sb('route', [128, NOB, 2], F32)
        desti = sb('desti', [128, NOB, 2], I32)
        xv_b = xv.rearrange("(n p) d -> n p d", p=128)

        def rms_rinv(stk, xin, kxin, junk, kjunk, pool_pow=False):
            kb.act(junk[:], xin[:], AF.Square, [kxin], [kjunk, stk[1]], accum=stk[0][:, 0:1])
            if pool_pow:
                kb.ts('dve', stk[0][:, 1:2], stk[0][:, 0:1], 1.0 / 1024, EPS, ALU.mult, ALU.add, [stk[1]], [stk[1]])
                kb.tt('pool', stk[0][:, 2:3], stk[0][:, 1:2], mhalf[:], ALU.pow, [stk[1], 'mhalf'], [stk[1]])
                return stk[0][:, 2:3]
            kb.act(stk[0][:, 1:2], stk[0][:, 0:1], AF.Ln, [stk[1], 'epsc'], [stk[1]], bias=epsc[:], scale=1.0 / 1024)
            kb.act(stk[0][:, 2:3], stk[0][:, 1:2], AF.Exp, [stk[1]], [stk[1]], scale=-0.5)
            return stk[0][:, 2:3]

        with ExitStack() as sc_:
            sbc = lambda name, shape, dt: kb.sb(sc_, name, shape, dt)
            kb.ps = [sc_.enter_context(nc.psum_tensor('psC%d' % k, [128, 512], F32)) for k in range(8)]
            gffn = sbc('gffn', [128, 1024], F32); wr = sbc('wr', [128, 8, 36], F32); brt = sbc('brt', [128, 36], F32)
            ustrF = sbc('ustrF', [128, 128], F32); ustrB = sbc('ustrB', [128, 128], BF16); onesB = sbc('onesB', [128, 128], BF16)
            iota32 = sbc('iota32', [128, 32], F32)
            tot = sbc('tot', [128, 32], F32)
            kb.dma(gffn[:], gffn_d, w=['gffn']); kb.dma(wr[:], w_rt_d.rearrange("(c p) n -> p c n", p=128), w=['wr'])
            kb.dma(brt[:], b_rt_d, w=['brt']); kb.dma(ustrF[:], ustr_d, w=['ustrF']); kb.dma(iota32[:], iota32_d, w=['iota32'])
            kb.copy('dve', ustrB[:], ustrF[:], ['ustrF'], ['ustrB'])
            kb.memset('pool', onesB[:], 1.0, ['onesB'])
            kb.memset('pool', tot[:], 0.0, ['tot'])
            mix_r = Ring(kb, sc_, 'mixT', [128, 8, 128], BF16, 4)
            xt_r = Ring(kb, sc_, 'xtc', [128, 1024], F32, 4)
            x1_r = Ring(kb, sc_, 'x1t', [128, 1024], F32, 3)
            hn_r = Ring(kb, sc_, 'hnF', [128, 1024], F32, 2)
            hb_r = Ring(kb, sc_, 'hnb', [128, 1024], BF16, 10)
            jk_r = Ring(kb, sc_, 'jkc', [128, 1024], BF16, 2)
            hnT_r = Ring(kb, sc_, 'hnT', [128, 8, 128], F32, 2)
            st_r = Ring(kb, sc_, 'stc', [128, 4], F32, 4)
            L4 = sbc('L4', [128, 4, 36], F32); sm4 = sbc('sm4', [128, 4, 16], F32)
            oh4 = sbc('oh4', [128, 4, 4], F32); oh1 = sbc('oh1', [128, 4, 8], F32); oh2 = sbc('oh2', [128, 4, 8], F32)
            t48 = sbc('t48', [128, 4, 4, 8], F32); ein = sbc('ein', [128, 4, 8], F32); ein2 = sbc('ein2', [128, 4, 8], F32)
            OH1 = sbc('OH1', [128, 4, 4, 8], F32); OH2 = sbc('OH2', [128, 4, 4, 8], F32); cntB = sbc('cntB', [128, 4, 32], BF16)
            pre = sbc('pre', [128, 4, 32], F32); t32 = sbc('t32', [128, 4, 32], F32); e4 = sbc('e4', [128, 4, 4], F32)
            fl32 = lambda t: t[:].rearrange("p g j -> p (g j)")
            cst = {}

            def c_load(ob):
                vb = 2 * ob + 1
                mixT, kmix = mix_r.next(); xt, kx = xt_r.next()
                cst[ob] = dict(mixT=mixT, kmix=kmix, xt=xt, kx=kx)
                kb.dma(mixT[:, 4:8, :], mlo_s[:, :, ob * 128:(ob + 1) * 128].rearrange("h p t -> p h t"), r=[('mlo_s', ob)], w=[(kmix, 'ml')])
                kb.dma(xt[:], xv_b[vb], w=[kx])

            def c_part1(ob):
                d_ = cst[ob]
                mixT, kmix, xt, kx = d_['mixT'], d_['kmix'], d_['xt'], d_['kx']
                x1t, kx1 = x1_r.next()
                pst, kp = kb.psum()
                pstb = pst[:].bitcast(BF16)
                for c in range(4):
                    kb.tr(pstb[:, c * 128:(c + 1) * 128], attnTok[:, ob, c * 128:(c + 1) * 128], identB[:], [('attnTok', ob // 4), 'identB'], [kp])
                kb.copy('act', mixT[:, 0:4, :].rearrange("p c t -> p (c t)"), pstb[:, 0:512], [kp], [(kmix, 'at')])
                for half in range(2):
                    ps, kp = kb.psum()
                    for c in range(8):
                        kb.mm(ps[:, :], mixT[:, c, :], wo[:, c, half * 512:(half + 1) * 512], c == 0, c == 7, [(kmix, 'at'), (kmix, 'ml'), 'wo'], [kp])
                    kb.tt('dve', x1t[:, half * 512:(half + 1) * 512], ps[:, :], xt[:, half * 512:(half + 1) * 512], ALU.add, [kp, kx], [kx1])
                kb.dma(x1_s[ob * 128:(ob + 1) * 128, :], x1t[:], r=[kx1], w=[('x1_s', ob)])
                jk, kjk = jk_r.next(); stk = st_r.next()
                rinv = rms_rinv(stk, x1t, kx1, jk, kjk)
                d_.update(x1t=x1t, kx1=kx1, stk=stk, rinv=rinv)

            def c_part2(ob):
                d_ = cst[ob]
                x1t, kx1, stk, rinv = d_['x1t'], d_['kx1'], d_['stk'], d_['rinv']
                hnF, khn = hn_r.next(); hnb, khb = hb_r.next()
                d_.update(hnb=hnb, khb=khb)
                kb.stt(hnF[:], x1t[:], rinv, gffn[:], ALU.mult, ALU.mult, [kx1, stk[1], 'gffn'], [khn])
                kb.copy('act', hnb[:], hnF[:], [khn], [khb])
                hnT, khT_ = hnT_r.next()
                for q4 in range(2):
                    pst, kp = kb.psum()
                    for c in range(4):
                        kb.tr(pst[:, c * 128:(c + 1) * 128], hnF[:, (q4 * 4 + c) * 128:(q4 * 4 + c + 1) * 128], identF[:], [khn, 'identF'], [kp])
                    kb.copy('act' if q4 == 0 else 'dve', hnT[:, q4 * 4:q4 * 4 + 4, :].rearrange("p c t -> p (c t)"), pst[:, :], [kp], [khT_])
                psl, kpl = kb.psum()
                for c in range(8):
                    kb.mm(psl[:, 0:36], hnT[:, c, :], wr[:, c, :], c == 0, c == 7, [khT_, 'wr'], [kpl])
                kb.tt('dve', L4[:, ob % 4, :], psl[:, 0:36], brt[:], ALU.add, [kpl, 'brt'], [('L4', ob % 4)])

            def c_route(qd):
                LK = [('L4', i) for i in range(4)]
                A = lambda i: sm4[:, :, i]
                Ab = lambda i, n: bc(sm4[:, :, i:i + 1], [128, 4, n])
                red = lambda out, in_, op, r, w: kb.P.add('dve', lambda e: e.tensor_reduce(out=out, in_=in_, axis=AX.X, op=op), r, w)
                red(A(0), L4[:, :, 0:4], ALU.max, LK, ['s0'])
                kb.tt('dve', oh4[:], L4[:, :, 0:4], Ab(0, 4), ALU.is_equal, LK + ['s0'], ['oh4'])
                kb.tt('dve', e4[:], L4[:, :, 0:4], Ab(0, 4), ALU.subtract, LK + ['s0'], ['e4'])
                kb.act(e4[:], e4[:], AF.Exp, ['e4'], ['e4'])
                red(A(2), e4[:], ALU.add, ['e4'], ['s2'])
                kb.P.add('dve', lambda e: e.reciprocal(out=A(3), in_=A(2)), ['s2'], ['s3'])
                L48 = L4[:, :, 4:36].rearrange("p b (g j) -> p b g j", j=8)
                kb.tt('dve', t48[:], L48, bc(oh4[:].unsqueeze(3), [128, 4, 4, 8]), ALU.mult, LK + ['oh4'], ['t48'])
                red(ein[:], t48[:].rearrange("p b g j -> p b j g"), ALU.add, ['t48'], ['ein'])
                red(A(4), ein[:], ALU.max, ['ein'], ['s4'])
                kb.tt('dve', oh1[:], ein[:], Ab(4, 8), ALU.is_equal, ['ein', 's4'], ['oh1'])
                kb.stt(ein2[:], oh1[:], -1e30, ein[:], ALU.mult, ALU.add, ['oh1', 'ein'], ['ein2'])
                red(A(5), ein2[:], ALU.max, ['ein2'], ['s5'])
                kb.tt('dve', oh2[:], ein2[:], Ab(5, 8), ALU.is_equal, ['ein2', 's5'], ['oh2'])
                kb.tt('dve', A(6), A(5), A(4), ALU.subtract, ['s5', 's4'], ['s6'])
                kb.act(A(6), A(6), AF.Exp, ['s6'], ['s6'])
                kb.ts('dve', A(7), A(6), 1.0, None, ALU.add, None, ['s6'], ['s7'])
                kb.P.add('dve', lambda e: e.reciprocal(out=A(7), in_=A(7)), ['s7'], ['s7'])
                kb.tt('dve', A(8), A(7), A(6), ALU.mult, ['s7', 's6'], ['s8'])
                rk_ = [('route', qd * 4 + i) for i in range(4)]
                kb.tt('dve', route[:, qd * 4:qd * 4 + 4, 0], A(7), A(3), ALU.mult, ['s7', 's3'], rk_)
                kb.tt('dve', route[:, qd * 4:qd * 4 + 4, 1], A(8), A(3), ALU.mult, ['s8', 's3'], rk_)
                o4 = bc(oh4[:].unsqueeze(3), [128, 4, 4, 8])
                kb.tt('dve', OH1[:], o4, bc(oh1[:].unsqueeze(2), [128, 4, 4, 8]), ALU.mult, ['oh4', 'oh1'], ['OH1'])
                kb.tt('dve', OH2[:], o4, bc(oh2[:].unsqueeze(2), [128, 4, 4, 8]), ALU.mult, ['oh4', 'oh2'], ['OH2'])
                f3 = lambda t: t[:].rearrange("p b g j -> p b (g j)")
                kb.tt('dve', cntB[:], f3(OH1), f3(OH2), ALU.add, ['OH1', 'OH2'], ['cntB'])
                psp, kpp = kb.psum()
                for b_ in range(4):
                    kb.mm(psp[:, b_ * 32:(b_ + 1) * 32], ustrB[:], cntB[:, b_, :], True, b_ == 0, ['ustrB', 'cntB'], [kpp])
                    for b2 in range(b_):
                        kb.mm(psp[:, b_ * 32:(b_ + 1) * 32], onesB[:], cntB[:, b2, :], False, b2 == b_ - 1, ['onesB', 'cntB'], [kpp])
                kb.tt('dve', pre[:], psp[:, 0:128].rearrange("p (b e) -> p b e", e=32), bc(tot[:].unsqueeze(1), [128, 4, 32]), ALU.add, [kpp, 'tot'], ['pre'])
                pst2, kpt2 = kb.psum()
                for b_ in range(4):
                    kb.mm(pst2[:, 0:32], onesB[:], cntB[:, b_, :], b_ == 0, b_ == 3, ['onesB', 'cntB'], [kpt2])
                kb.tt('dve', tot[:], tot[:], pst2[:, 0:32], ALU.add, ['tot', kpt2], ['tot'])
                dk_ = [('desti', qd * 4 + i) for i in range(4)]
                for k2, OHk in ((0, OH1), (1, OH2)):
                    kb.tt('dve', t32[:], f3(OHk), pre[:], ALU.mult, ['OH1', 'OH2', 'pre'], ['t32'])
                    red(A(9), t32[:], ALU.add, ['t32'], ['s9'])
                    kb.tt('dve', t32[:], f3(OHk), bc(iota32[:].unsqueeze(1), [128, 4, 32]), ALU.mult, ['OH1', 'OH2', 'iota32'], ['t32'])
                    red(A(10), t32[:], ALU.add, ['t32'], ['s10'])
                    kb.stt(A(11), A(10), float(CAP), A(9), ALU.mult, ALU.add, ['s10', 's9'], ['s11'])
                    kb.ts('dve', A(11), A(11), float(NEXP * CAP - 1), None, ALU.min, None, ['s11'], ['s11'])
                    kb.copy('dve', desti[:, qd * 4:qd * 4 + 4, k2], A(11), ['s11'], dk_)

            def c_scatter(ob):
                d_ = cst.pop(ob)
                hnb, khb = d_['hnb'], d_['khb']
                for k2 in range(2):
                    kb.P.add('pool', (lambda ob, k2, hnb: lambda e: e.indirect_dma_start(
                        out=xs_s[:, :], out_offset=bass.IndirectOffsetOnAxis(ap=desti[:, ob, k2:k2 + 1], axis=0),
                        in_=hnb[:], in_offset=None))(ob, k2, hnb),
                        [khb, ('desti', ob)], ['xs_s'], dma=True)

            c_load(0)
            if NOB > 1:
                c_load(1)
            c_part1(0)
            pend_sc = []
            for ob in range(NOB):
                if ob + 2 < NOB:
                    c_load(ob + 2)
                if ob + 1 < NOB:
                    c_part1(ob + 1)
                c_part2(ob)
                if pend_sc:
                    c_scatter(pend_sc.pop(0))
                if ob % 4 == 3:
                    c_route(ob // 4)
                    pend_sc.extend(range(ob - 3, ob + 1))
            while pend_sc:
                c_scatter(pend_sc.pop(0))
            if dbg:
                o = dbg_out('d_route', [128, NOB * 2], F32)
                kb.dma(o, route[:].rearrange("p a b -> p (a b)"), r=[('route', ob) for ob in range(NOB)], w=['d_route'])
                o = dbg_out('d_desti', [128, NOB * 2], I32)
                kb.dma(o, desti[:].rearrange("p a b -> p (a b)"), r=[('desti', ob) for ob in range(NOB)], w=['d_desti'])
                o = dbg_out('d_x1', [OT, 1024], F32)
                kb.dma(o, x1_s, r=[('x1_s', ob) for ob in range(NOB)], w=['d_x1'])
        P.barrier()
        if stop_after == 'C':
            P.add('sp', None, reads=[k for k in dbgo.keys() if k != 'd_mlo'] + [('d_mlo', gob) for gob in range(NOB)] + ['xs_s'])
            P.emit(top)
            return nc, D, dbgo

        NBLK = CAP // 128
        with ExitStack() as sd_:
            sbd = lambda name, shape, dt: kb.sb(sd_, name, shape, dt)
            kb.ps = [sd_.enter_context(nc.psum_tensor('psD%d' % k, [128, 512], F32)) for k in range(8)]
            wg_r = Ring(kb, sd_, 'wg', [128, 8, 256], BF16, 3); wu_r = Ring(kb, sd_, 'wu', [128, 8, 256], BF16, 3)
            wd_r = Ring(kb, sd_, 'wd', [128, 2, 1024], BF16, 3)
            xsb_r = Ring(kb, sd_, 'xsb', [128, 1024], BF16, 3 * NBLK)
            xsT_r = Ring(kb, sd_, 'xsT', [128, 8, CAP], BF16, 2)
            sg_r = Ring(kb, sd_, 'sg', [128, CAP], F32, 2)
            aT_r = Ring(kb, sd_, 'actT', [128, 2, CAP], BF16, 2)
            yb_r = Ring(kb, sd_, 'yb', [128, 1024], F32, 3)
            dst = {}

            def d_load(e_):
                wg, kwg = wg_r.next(); wu, kwu = wu_r.next(); wd, kwd = wd_r.next()
                kb.dma(wg[:], w_ge_d[e_].rearrange("(c p) f -> p c f", p=128), w=[kwg], q='pool')
                kb.dma(wu[:], w_ue_d[e_].rearrange("(c p) f -> p c f", p=128), w=[kwu], q='pool')
                kb.dma(wd[:], w_de_d[e_].rearrange("(c p) n -> p c n", p=128), w=[kwd], q='pool')
                xs_l = []
                for blk in range(NBLK):
                    xsb, kxb = xsb_r.next()
                    r0 = e_ * CAP + blk * 128
                    kb.dma(xsb[:], xs_s[r0:r0 + 128, :], r=['xs_s'], w=[kxb])
                    xs_l.append((xsb, kxb))
                dst[e_] = (wg, kwg, wu, kwu, wd, kwd, xs_l)

            dst2 = {}

            def d_tr(e_):
                wg, kwg, wu, kwu, wd, kwd, xs_l = dst.pop(e_)
                xsT, kxT = xsT_r.next()
                for blk in range(NBLK):
                    xsb, kxb = xs_l[blk]
                    pst, kp = kb.psum()
                    pstb = pst[:].bitcast(BF16)
                    for c in range(8):
                        kb.tr(pstb[:, c * 128:(c + 1) * 128], xsb[:, c * 128:(c + 1) * 128], identB[:], [kxb, 'identB'], [kp])
                    kb.copy('dve' if blk % 2 == 0 else 'act', xsT[:, :, blk * 128:(blk + 1) * 128], pstb.rearrange("p (c t) -> p c t", c=8), [kp], [kxT])
                dst2[e_] = (wg, kwg, wu, kwu, wd, kwd, xsT, kxT)

            def d_gateup(e_):
                wg, kwg, wu, kwu, wd, kwd, xsT, kxT = dst2[e_]
                aT, kaT = aT_r.next()
                dst2[e_] = (wd, kwd, aT, kaT)
                for ft in range(2):
                    psg_, kpg_ = kb.psum(); psu_, kpu_ = kb.psum()
                    for c in range(8):
                        kb.mm(psg_[:, 0:CAP], wg[:, c, ft * 128:(ft + 1) * 128], xsT[:, c, :], c == 0, c == 7, [kwg, kxT], [kpg_])
                    for c in range(8):
                        kb.mm(psu_[:, 0:CAP], wu[:, c, ft * 128:(ft + 1) * 128], xsT[:, c, :], c == 0, c == 7, [kwu, kxT], [kpu_])
                    sg, ksg = sg_r.next()
                    kb.act(sg[:], psg_[:, 0:CAP], AF.Silu, [kpg_], [ksg])
                    kb.tt('dve', aT[:, ft, :], sg[:], psu_[:, 0:CAP], ALU.mult, [ksg, kpu_], [kaT])

            def d_down(e_):
                wd, kwd, aT, kaT = dst2.pop(e_)
                for blk in range(NBLK):
                    yb, kyb = yb_r.next()
                    for half in range(2):
                        psy, kpy = kb.psum()
                        for ft in range(2):
                            kb.mm(psy[:, :], aT[:, ft, blk * 128:(blk + 1) * 128], wd[:, ft, half * 512:(half + 1) * 512], ft == 0, ft == 1, [kaT, kwd], [kpy])
                        kb.copy('act' if half == 0 else 'dve', yb[:, half * 512:(half + 1) * 512], psy[:, :], [kpy], [kyb])
                    r0 = e_ * CAP + blk * 128
                    kb.dma(ys_s[r0:r0 + 128, :], yb[:], r=[kyb], w=['ys_s'])

            d_load(0)
            d_load(1)
            d_tr(0)
            for e_ in range(NEXP):
                if e_ + 2 < NEXP:
                    d_load(e_ + 2)
                d_gateup(e_)
                if e_ + 1 < NEXP:
                    d_tr(e_ + 1)
                d_down(e_)
        P.barrier()

        with ExitStack() as se_:
            sbe = lambda name, shape, dt: kb.sb(se_, name, shape, dt)
            kb.ps = [se_.enter_context(nc.psum_tensor('psE%d' % k, [128, 512], F32)) for k in range(8)]
            wpg = sbe('wpg', [128, 8, 1024], BF16); wple = sbe('wple', [128, 2, 1024], BF16)
            kb.dma(wpg[:], w_pg_d.rearrange("(c p) n -> p c n", p=128), w=['wpg'], q='pool')
            kb.dma(wple[:], w_ple_d.rearrange("(c p) n -> p c n", p=128), w=['wple'], q='pool')
            gple = sbe('gple', [128, 1024], F32); gfin = sbe('gfin', [128, 1024], F32)
            kb.dma(gple[:], gple_d, w=['gple']); kb.dma(gfin[:], gfin_d, w=['gfin'])
            x1_r = Ring(kb, se_, 'x1e', [128, 1024], F32, 4)
            y_r = Ring(kb, se_, 'ye', [128, 1024], F32, 8)
            x2_r = Ring(kb, se_, 'x2e', [128, 1024], F32, 3)
            hp_r = Ring(kb, se_, 'hpe', [128, 1024], BF16, 3)
            hpT_r = Ring(kb, se_, 'hpT', [128, 8, 128], BF16, 3)
            jk_r = Ring(kb, se_, 'jke', [128, 1024], BF16, 2)
            st_r = Ring(kb, se_, 'ste', [128, 4], F32, 4)
            sgm_r = Ring(kb, se_, 'sgm', [128, 1024], F32, 2)
            pt_r2 = Ring(kb, se_, 'pte', [128, 256], F32, 4)
            pb_r = Ring(kb, se_, 'pbe', [128, 256], BF16, 2)
            pT_r = Ring(kb, se_, 'pTe', [128, 2, 128], BF16, 3)
            x3_r = Ring(kb, se_, 'x3e', [128, 1024], F32, 2)
            o_r = Ring(kb, se_, 'oe', [128, 1024], F32, 2)
            est = {}

            def e_load(ob):
                x1t, kx1 = x1_r.next(); y1, ky1 = y_r.next(); y2, ky2 = y_r.next(); ptile, kpt_ = pt_r2.next()
                kb.dma(x1t[:], x1_s[ob * 128:(ob + 1) * 128, :], r=[('x1_s', ob)], w=[kx1])
                for k2, (yt, kyt) in enumerate(((y1, ky1), (y2, ky2))):
                    kb.P.add('pool', (lambda ob, k2, yt: lambda e: e.indirect_dma_start(
                        out=yt[:], out_offset=None, in_=ys_s[:, :],
                        in_offset=bass.IndirectOffsetOnAxis(ap=desti[:, ob, k2:k2 + 1], axis=0)))(ob, k2, yt), ['ys_s', ('desti', ob)], [kyt], dma=True)
                kb.dma(ptile[:], pvd[ob * 128:(ob + 1) * 128, :], w=[kpt_])
                est[ob] = (x1t, kx1, y1, ky1, y2, ky2, ptile, kpt_)

            def e_compute(ob):
                x1t, kx1, y1, ky1, y2, ky2, ptile, kpt_ = est.pop(ob)
                x2, kx2 = x2_r.next()
                kb.stt(x2[:], y1[:], route[:, ob, 0:1], x1t[:], ALU.mult, ALU.add, [ky1, ('route', ob), kx1], [kx2])
                kb.stt(x2[:], y2[:], route[:, ob, 1:2], x2[:], ALU.mult, ALU.add, [ky2, ('route', ob), kx2], [kx2])
                jk, kjk = jk_r.next(); stk = st_r.next()
                rinv = rms_rinv(stk, x2, kx2, jk, kjk, pool_pow=True)
                hp, khp = hp_r.next()
                kb.stt(hp[:], x2[:], rinv, gple[:], ALU.mult, ALU.mult, [kx2, stk[1], 'gple'], [khp])
                est1[ob] = (x2, kx2, hp, khp, ptile, kpt_)

            def e_compute1b(ob):
                x2, kx2, hp, khp, ptile, kpt_ = est1.pop(ob)
                hpT, khpT = hpT_r.next()
                pst, kp = kb.psum()
                pstb = pst[:].bitcast(BF16)
                for c in range(8):
                    kb.tr(pstb[:, c * 128:(c + 1) * 128], hp[:, c * 128:(c + 1) * 128], identB[:], [khp, 'identB'], [kp])
                kb.copy('act', hpT[:].rearrange("p c t -> p (c t)"), pstb, [kp], [khpT])
                pbt, kpb = pb_r.next(); pT, kpT = pT_r.next()
                kb.copy('act', pbt[:], ptile[:], [kpt_], [kpb])
                pst, kp = kb.psum()
                pstb = pst[:].bitcast(BF16)
                for c in range(2):
                    kb.tr(pstb[:, c * 128:(c + 1) * 128], pbt[:, c * 128:(c + 1) * 128], identB[:], [kpb, 'identB'], [kp])
                kb.copy('dve', pT[:].rearrange("p c t -> p (c t)"), pstb[:, 0:256], [kp], [kpT])
                est2[ob] = (x2, kx2, hpT, khpT, pT, kpT)

            def e_compute2(ob):
                x2, kx2, hpT, khpT, pT, kpT = est2.pop(ob)
                sgm, ksg = sgm_r.next(); x3, kx3 = x3_r.next()
                for half in range(2):
                    hs = slice(half * 512, (half + 1) * 512)
                    psg_, kpg_ = kb.psum()
                    for c in range(8):
                        kb.mm(psg_[:, :], hpT[:, c, :], wpg[:, c, hs], c == 0, c == 7, [khpT, 'wpg'], [kpg_])
                    kb.act(sgm[:, hs], psg_[:, :], AF.Sigmoid, [kpg_], [ksg])
                    psp, kpp = kb.psum()
                    for c in range(2):
                        kb.mm(psp[:, :], pT[:, c, :], wple[:, c, hs], c == 0, c == 1, [kpT, 'wple'], [kpp])
                    kb.tt('dve', x3[:, hs], psp[:, :], sgm[:, hs], ALU.mult, [kpp, ksg], [kx3])
                kb.tt('dve', x3[:], x3[:], x2[:], ALU.add, [kx3, kx2], [kx3])
                jk, kjk = jk_r.next(); stk = st_r.next()
                rinv = rms_rinv(stk, x3, kx3, jk, kjk, pool_pow=True)
                ot, kot = o_r.next()
                kb.stt(ot[:], x3[:], rinv, gfin[:], ALU.mult, ALU.mult, [kx3, stk[1], 'gfin'], [kot])
                kb.dma(out_d[ob * 128:(ob + 1) * 128, :], ot[:], r=[kot], w=['out'])

            est2 = {}
            est1 = {}
            e_load(0)
            if NOB > 1:
                e_load(1)
            e_compute(0)
            e_compute1b(0)
            for ob in range(NOB):
                if ob + 2 < NOB:
                    e_load(ob + 2)
                if ob + 1 < NOB:
                    e_compute(ob + 1)
                e_compute2(ob)
                if ob + 1 < NOB:
                    e_compute1b(ob + 1)

        P.add('sp', None, reads=['out'] + [k for k in dbgo.keys() if k != 'd_mlo'] + ([('d_mlo', gob) for gob in range(NOB)] if dbg else []))
        P.emit(top)
    return nc, D, dbgo


def _consts():
    c = {}
    c['ident'] = np.eye(128, dtype=np.float32)
    i = np.arange(128)
    c['trilT'] = (i[:, None] <= i[None, :]).astype(np.float32)
    c['attmask'] = ((i[:, None] // 64) <= (i[None, :] // 64)).astype(np.float32)
    pb = np.zeros((96, 96), np.float32)
    for j in range(16):
        pb[64 + j + 16, 64 + j] = -1.0
        pb[64 + j, 64 + j + 16] = 1.0
    c['permB'] = pb
    inv = (np.float32(1.0) / (np.float32(10000.0) ** (np.arange(0, 32, 2, dtype=np.float32) / np.float32(32)))).astype(np.float32)
    f = np.zeros((96, 1), np.float32)
    f[64:80, 0] = inv
    f[80:96, 0] = inv
    c['invfreq'] = f
    dm = np.zeros((4, 4, GB), np.float32)
    for k in range(4):
        dm[k, k, :] = 1.0
    c['dmask'] = dm.reshape(4, 4 * GB)
    c['ustrict'] = (i[:, None] < i[None, :]).astype(np.float32)
    c['iota32'] = np.broadcast_to(np.arange(32, dtype=np.float32), (128, 32)).copy()
    return c


def _prep_weights(I):
    f = lambda k: np.asarray(I[k], dtype=np.float32)
    w = {}
    w_in = f('w_in')[0]
    offs = np.cumsum([0, 256, 128, 32, 512, 512, 4, 4])
    cq, ckv, kr, xm, z, gi, gf = [w_in[:, offs[i]:offs[i + 1]] for i in range(7)]
    w['w_in_r'] = np.ascontiguousarray(np.concatenate(
        [ckv, xm, np.zeros((1024, 64), np.float32), kr, gi, gf, cq, z], axis=1))
    assert w['w_in_r'].shape[1] == NCOL
    w['gmix_col'] = np.ascontiguousarray(f('norm_mix_g')[0].reshape(8, 128).T)
    w['gq_col'] = np.ascontiguousarray(f('q_norm_g')[0].reshape(2, 128).T)
    w['gkv_col'] = np.ascontiguousarray(f('kv_norm_g')[0].reshape(128, 1))
    w['w_uq'] = f('w_uq')[0]
    w['w_ukv'] = f('w_ukv')[0]
    cw = f('conv_w')[0]
    w['convw_col'] = np.ascontiguousarray(cw.reshape(4, 4, 128).transpose(2, 1, 0).reshape(128, 16))
    w['convb_col'] = np.ascontiguousarray(f('conv_b')[0].reshape(4, 128).T)
    w['w_mq'] = f('w_mq')[0]; w['w_mk'] = f('w_mk')[0]; w['w_mv'] = f('w_mv')[0]
    w['bi_col'] = f('b_igate')[0].reshape(4, 1); w['bf_col'] = f('b_fgate')[0].reshape(4, 1)
    w['gmh_col'] = np.ascontiguousarray(f('mh_norm_g')[0].reshape(4, 128).T)
    w['skip_col'] = np.ascontiguousarray(f('ml_skip')[0].reshape(4, 128).T)
    w['w_o'] = f('w_o')[0]
    rep = lambda v: np.ascontiguousarray(np.broadcast_to(v.reshape(1, -1), (128, v.size)))
    w['gffn_rep'] = rep(f('norm_ffn_g')[0])
    w['w_router'] = np.ascontiguousarray(np.concatenate([f('w_router_group')[0], f('w_router_expert')[0]], axis=1))
    w['b_router_rep'] = rep(np.concatenate([f('b_router_group')[0], f('b_router_expert')[0]]))
    w['w_gate_e'] = f('w_gate_e')[0]; w['w_up_e'] = f('w_up_e')[0]; w['w_down_e'] = f('w_down_e')[0]
    w['gple_rep'] = rep(f('norm_ple_g')[0])
    w['w_ple'] = f('w_ple')[0]; w['w_ple_gate'] = f('w_ple_gate')[0]
    w['gfin_rep'] = rep(f('final_norm_g'))
    return w


def _prep_core(I, b, r, NVB):
    S = NVB * 128
    x = np.asarray(I['x'], dtype=np.float32)[b]
    pos = np.asarray(I['positions'])[b].astype(np.int32)
    p = np.asarray(I['p'], dtype=np.float32)[0, b]
    m = {}
    if r == 1:
        xv = x[:S]; posv = pos[:S]; valid = np.ones(S, np.float32); first_real = 0
    else:
        xv = np.concatenate([np.zeros((128, 1024), np.float32), x[:S - 128]], 0)
        posv = np.concatenate([np.zeros(128, np.int32), pos[:S - 128]])
        valid = np.concatenate([np.zeros(128, np.float32), np.ones(S - 128, np.float32)])
    own = np.zeros(S, bool)
    for vb in range(1, NVB, 2):
        own[vb * 128:(vb + 1) * 128] = True
    real_idx = np.arange(S) - (0 if r == 1 else 128)
    m['xv'] = np.ascontiguousarray(xv)
    m['posk'] = np.ascontiguousarray(np.broadcast_to(posv[None, :], (32, S)))
    m['posq'] = np.ascontiguousarray(np.broadcast_to(posv[own][None, :], (32, S // 2)))
    m['pv'] = np.ascontiguousarray(p[real_idx[own]])
    m['valid4'] = np.ascontiguousarray(np.broadcast_to(valid[None, :], (4, S)))
    m['ibias4'] = np.ascontiguousarray(np.broadcast_to(((valid - 1.0) * 1e4)[None, :], (4, S))).astype(np.float32)
    m['kbias'] = ((valid - 1.0) * 3e4).reshape(1, S).astype(np.float32)
    return m, real_idx[own]


_CACHE = {}


def kernel(**inputs):
    NVB = 64
    if 'nc' not in _CACHE:
        _CACHE['nc'] = build(NVB)
    nc, D, _ = _CACHE['nc']
    consts = _consts()
    w = _prep_weights(inputs)
    in_maps = []
    idx = []
    for core in range(8):
        b, r = core // 2, core % 2
        m, ridx = _prep_core(inputs, b, r, NVB)
        m.update(consts)
        m.update(w)
        in_maps.append({k: v for k, v in m.items() if k in D})
        idx.append((b, ridx))
    res = run_bass_kernel_spmd(nc, in_maps, core_ids=list(range(8)))
    out = np.zeros((4, 8192, 1024), np.float32)
    for core in range(8):
        b, ridx = idx[core]
        out[b, ridx] = np.asarray(res.results[core]['out'], dtype=np.float32)
    return out
```
